# Optimizing a Trainium2 kernel written in Bass

```python
import jax, jax.numpy as jnp
from jax import lax
import numpy as np

D_MODEL = 1024
BATCH = 4
SEQ = 8192
DEPTH = 2

CHUNK = 64
Q_BLOCK = 128
HEAD_DIM = 64
ATTN_HEADS = D_MODEL // 128
CONV_GROUPS = D_MODEL // 256
HGRN_HEADS = D_MODEL // 256
CONV_WIDTH = 3
ATTN_W = ATTN_HEADS * HEAD_DIM
CONV_C = CONV_GROUPS * HEAD_DIM
HGRN_W = HGRN_HEADS * HEAD_DIM
D_MIX = ATTN_W + CONV_C + HGRN_W
IN_SIZES = (ATTN_W, ATTN_W, ATTN_W, ATTN_HEADS,
            CONV_C, CONV_C, CONV_C,
            HGRN_W, HGRN_W, HGRN_W, HGRN_W)
D_IN = sum(IN_SIZES)
IN_SPLITS = [int(v) for v in np.cumsum(IN_SIZES)[:-1]]
D_FF = (7 * D_MODEL) // 2
N_EXPERTS = 8
TOP_K = 2
N_DENSE = (DEPTH + 1) // 2
N_MOE = DEPTH // 2
EPS = 1e-6
FORGET_BIAS_MEAN = 3.0
MASK_VALUE = -1e30
MASK_LOG_DECAY = -1e4
TINY = 1e-30

kernel_name = "hybrid_fox_shortconv_hgrn2_moe"


def rms(x):
    xf = x.astype(jnp.float32)
    return xf * lax.rsqrt(jnp.mean(xf * xf, axis=-1, keepdims=True) + EPS)


def rms_norm(x, g):
    return (rms(x) * g.astype(jnp.float32)).astype(x.dtype)


def swiglu(x, w_gate, w_up, w_down):
    return (jax.nn.silu(x @ w_gate) * (x @ w_up)) @ w_down


def forgetting_attention(q, k, v, f_logit, g_q, g_k):
    B, S, H, Dh = q.shape
    q = rms(q) * g_q.astype(jnp.float32)
    k = rms(k) * g_k.astype(jnp.float32)
    v = v.astype(jnp.float32)
    dcum = jnp.cumsum(jax.nn.log_sigmoid(f_logit.astype(jnp.float32)), axis=1)
    scale = np.float32(1.0 / np.sqrt(Dh))
    nb = S // Q_BLOCK
    q_blocks = q.reshape(B, nb, Q_BLOCK, H, Dh).transpose(1, 0, 3, 2, 4)
    d_blocks = dcum.reshape(B, nb, Q_BLOCK, H).transpose(1, 0, 3, 2)
    k_t = k.transpose(0, 2, 1, 3)
    v_t = v.transpose(0, 2, 1, 3)
    d_k = dcum.transpose(0, 2, 1)
    k_pos = jnp.arange(S)

    def block(args):
        q_i, d_i, i = args
        q_pos = i * Q_BLOCK + jnp.arange(Q_BLOCK)
        logits = (jnp.einsum('bhqd,bhkd->bhqk', q_i, k_t) * scale
                  + d_i[..., None] - d_k[:, :, None, :])
        mask = k_pos[None, :] <= q_pos[:, None]
        p = jax.nn.softmax(jnp.where(mask, logits, MASK_VALUE), axis=-1)
        return jnp.einsum('bhqk,bhkd->bhqd', p, v_t)

    o = lax.map(block, (q_blocks, d_blocks, jnp.arange(nb)))
    return o.transpose(1, 0, 3, 2, 4).reshape(B, S, H * Dh)


def short_conv(x_in, b_gate, c_gate, w):
    S = x_in.shape[1]
    u = (c_gate * x_in).astype(jnp.float32)
    up = jnp.pad(u, ((0, 0), (CONV_WIDTH - 1, 0), (0, 0)))
    wf = w.astype(jnp.float32)
    y = up[:, 0:S] * wf[0]
    for j in range(1, CONV_WIDTH):
        y = y + up[:, j:j + S] * wf[j]
    return b_gate.astype(jnp.float32) * y


def hgrn2(q, f_logit, i_in, lb):
    B, S, H, d = q.shape
    q = jax.nn.silu(q.astype(jnp.float32))
    z = f_logit.astype(jnp.float32)
    lb = lb.reshape(H, d).astype(jnp.float32)
    f = lb + (1.0 - lb) * jax.nn.sigmoid(z)
    log_f = jnp.log(jnp.maximum(f, TINY))
    k = (1.0 - lb) * jax.nn.sigmoid(-z)
    v = i_in.astype(jnp.float32)
    n_chunks = S // CHUNK

    def to_chunks(t):
        return t.reshape(B, n_chunks, CHUNK, H, d).transpose(1, 0, 3, 2, 4)

    causal = jnp.tril(jnp.ones((CHUNK, CHUNK), dtype=bool))

    def step(state, inp):
        q_c, k_c, v_c, l_c = inp
        c = jnp.cumsum(l_c, axis=2)
        o_inter = jnp.einsum('bhtk,bhkv->bhtv', q_c * jnp.exp(c), state)
        diff = c[:, :, :, None, :] - c[:, :, None, :, :]
        decay = jnp.exp(jnp.where(causal[:, :, None], diff, MASK_LOG_DECAY))
        a = jnp.einsum('bhtk,bhsk,bhtsk->bhts', q_c, k_c, decay)
        o = o_inter + jnp.einsum('bhts,bhsv->bhtv', a, v_c)
        c_last = c[:, :, -1:, :]
        k_dec = k_c * jnp.exp(c_last - c)
        state = (jnp.exp(c_last[:, :, 0, :])[..., None] * state
                 + jnp.einsum('bhsk,bhsv->bhkv', k_dec, v_c))
        return state, o

    state0 = jnp.zeros((B, H, d, d), jnp.float32)
    _, o = lax.scan(step, state0, (to_chunks(q), to_chunks(k), to_chunks(v), to_chunks(log_f)))
    return o.transpose(1, 0, 3, 2, 4).reshape(B, S, H * d)


def moe_swiglu(h, w_router, b_router, w_gate, w_up, w_down):
    B, S, D = h.shape
    t = h.reshape(B * S, D)
    logits = (t @ w_router).astype(jnp.float32) + b_router.astype(jnp.float32)
    top_v, top_i = lax.top_k(logits, TOP_K)
    top_w = jax.nn.softmax(top_v, axis=-1)
    gates = jnp.sum(jax.nn.one_hot(top_i, N_EXPERTS, dtype=jnp.float32) * top_w[..., None], axis=1)
    out = jnp.zeros_like(t)
    for e in range(N_EXPERTS):
        out = out + gates[:, e:e + 1].astype(t.dtype) * swiglu(t, w_gate[e], w_up[e], w_down[e])
    return out.reshape(B, S, D)


def setup_inputs(seed: int = 0) -> dict:
    key = jax.random.key(seed)
    ks = jax.random.split(key, 24)
    f32 = jnp.float32
    nrm = lambda k, shape, s: jax.random.normal(k, shape, f32) * s
    return {
        "x": nrm(ks[0], (BATCH, SEQ, D_MODEL), 1.0),
        "norm_mix": 1.0 + nrm(ks[1], (DEPTH, D_MODEL), 0.02),
        "w_in": nrm(ks[2], (DEPTH, D_MODEL, D_IN), D_MODEL ** -0.5),
        "attn_f_bias": FORGET_BIAS_MEAN + nrm(ks[3], (DEPTH, ATTN_HEADS), 0.1),
        "q_norm_gain": 1.0 + nrm(ks[4], (DEPTH, HEAD_DIM), 0.02),
        "k_norm_gain": 1.0 + nrm(ks[5], (DEPTH, HEAD_DIM), 0.02),
        "conv_w": nrm(ks[6], (DEPTH, CONV_WIDTH, CONV_C), CONV_WIDTH ** -0.5),
        "hgrn_lb_logits": nrm(ks[7], (DEPTH, HGRN_W), 0.1),
        "mix_out_gain": 1.0 + nrm(ks[8], (DEPTH, D_MIX), 0.02),
        "w_out": nrm(ks[9], (DEPTH, D_MIX, D_MODEL), D_MIX ** -0.5),
        "norm_ffn": 1.0 + nrm(ks[10], (DEPTH, D_MODEL), 0.02),
        "ffn_w_gate": nrm(ks[11], (N_DENSE, D_MODEL, D_FF), D_MODEL ** -0.5),
        "ffn_w_up": nrm(ks[12], (N_DENSE, D_MODEL, D_FF), D_MODEL ** -0.5),
        "ffn_w_down": nrm(ks[13], (N_DENSE, D_FF, D_MODEL), D_FF ** -0.5),
        "moe_router_w": nrm(ks[14], (N_MOE, D_MODEL, N_EXPERTS), D_MODEL ** -0.5),
        "moe_router_b": nrm(ks[15], (N_MOE, N_EXPERTS), 0.01),
        "moe_w_gate": nrm(ks[16], (N_MOE, N_EXPERTS, D_MODEL, D_FF), D_MODEL ** -0.5),
        "moe_w_up": nrm(ks[17], (N_MOE, N_EXPERTS, D_MODEL, D_FF), D_MODEL ** -0.5),
        "moe_w_down": nrm(ks[18], (N_MOE, N_EXPERTS, D_FF, D_MODEL), D_FF ** -0.5),
    }


def reference(x, norm_mix, w_in, attn_f_bias, q_norm_gain, k_norm_gain, conv_w,
              hgrn_lb_logits, mix_out_gain, w_out, norm_ffn, ffn_w_gate, ffn_w_up,
              ffn_w_down, moe_router_w, moe_router_b, moe_w_gate, moe_w_up, moe_w_down):
    B, S, _ = x.shape
    dt = x.dtype
    p_lb = jax.nn.softmax(hgrn_lb_logits.astype(jnp.float32), axis=0)
    lb_all = jnp.cumsum(p_lb, axis=0) - p_lb[0]
    for l in range(DEPTH):
        h = rms_norm(x, norm_mix[l])
        z = h @ w_in[l]
        (a_q, a_k, a_v, a_f, c_x, c_b, c_c,
         r_q, r_f, r_i, r_g) = jnp.split(z, IN_SPLITS, axis=-1)
        a_f = a_f + attn_f_bias[l].astype(dt)
        y_a = forgetting_attention(a_q.reshape(B, S, ATTN_HEADS, HEAD_DIM),
                                   a_k.reshape(B, S, ATTN_HEADS, HEAD_DIM),
                                   a_v.reshape(B, S, ATTN_HEADS, HEAD_DIM),
                                   a_f, q_norm_gain[l], k_norm_gain[l])
        y_c = short_conv(c_x, c_b, c_c, conv_w[l])
        y_r = hgrn2(r_q.reshape(B, S, HGRN_HEADS, HEAD_DIM),
                    r_f.reshape(B, S, HGRN_HEADS, HEAD_DIM),
                    r_i.reshape(B, S, HGRN_HEADS, HEAD_DIM), lb_all[l])
        y = jnp.concatenate([y_a, y_c, y_r], axis=-1)
        y = rms(y.reshape(B, S, D_MIX // HEAD_DIM, HEAD_DIM)).reshape(B, S, D_MIX)
        y = y * mix_out_gain[l].astype(jnp.float32)
        y = jnp.concatenate([y[..., :ATTN_W + CONV_C],
                             y[..., ATTN_W + CONV_C:] * jax.nn.silu(r_g.astype(jnp.float32))], axis=-1)
        x = x + y.astype(dt) @ w_out[l]
        h = rms_norm(x, norm_ffn[l])
        if l % 2 == 0:
            j = l // 2
            x = x + swiglu(h, ffn_w_gate[j], ffn_w_up[j], ffn_w_down[j])
        else:
            j = l // 2
            x = x + moe_swiglu(h, moe_router_w[j], moe_router_b[j],
                               moe_w_gate[j], moe_w_up[j], moe_w_down[j])
    return x
```

```python
import numpy as np
from contextlib import ExitStack
import concourse.bass as bass
import concourse.mybir as mybir
from concourse.bass_utils import run_bass_kernel_spmd

F32 = mybir.dt.float32
BF16 = mybir.dt.bfloat16
AF = mybir.ActivationFunctionType
ALU = mybir.AluOpType

D = 1024
DIN = 3336
DFF = 3584
NE = 8
EPS = 1e-6
TINY = 1e-30
C_Q, C_K, C_V, C_F = 0, 512, 1024, 1536
C_CX, C_CB, C_CC = 1544, 1800, 2056
C_RQ, C_RF, C_RI, C_RG = 2312, 2568, 2824, 3080

K_ID, K_BLK, K_TRI, K_ONES, K_SEL = 0, 128, 256, 384, 512
K_DM = 640
K_CM = K_DM + 2048
K_SEG = K_CM + 64
NCONST = K_SEG + 512

P_G1, P_G2, P_GQ, P_GK, P_CW, P_LB, P_MG, P_FB, P_RB, P_GQR, P_GKR = 0, 8, 16, 17, 18, 24, 28, 36, 44, 52, 116
NPAR = 180


def make_consts():
    c = np.zeros((128, NCONST), np.float32)
    p = np.arange(128)
    c[:, K_ID:K_ID + 128] = np.eye(128)
    c[:, K_BLK:K_BLK + 128] = ((p[:, None] // 64) == (p[None, :] // 64)) / 64.0
    c[:, K_TRI:K_TRI + 128] = (p[:, None] <= p[None, :])
    c[:, K_ONES:K_ONES + 128] = 1.0
    c[127, K_SEL:K_SEL + 128] = 1.0
    q = np.arange(512)
    for j in range(4):
        c[:, K_DM + j * 512:K_DM + (j + 1) * 512] = ((j * 128 + p[:, None]) <= q[None, :])
    t = np.arange(64)
    c[:, K_CM:K_CM + 64] = ((p[:, None] % 64) <= t[None, :])
    c[:, K_SEG:K_SEG + 512] = ((q % 64) != 0)[None, :]
    return c


def make_params(inp):
    L = 2
    P = np.zeros((L, 128, NPAR), np.float32)
    for l in range(L):
        P[l, :, P_G1:P_G1 + 8] = inp["norm_mix"][l].reshape(8, 128).T
        P[l, :, P_G2:P_G2 + 8] = inp["norm_ffn"][l].reshape(8, 128).T
        P[l, :, P_GQ] = np.tile(inp["q_norm_gain"][l], 2)
        P[l, :, P_GK] = np.tile(inp["k_norm_gain"][l], 2)
        P[l, :, P_CW:P_CW + 6] = inp["conv_w"][l].reshape(3, 2, 128).transpose(2, 0, 1).reshape(128, 6)
        P[l, :, P_LB:P_LB + 4] = inp["hgrn_lb_logits"].reshape(2, 2, 128).transpose(2, 0, 1).reshape(128, 4)
        P[l, :, P_MG:P_MG + 8] = inp["mix_out_gain"][l].reshape(8, 128).T
        P[l, :, P_FB:P_FB + 8] = inp["attn_f_bias"][l][None, :]
        P[l, :, P_RB:P_RB + 8] = inp["moe_router_b"][0][None, :]
        P[l, :, P_GQR:P_GQR + 64] = inp["q_norm_gain"][l][None, :]
        P[l, :, P_GKR:P_GKR + 64] = inp["k_norm_gain"][l][None, :]
    return P


class Buf:
    __slots__ = ("t", "w", "r", "nowaw")

    def __init__(self, t, nowaw=False):
        self.t = t
        self.w = {}
        self.r = {}
        self.nowaw = nowaw

    def __getitem__(self, idx):
        return self.t.ap()[idx]


class KB:
    def __init__(self, nc):
        self.nc = nc
        self.eng = {"pe": nc.tensor, "act": nc.scalar, "dve": nc.vector, "pool": nc.gpsimd, "sp": nc.sync}
        self.sems = {}
        self.cnt = {}
        self.waited = {e: {} for e in self.eng}
        self.nsem = 0
        self.stack = None
        self.ncall = 0
        import os as _os
        self.noself = _os.environ.get('NOSELF', '0') == '1'
        import os
        self.cut = int(os.environ['KCUT']) if 'KCUT' in os.environ else None
        for e in self.eng:
            self._newsem(e)

    def _newsem(self, key):
        self.nsem += 1
        self.sems[key] = self.nc.alloc_semaphore(name="s%d_%s" % (self.nsem, key.replace(":", "_")))
        self.cnt[key] = 0

    def sbuf(self, name, shape, dt):
        if self.stack is not None:
            return Buf(self.stack.enter_context(self.nc.sbuf_tensor(name, list(shape), dt)))
        return Buf(self.nc.alloc_sbuf_tensor(name, list(shape), dt))

    def psum(self, name, shape, dt):
        return Buf(self.nc.alloc_psum_tensor(name, list(shape), dt))

    def dram(self, name, shape, dt, kind=None):
        if kind is None:
            t = self.nc.dram_tensor(name, list(shape), dt)
        else:
            t = self.nc.dram_tensor(name, list(shape), dt, kind=kind)
        return Buf(t, nowaw=True)

    def _wait(self, en, toks):
        e = self.eng[en]
        wd = self.waited[en]
        for k, v in toks.items():
            if en == "pe" and k == "pe":
                continue
            if self.noself and k == en:
                continue
            if wd.get(k, 0) < v:
                e.wait_ge(self.sems[k], v)
                wd[k] = v

    def _deps(self, r, w):
        toks = {}
        for b in r:
            for k, v in b.w.items():
                if toks.get(k, 0) < v:
                    toks[k] = v
        for b in w:
            for k, v in b.r.items():
                if toks.get(k, 0) < v:
                    toks[k] = v
            if not b.nowaw:
                for k, v in b.w.items():
                    if toks.get(k, 0) < v:
                        toks[k] = v
        return toks

    def _mark(self, key, val, r, w):
        for b in r:
            if b.r.get(key, 0) < val:
                b.r[key] = val
        for b in w:
            if b.nowaw:
                if b.w.get(key, 0) < val:
                    b.w[key] = val
            else:
                b.w = {key: val}
            b.r = {}

    def op(self, en, fn, r=(), w=(), inc=True):
        self.ncall += 1
        if self.cut is not None and self.ncall > self.cut:
            return None
        self._wait(en, self._deps(r, w))
        inst = fn(self.eng[en])
        if inc:
            self.cnt[en] += 1
            inst.then_inc(self.sems[en], 1)
            val = self.cnt[en]
        else:
            val = self.cnt[en] + 1
        self._mark(en, val, r, w)
        return inst

    def dma(self, q, stream, out, in_, r=(), w=(), **kw):
        key = "d:" + stream
        self.ncall += 1
        if self.cut is not None and self.ncall > self.cut:
            return None
        if key not in self.sems:
            self._newsem(key)
        self._wait(q, self._deps(r, w))
        inst = self.eng[q].dma_start(out=out, in_=in_, **kw)
        self.cnt[key] += 16
        inst.then_inc(self.sems[key], 16)
        self._mark(key, self.cnt[key], r, w)
        return inst

    def barrier(self):
        for en in self.eng:
            self._wait(en, dict(self.cnt))

    def finish(self):
        self._wait("sp", dict(self.cnt))


def build(T, L=2, debug=False, stop=None):
    assert T % 512 == 0
    NB = T // 512
    NS = T // 128
    TBC = min(1024, T)
    NSC = TBC // 128
    nc = bass.Bass("TRN2", target_bir_lowering=False)
    kb = KB(nc)

    def ext_in(name, shape):
        return Buf(nc.dram_tensor(name, list(shape), F32, kind="ExternalInput"), nowaw=True)

    xin = ext_in("x", [T, D])
    consts_d = ext_in("consts", [128, NCONST])
    params_d = ext_in("params", [L, 128, NPAR])
    w_in_d = ext_in("w_in", [L, D, DIN])
    w_out_d = ext_in("w_out", [L, D, D])
    ffn_g_d = ext_in("ffn_w_gate", [1, D, DFF])
    ffn_u_d = ext_in("ffn_w_up", [1, D, DFF])
    ffn_d_d = ext_in("ffn_w_down", [1, DFF, D])
    wr_d = ext_in("moe_router_w", [1, D, NE])
    moe_g_d = ext_in("moe_w_gate", [1, NE, D, DFF])
    moe_u_d = ext_in("moe_w_up", [1, NE, D, DFF])
    moe_d_d = ext_in("moe_w_down", [1, NE, DFF, D])
    yout = Buf(nc.dram_tensor("y", [T, D], F32, kind="ExternalOutput"), nowaw=True)

    dbg = {}

    def scratch(name, shape, dt):
        if debug:
            b = Buf(nc.dram_tensor(name, list(shape), dt, kind="ExternalOutput"), nowaw=True)
            dbg[name] = b
            return b
        return kb.dram(name, shape, dt)

    qT_d = scratch("qT_s", [512, T], BF16)
    kT_d = scratch("kT_s", [512, T], BF16)
    v_d = scratch("v_s", [T, 512], BF16)
    yT_d = scratch("yT_s", [1024, T], BF16)
    xmid_d = scratch("xmid_s", [T, D], F32)

    cst = kb.sbuf("cst", [128, NCONST], F32)
    par = kb.sbuf("par", [128, L, NPAR], F32)
    identb = kb.sbuf("identb", [128, 128], BF16)
    blkb = kb.sbuf("blkb", [128, 128], BF16)
    onesb = kb.sbuf("onesb", [128, 128], BF16)
    dmaskb = kb.sbuf("dmaskb", [128, 4, 512], BF16)
    cmaskb = kb.sbuf("cmaskb", [128, 64], F32)
    epsc = kb.sbuf("epsc", [128, 1], F32)
    onec = kb.sbuf("onec", [128, 1], F32)
    lsig = kb.sbuf("lsig", [128, NS, 8], F32)
    negd = kb.sbuf("negd", [128, NS, 8], F32)
    cI = kb.sbuf("cI", [128, NS, 8], F32)
    lbt = kb.sbuf("lbt", [128, 2], F32)
    omlt = kb.sbuf("omlt", [128, 2], F32)
    nomlt = kb.sbuf("nomlt", [128, 2], F32)
    bshift = kb.sbuf("bshift", [128, 1], F32)
    small = kb.sbuf("small", [128, 64], F32)

    ps = [kb.psum("ps%d" % i, [128, 512], F32) for i in range(8)]

    kb.dma("sp", "ld", cst[:, :], consts_d[:, :], w=[cst])
    kb.dma("sp", "ld", par[:, :, :], params_d.t.ap().rearrange("l p n -> p l n"), w=[par])
    kb.op("dve", lambda e: e.tensor_copy(out=identb[:, :], in_=cst[:, K_ID:K_ID + 128]), r=[cst], w=[identb])
    kb.op("dve", lambda e: e.tensor_copy(out=blkb[:, :], in_=cst[:, K_BLK:K_BLK + 128]), r=[cst], w=[blkb])
    kb.op("dve", lambda e: e.tensor_copy(out=onesb[:, :], in_=cst[:, K_ONES:K_ONES + 128]), r=[cst], w=[onesb])
    kb.op("dve", lambda e: e.tensor_copy(out=dmaskb[:, :, :], in_=cst[:, K_DM:K_DM + 2048].rearrange("p (j q) -> p j q", q=512)), r=[cst], w=[dmaskb])
    kb.op("dve", lambda e: e.tensor_copy(out=cmaskb[:, :], in_=cst[:, K_CM:K_CM + 64]), r=[cst], w=[cmaskb])
    kb.op("dve", lambda e: e.memset(epsc[:, :], EPS), w=[epsc])
    kb.op("dve", lambda e: e.memset(onec[:, :], 1.0), w=[onec])

    def rstd_from_ms(ms_ap, ms_bufs, out_b, out_ap, tmp_b, tmp_ap):
        kb.op("act", lambda e: e.activation(out=tmp_ap, in_=ms_ap, func=AF.Ln, bias=epsc[:, 0:1], scale=1.0), r=ms_bufs + [epsc], w=[tmp_b])
        kb.op("act", lambda e: e.activation(out=out_ap, in_=tmp_ap, func=AF.Exp, scale=-0.5), r=[tmp_b], w=[out_b])

    for l in range(L):
        x_src = xin if l == 0 else xmid_d
        x_dst = xmid_d if l == 0 else yout
        pl = lambda c0, n=1: par[:, l, c0:c0 + n]

        stackA = ExitStack()
        kb.stack = stackA
        win = kb.sbuf("win%d" % l, [128, 8, DIN], BF16)
        for c in range(8):
            kb.dma("pool", "w", win[:, c, :], w_in_d[l, c * 128:(c + 1) * 128, :], w=[win])
        xt = kb.sbuf("xt%d" % l, [128, 4, D], F32)
        hb = kb.sbuf("hb%d" % l, [128, 4, D], BF16)
        hT = kb.sbuf("hT%d" % l, [128, 8, 512], BF16)
        junk = kb.sbuf("junk%d" % l, [128, D], F32)
        st4 = kb.sbuf("st4%d" % l, [128, 8], F32)
        fa = [kb.sbuf("fa%d_%d" % (l, i), [128, 512], F32) for i in range(6)]
        fb = [kb.sbuf("fb%d_%d" % (l, i), [128, 512], BF16) for i in range(3)]
        ob = [kb.sbuf("ob%d_%d" % (l, i), [128, 512], BF16) for i in range(2)]
        vb = kb.sbuf("vb%d" % l, [128, 4, 512], BF16)
        vi = kb.sbuf("vi%d" % l, [128, 4, 256], BF16)
        ucv = [kb.sbuf("ucv%d_%d" % (l, j), [128, 514], F32) for j in range(2)]
        sig = kb.sbuf("sig%d" % l, [128, 512], F32)
        kk = kb.sbuf("kk%d" % l, [128, 512], F32)
        cc = kb.sbuf("cc%d" % l, [128, 512], F32)
        qs = kb.sbuf("qs%d" % l, [128, 512], F32)
        gs = [kb.sbuf("gs%d_%d" % (l, j), [128, 512], F32) for j in range(2)]
        qe = [kb.sbuf("qe%d_%d" % (l, j), [128, 512], BF16) for j in range(2)]
        ke = [kb.sbuf("ke%d_%d" % (l, j), [128, 512], BF16) for j in range(2)]
        qec = [kb.sbuf("qec%d_%d" % (l, j), [128, 512], BF16) for j in range(2)]
        kdT = kb.sbuf("kdT%d" % l, [128, 512], BF16)
        kd = kb.sbuf("kd%d" % l, [128, 4, 256], BF16)
        ecl = kb.sbuf("ecl%d" % l, [128, 2, 8], F32)
        state = kb.sbuf("state%d" % l, [128, 2, 64], F32)
        stateb = kb.sbuf("stateb%d" % l, [128, 2, 64], BF16)
        atsb = kb.sbuf("atsb%d" % l, [128, 128], BF16)
        osb = kb.sbuf("osb%d" % l, [128, 2, 512], F32)
        vi2 = kb.sbuf("vi2_%d" % l, [128, 8, 256], BF16)
        kd2 = kb.sbuf("kd2_%d" % l, [128, 8, 256], BF16)
        segm = kb.sbuf("segm%d" % l, [128, 512], F32)

        kb.op("dve", lambda e: e.tensor_copy(out=segm[:, :], in_=cst[:, K_SEG:K_SEG + 512]), r=[cst], w=[segm])
        kb.op("dve", lambda e: e.memset(state[:, :, :], 0.0), w=[state])
        kb.op("dve", lambda e: e.memset(stateb[:, :, :], 0.0), w=[stateb])
        for j in range(2):
            kb.op("dve", lambda e: e.memset(ucv[j][:, :], 0.0), w=[ucv[j]])
        if l == 0:
            kb.op("dve", lambda e: e.memset(lbt[:, :], 0.0), w=[lbt])
        else:
            kb.op("dve", lambda e: e.tensor_tensor(out=small[:, 0:2], in0=pl(P_LB + 2, 2), in1=pl(P_LB, 2), op=ALU.subtract), r=[par], w=[small])
            kb.op("act", lambda e: e.activation(out=lbt[:, :], in_=small[:, 0:2], func=AF.Sigmoid), r=[small], w=[lbt])
        kb.op("dve", lambda e: e.tensor_scalar(out=omlt[:, :], in0=lbt[:, :], scalar1=-1.0, scalar2=1.0, op0=ALU.mult, op1=ALU.add), r=[lbt], w=[omlt])
        kb.op("dve", lambda e: e.tensor_scalar(out=nomlt[:, :], in0=omlt[:, :], scalar1=-1.0, scalar2=None, op0=ALU.mult), r=[omlt], w=[nomlt])
        kb.op("dve", lambda e: e.tensor_reduce(out=small[:, 8:9], in_=pl(P_GQR, 64), axis=mybir.AxisListType.X, op=ALU.max, apply_absolute_value=True), r=[par], w=[small])
        kb.op("dve", lambda e: e.tensor_reduce(out=small[:, 9:10], in_=pl(P_GKR, 64), axis=mybir.AxisListType.X, op=ALU.max, apply_absolute_value=True), r=[par, small], w=[small])
        kb.op("dve", lambda e: e.scalar_tensor_tensor(out=bshift[:, :], in0=small[:, 8:9], scalar=8.0, in1=small[:, 9:10], op0=ALU.mult, op1=ALU.mult), r=[small], w=[bshift])

        rot = {"fm": 0, "tm": 0, "fa": 0, "fb": 0, "ob": 0}

        def nxt(name, n):
            v = rot[name]
            rot[name] = (v + 1) % n
            return v

        def fm_chunk(col0):
            p = ps[1 + nxt("fm", 2)]
            for c in range(8):
                kb.op("pe", lambda e: e.matmul(p[:, :], lhsT=win[:, c, col0:col0 + 128], rhs=hT[:, c, :], start=(c == 0), stop=(c == 7)),
                      r=[win, hT], w=[p], inc=(c == 7))
            return p

        def headnorm_store(src_ap, src_bufs, gain_ap, dst_d, row0, tok0, mul_b=None):
            sq = fb[nxt("fb", 3)]
            kb.op("act", lambda e: e.activation(out=sq[:, :], in_=src_ap, func=AF.Square), r=src_bufs, w=[sq])
            pm = ps[5]
            kb.op("pe", lambda e: e.matmul(pm[:, :], lhsT=blkb[:, :], rhs=sq[:, :], start=True, stop=True), r=[blkb, sq], w=[pm])
            t1 = fa[nxt("fa", 6)]
            rs = fa[nxt("fa", 6)]
            rstd_from_ms(pm[:, :], [pm], rs, rs[:, :], t1, t1[:, :])
            o = ob[nxt("ob", 2)]
            if mul_b is None:
                kb.op("dve", lambda e: e.scalar_tensor_tensor(out=o[:, :], in0=src_ap, scalar=gain_ap, in1=rs[:, :], op0=ALU.mult, op1=ALU.mult),
                      r=src_bufs + [rs, par], w=[o])
            else:
                kb.op("dve", lambda e: e.scalar_tensor_tensor(out=t1[:, :], in0=src_ap, scalar=gain_ap, in1=rs[:, :], op0=ALU.mult, op1=ALU.mult),
                      r=src_bufs + [rs, par], w=[t1])
                kb.op("dve", lambda e: e.tensor_tensor(out=o[:, :], in0=t1[:, :], in1=mul_b[:, :], op=ALU.mult), r=[t1, mul_b], w=[o])
            kb.dma("sp", "st", dst_d[row0:row0 + 128, tok0:tok0 + 512], o[:, :], r=[o], w=[dst_d])

        for tb in range(NB):
            tok0 = tb * 512
            kb.dma("sp", "ld", xt[:, :, :], x_src[tok0:tok0 + 512, :].rearrange("(s p) d -> p s d", p=128), r=[x_src], w=[xt])
            for s in range(4):
                kb.op("act", lambda e: e.activation(out=junk[:, :], in_=xt[:, s, :], func=AF.Square, accum_out=st4[:, s:s + 1]), r=[xt], w=[junk, st4])
            rstd_tm(kb, st4, epsc, 4)
            for s in range(4):
                kb.op("act", lambda e: e.activation(out=hb[:, s, :], in_=xt[:, s, :], func=AF.Identity, scale=st4[:, 4 + s:5 + s]), r=[xt, st4], w=[hb])
            pT = ps[0]
            pTb = pT.t.ap().bitcast(BF16)
            for c in range(8):
                half = (c % 2) * 512
                for s in range(4):
                    kb.op("pe", lambda e: e.transpose(pTb[:, half + s * 128:half + (s + 1) * 128], hb[:, s, c * 128:(c + 1) * 128], identb[:, :]),
                          r=[hb, identb], w=[pT], inc=(s == 3))
                en = "dve" if c % 2 == 0 else "act"
                if en == "dve":
                    kb.op("dve", lambda e: e.tensor_scalar(out=hT[:, c, :], in0=pTb[:, half:half + 512], scalar1=pl(P_G1 + c), scalar2=None, op0=ALU.mult), r=[pT, par], w=[hT])
                else:
                    kb.op("act", lambda e: e.activation(out=hT[:, c, :], in_=pTb[:, half:half + 512], func=AF.Identity, scale=pl(P_G1 + c)), r=[pT, par], w=[hT])

            for which, col0, dst, gcol in (("q", C_Q, qT_d, P_GQ), ("k", C_K, kT_d, P_GK)):
                for c in range(4):
                    p = fm_chunk(col0 + c * 128)
                    headnorm_store(p[:, :], [p], pl(gcol), dst, c * 128, tok0)

            for s in range(4):
                p = ps[3 + nxt("tm", 2)]
                for c in range(8):
                    kb.op("pe", lambda e: e.matmul(p[:, :], lhsT=hT[:, c, s * 128:(s + 1) * 128], rhs=win[:, c, C_V:C_V + 512], start=(c == 0), stop=(c == 7)),
                          r=[win, hT], w=[p], inc=(c == 7))
                kb.op("act", lambda e: e.activation(out=vb[:, s, :], in_=p[:, :], func=AF.Copy), r=[p], w=[vb])
                p2 = ps[3 + nxt("tm", 2)]
                for c in range(8):
                    kb.op("pe", lambda e: e.matmul(p2[:, 0:256], lhsT=hT[:, c, s * 128:(s + 1) * 128], rhs=win[:, c, C_RI:C_RI + 256], start=(c == 0), stop=(c == 7)),
                          r=[win, hT], w=[p2], inc=False)
                for c in range(8):
                    kb.op("pe", lambda e: e.matmul(p2[:, 256:264], lhsT=hT[:, c, s * 128:(s + 1) * 128], rhs=win[:, c, C_F:C_F + 8], start=(c == 0), stop=(c == 7)),
                          r=[win, hT], w=[p2], inc=(c == 7))
                kb.op("act", lambda e: e.activation(out=vi[:, s, :], in_=p2[:, 0:256], func=AF.Copy), r=[p2], w=[vi])
                blk = tb * 4 + s
                kb.op("dve", lambda e: e.tensor_tensor(out=small[:, 16:24], in0=p2[:, 256:264], in1=pl(P_FB, 8), op=ALU.add), r=[p2, par, vi], w=[small])
                kb.op("act", lambda e: e.activation(out=small[:, 24:32], in_=small[:, 16:24], func=AF.Abs), r=[small], w=[small])
                kb.op("act", lambda e: e.activation(out=small[:, 32:40], in_=small[:, 24:32], func=AF.Exp, scale=-1.0), r=[small], w=[small])
                kb.op("act", lambda e: e.activation(out=small[:, 40:48], in_=small[:, 32:40], func=AF.Ln, bias=onec[:, 0:1], scale=1.0), r=[small, onec], w=[small])
                kb.op("dve", lambda e: e.tensor_single_scalar(out=small[:, 48:56], in_=small[:, 16:24], scalar=0.0, op=ALU.min), r=[small], w=[small])
                kb.op("dve", lambda e: e.tensor_tensor(out=lsig[:, blk, :], in0=small[:, 48:56], in1=small[:, 40:48], op=ALU.subtract), r=[small], w=[lsig])
            kb.dma("sp", "st", v_d[tok0:tok0 + 512, :].rearrange("(s p) f -> p s f", p=128), vb[:, :, :], r=[vb], w=[v_d])

            for j in range(2):
                px = fm_chunk(C_CX + j * 128)
                t0 = fa[nxt("fa", 6)]
                kb.op("act", lambda e: e.activation(out=t0[:, :], in_=px[:, :], func=AF.Copy), r=[px], w=[t0])
                pc = fm_chunk(C_CC + j * 128)
                u = ucv[j]
                kb.op("dve", lambda e: e.tensor_tensor(out=u[:, 2:514], in0=t0[:, :], in1=pc[:, :], op=ALU.mult), r=[t0, pc], w=[u])
                t1 = fa[nxt("fa", 6)]
                kb.op("dve", lambda e: e.tensor_scalar(out=t1[:, :], in0=u[:, 0:512], scalar1=pl(P_CW + 0 * 2 + j), scalar2=None, op0=ALU.mult), r=[u, par], w=[t1])
                kb.op("dve", lambda e: e.scalar_tensor_tensor(out=t1[:, :], in0=u[:, 1:513], scalar=pl(P_CW + 1 * 2 + j), in1=t1[:, :], op0=ALU.mult, op1=ALU.add), r=[u, par, t1], w=[t1])
                kb.op("dve", lambda e: e.scalar_tensor_tensor(out=t1[:, :], in0=u[:, 2:514], scalar=pl(P_CW + 2 * 2 + j), in1=t1[:, :], op0=ALU.mult, op1=ALU.add), r=[u, par, t1], w=[t1])
                pbg = fm_chunk(C_CB + j * 128)
                kb.op("dve", lambda e: e.tensor_tensor(out=t0[:, :], in0=t1[:, :], in1=pbg[:, :], op=ALU.mult), r=[t1, pbg], w=[t0])
                kb.op("dve", lambda e: e.tensor_copy(out=small[:, 56:58], in_=u[:, 512:514]), r=[u], w=[small])
                kb.op("dve", lambda e: e.tensor_copy(out=u[:, 0:2], in_=small[:, 56:58]), r=[small], w=[u])
                headnorm_store(t0[:, :], [t0], pl(P_MG + 4 + j), yT_d, 512 + j * 128, tok0)

            for j in range(2):
                pf = fm_chunk(C_RF + j * 128)
                kb.op("act", lambda e: e.activation(out=sig[:, :], in_=pf[:, :], func=AF.Sigmoid), r=[pf], w=[sig])
                tf = fa[nxt("fa", 6)]
                kb.op("dve", lambda e: e.tensor_scalar(out=tf[:, :], in0=sig[:, :], scalar1=omlt[:, j:j + 1], scalar2=lbt[:, j:j + 1], op0=ALU.mult, op1=ALU.add), r=[sig, omlt, lbt], w=[tf])
                kb.op("dve", lambda e: e.tensor_single_scalar(out=tf[:, :], in_=tf[:, :], scalar=TINY, op=ALU.max), r=[tf], w=[tf])
                kb.op("act", lambda e: e.activation(out=tf[:, :], in_=tf[:, :], func=AF.Ln), r=[tf], w=[tf])
                kb.op("dve", lambda e: e.tensor_scalar(out=kk[:, :], in0=sig[:, :], scalar1=nomlt[:, j:j + 1], scalar2=omlt[:, j:j + 1], op0=ALU.mult, op1=ALU.add), r=[sig, omlt, nomlt], w=[kk])
                kb.op("dve", lambda e: e.tensor_tensor_scan(out=cc[:, :], data0=segm[:, :], data1=tf[:, :], initial=0.0, op0=ALU.mult, op1=ALU.add), r=[segm, tf], w=[cc])
                c3 = cc.t.ap().rearrange("p (n t) -> p n t", t=64)
                pq = fm_chunk(C_RQ + j * 128)
                kb.op("act", lambda e: e.activation(out=qs[:, :], in_=pq[:, :], func=AF.Silu), r=[pq], w=[qs])
                pg = fm_chunk(C_RG + j * 128)
                kb.op("act", lambda e: e.activation(out=gs[j][:, :], in_=pg[:, :], func=AF.Silu), r=[pg], w=[gs[j]])
                d1 = fa[nxt("fa", 6)]
                kb.op("dve", lambda e: e.tensor_tensor(out=d1.t.ap().rearrange("p (n t) -> p n t", t=64), in0=c3, in1=c3[:, :, 31:32].to_broadcast([128, 8, 64]), op=ALU.subtract), r=[cc], w=[d1])
                ex = fa[nxt("fa", 6)]
                kb.op("act", lambda e: e.activation(out=ex[:, :], in_=d1[:, :], func=AF.Exp), r=[d1], w=[ex])
                kb.op("dve", lambda e: e.tensor_tensor(out=qe[j][:, :], in0=qs[:, :], in1=ex[:, :], op=ALU.mult), r=[qs, ex], w=[qe[j]])
                kb.op("act", lambda e: e.activation(out=ex[:, :], in_=d1[:, :], func=AF.Exp, scale=-1.0), r=[d1], w=[ex])
                kb.op("dve", lambda e: e.tensor_tensor(out=ke[j][:, :], in0=kk[:, :], in1=ex[:, :], op=ALU.mult), r=[kk, ex], w=[ke[j]])
                kb.op("act", lambda e: e.activation(out=ex[:, :], in_=cc[:, :], func=AF.Exp), r=[cc], w=[ex])
                kb.op("dve", lambda e: e.tensor_tensor(out=qec[j][:, :], in0=qs[:, :], in1=ex[:, :], op=ALU.mult), r=[qs, ex], w=[qec[j]])
                kb.op("dve", lambda e: e.tensor_tensor(out=d1.t.ap().rearrange("p (n t) -> p n t", t=64), in0=c3[:, :, 63:64].to_broadcast([128, 8, 64]), in1=c3, op=ALU.subtract), r=[cc], w=[d1])
                kb.op("act", lambda e: e.activation(out=ex[:, :], in_=d1[:, :], func=AF.Exp), r=[d1], w=[ex])
                kb.op("dve", lambda e: e.tensor_tensor(out=kdT[:, :], in0=kk[:, :], in1=ex[:, :], op=ALU.mult), r=[kk, ex], w=[kdT])
                kb.op("act", lambda e: e.activation(out=ecl[:, j, :], in_=c3[:, :, 63], func=AF.Exp), r=[cc], w=[ecl])
                pT = ps[0]
                pTb = pT.t.ap().bitcast(BF16)
                for s in range(4):
                    kb.op("pe", lambda e: e.transpose(pTb[:, s * 128:(s + 1) * 128], kdT[:, s * 128:(s + 1) * 128], identb[:, :]), r=[kdT, identb], w=[pT], inc=(s == 3))
                kb.op("dve", lambda e: e.tensor_copy(out=kd[:, :, j * 128:(j + 1) * 128], in_=pTb[:, 0:512].rearrange("p (s f) -> p s f", f=128)), r=[pT], w=[kd])

            for pr in range(2):
                for dst in range(2):
                    kb.dma("sp", "cp", vi2.t.ap().rearrange("p (s two) f -> p s two f", two=2)[dst * 64:(dst + 1) * 64, :, pr, :], vi[pr * 64:(pr + 1) * 64, :, :], r=[vi], w=[vi2])
                    kb.dma("sp", "cp", kd2.t.ap().rearrange("p (s two) f -> p s two f", two=2)[dst * 64:(dst + 1) * 64, :, pr, :], kd[pr * 64:(pr + 1) * 64, :, :], r=[kd], w=[kd2])
            for ch in range(8):
                cols = slice(ch * 64, (ch + 1) * 64)
                for hh in range(2):
                    pb = hh * 64
                    ph = ps[6 + hh]
                    for j in range(2):
                        kb.op("pe", lambda e: e.matmul(ph[pb:pb + 64, j * 64:(j + 1) * 64], lhsT=ke[j][pb:pb + 64, cols], rhs=qe[j][pb:pb + 64, cols], start=True, stop=True, tile_position=(pb, pb)),
                              r=[ke[j], qe[j]], w=[ph], inc=(j == 1))
                for hh in range(2):
                    pb = hh * 64
                    ph = ps[6 + hh]
                    kb.op("dve", lambda e: e.tensor_tensor(out=atsb[pb:pb + 64, 0:128].rearrange("p (h t) -> p h t", t=64), in0=ph[pb:pb + 64, 0:128].rearrange("p (h t) -> p h t", t=64),
                                                           in1=cmaskb[pb:pb + 64, :].unsqueeze(1).to_broadcast([64, 2, 64]), op=ALU.mult), r=[ph, cmaskb], w=[atsb])
                for hh in range(2):
                    pb = hh * 64
                    ph = ps[6 + hh]
                    for j in range(2):
                        hd = j * 2 + hh
                        oslc = ph[pb:pb + 64, 128 + j * 64:128 + (j + 1) * 64]
                        kb.op("pe", lambda e: e.matmul(oslc, lhsT=vi2[pb:pb + 64, ch, hd * 64:(hd + 1) * 64], rhs=atsb[pb:pb + 64, j * 64:(j + 1) * 64], start=True, stop=False, tile_position=(pb, pb)),
                              r=[vi2, atsb], w=[ph], inc=False)
                        kb.op("pe", lambda e: e.matmul(oslc, lhsT=stateb[pb:pb + 64, j, :], rhs=qec[j][pb:pb + 64, cols], start=False, stop=True, tile_position=(pb, pb)),
                              r=[stateb, qec[j]], w=[ph], inc=False)
                        kb.op("pe", lambda e: e.matmul(ph[pb:pb + 64, 256 + j * 64:256 + (j + 1) * 64], lhsT=kd2[pb:pb + 64, ch, hd * 64:(hd + 1) * 64], rhs=vi2[pb:pb + 64, ch, hd * 64:(hd + 1) * 64], start=True, stop=True, tile_position=(pb, pb)),
                              r=[kd2, vi2], w=[ph], inc=(j == 1))
                kb.op("dve", lambda e: e.tensor_tensor(out=state[:, :, :], in0=state[:, :, :], in1=ecl[:, :, ch:ch + 1].to_broadcast([128, 2, 64]), op=ALU.mult), r=[state, ecl], w=[state])
                for hh in range(2):
                    pb = hh * 64
                    ph = ps[6 + hh]
                    kb.op("act", lambda e: e.activation(out=osb[pb:pb + 64, :, cols], in_=ph[pb:pb + 64, 128:256].rearrange("p (j t) -> p j t", t=64), func=AF.Copy), r=[ph], w=[osb])
                    kb.op("dve", lambda e: e.tensor_tensor(out=state[pb:pb + 64, :, :], in0=state[pb:pb + 64, :, :], in1=ph[pb:pb + 64, 256:384].rearrange("p (j v) -> p j v", v=64), op=ALU.add), r=[state, ph, osb], w=[state])
                kb.op("dve", lambda e: e.tensor_copy(out=stateb[:, :, :], in_=state[:, :, :]), r=[state], w=[stateb])
            for j in range(2):
                headnorm_store(osb[:, j, :], [osb], pl(P_MG + 6 + j), yT_d, 768 + j * 128, tok0, mul_b=gs[j])

        lflat = lsig.t.ap().rearrange("p n h -> p (n h)")
        NW = NS * 8
        tri = cst[:, K_TRI:K_TRI + 128]
        onesf = cst[:, K_ONES:K_ONES + 128]
        sel = cst[:, K_SEL:K_SEL + 128]
        pcs, ptot = ps[1], ps[2]
        kb.op("pe", lambda e: e.matmul(pcs[:, 0:NW], lhsT=tri, rhs=lflat, start=True, stop=True), r=[cst, lsig], w=[pcs])
        kb.op("pe", lambda e: e.matmul(ptot[:, 0:NW], lhsT=onesf, rhs=lflat, start=True, stop=True), r=[cst, lsig], w=[ptot])
        tot = fa[0]
        kb.op("act", lambda e: e.activation(out=tot[:, 0:NW], in_=ptot[:, 0:NW], func=AF.Copy), r=[ptot], w=[tot])
        kb.op("dve", lambda e: e.memset(cI[:, 0, :], 0.0), w=[cI])
        for b in range(1, NS):
            kb.op("dve", lambda e: e.tensor_tensor(out=cI[:, b, :], in0=cI[:, b - 1, :], in1=tot[:, (b - 1) * 8:b * 8], op=ALU.add), r=[cI, tot], w=[cI])
        dcum = fa[1]
        kb.op("dve", lambda e: e.tensor_tensor(out=dcum[:, 0:NW], in0=pcs[:, 0:NW], in1=cI.t.ap().rearrange("p n h -> p (n h)"), op=ALU.add), r=[pcs, cI], w=[dcum])
        kb.op("dve", lambda e: e.tensor_scalar(out=negd.t.ap().rearrange("p n h -> p (n h)"), in0=dcum[:, 0:NW], scalar1=-1.0, scalar2=None, op0=ALU.mult), r=[dcum], w=[negd])
        pbc = ps[3]
        kb.op("pe", lambda e: e.matmul(pbc[:, 0:NW], lhsT=sel, rhs=dcum[:, 0:NW], start=True, stop=True), r=[cst, dcum], w=[pbc])
        kb.op("dve", lambda e: e.tensor_scalar(out=cI.t.ap().rearrange("p n h -> p (n h)"), in0=pbc[:, 0:NW], scalar1=bshift[:, 0:1], scalar2=None, op0=ALU.subtract), r=[pbc, bshift], w=[cI])

        kb.barrier()
        if stop == ("A", l):
            kb.finish()
            return nc, dbg
        stackA.close()
        stackB = ExitStack()
        kb.stack = stackB
        obB = [kb.sbuf("obB%d_%d" % (l, i), [128, 512], BF16) for i in range(2)]
        kTc = kb.sbuf("kTc%d" % l, [128, T], BF16)
        qTc = kb.sbuf("qTc%d" % l, [128, T], BF16)
        vh = kb.sbuf("vh%d" % l, [128, NS, 128], BF16)
        pTt = [kb.sbuf("pTt%d_%d" % (l, i), [128, 512], BF16) for i in range(3)]
        biasb = [kb.sbuf("biasb%d_%d" % (l, i), [128, NS], F32) for i in range(2)]
        rden = kb.sbuf("rden%d" % l, [128, 512], F32)
        on = kb.sbuf("on%d" % l, [128, 512], F32)
        rotb = {"s": 0, "p": 0, "b": 0, "o": 0, "ob": 0}

        def nxb(name, n):
            v = rotb[name]
            rotb[name] = (v + 1) % n
            return v

        for c in range(4):
            kb.dma("sp", "ld", kTc[:, :], kT_d[c * 128:(c + 1) * 128, :], r=[kT_d], w=[kTc])
            kb.dma("sp", "ld", qTc[:, :], qT_d[c * 128:(c + 1) * 128, :], r=[qT_d], w=[qTc])
            kb.dma("sp", "ld", vh[:, :, :], v_d[:, c * 128:(c + 1) * 128].rearrange("(n p) f -> p n f", p=128), r=[v_d], w=[vh])
            for I in range(NB):
                oi = nxb("o", 2)
                pO, pD = ps[3 + oi], ps[5 + oi]
                nJ = 4 * (I + 1)
                for hh in range(2):
                    pb = hh * 64
                    h = 2 * c + hh
                    bb = biasb[nxb("b", 2)]
                    kb.op("dve", lambda e: e.tensor_scalar(out=bb[:, 0:nJ], in0=negd[:, 0:nJ, h], scalar1=cI[:, 4 * I + 1, h:h + 1], scalar2=None, op0=ALU.add), r=[negd, cI], w=[bb])
                    for J in range(nJ):
                        pS = ps[nxb("s", 3)]
                        kb.op("pe", lambda e: e.matmul(pS[:, :], lhsT=kTc[pb:pb + 64, J * 128:(J + 1) * 128], rhs=qTc[pb:pb + 64, I * 512:(I + 1) * 512], start=True, stop=True, tile_position=(pb, 0)),
                              r=[kTc, qTc], w=[pS])
                        pt = pTt[nxb("p", 3)]
                        kb.op("act", lambda e: e.activation(out=pt[:, :], in_=pS[:, :], func=AF.Exp, bias=bb[:, J:J + 1], scale=0.125), r=[pS, bb], w=[pt])
                        if J >= 4 * I:
                            kb.op("dve", lambda e: e.tensor_tensor(out=pt[:, :], in0=pt[:, :], in1=dmaskb[:, J - 4 * I, :], op=ALU.mult), r=[pt, dmaskb], w=[pt])
                        kb.op("pe", lambda e: e.matmul(pO[pb:pb + 64, :], lhsT=vh[:, J, pb:pb + 64], rhs=pt[:, :], start=(J == 0), stop=(J == nJ - 1), tile_position=(0, pb)), r=[vh, pt], w=[pO], inc=False)
                        kb.op("pe", lambda e: e.matmul(pD[pb:pb + 64, :], lhsT=onesb[:, 0:64], rhs=pt[:, :], start=(J == 0), stop=(J == nJ - 1), tile_position=(0, pb)), r=[onesb, pt], w=[pD])
                kb.op("dve", lambda e: e.reciprocal(out=rden[:, :], in_=pD[:, :]), r=[pD], w=[rden])
                kb.op("dve", lambda e: e.tensor_tensor(out=on[:, :], in0=pO[:, :], in1=rden[:, :], op=ALU.mult), r=[pO, rden], w=[on])
                sq = pTt[nxb("p", 3)]
                kb.op("act", lambda e: e.activation(out=sq[:, :], in_=on[:, :], func=AF.Square), r=[on], w=[sq])
                pm = ps[7]
                kb.op("pe", lambda e: e.matmul(pm[:, :], lhsT=blkb[:, :], rhs=sq[:, :], start=True, stop=True), r=[blkb, sq], w=[pm])
                rstd_from_ms(pm[:, :], [pm], rden, rden[:, :], rden, rden[:, :])
                o = obB[nxb("ob", 2)]
                kb.op("dve", lambda e: e.scalar_tensor_tensor(out=o[:, :], in0=on[:, :], scalar=pl(P_MG + c), in1=rden[:, :], op0=ALU.mult, op1=ALU.mult), r=[on, rden, par], w=[o])
                kb.dma("sp", "st", yT_d[c * 128:(c + 1) * 128, I * 512:(I + 1) * 512], o[:, :], r=[o], w=[yT_d])

        kb.barrier()
        if stop == ("B", l):
            kb.finish()
            return nc, dbg
        stackB.close()
        stackC = ExitStack()
        kb.stack = stackC
        junk = kb.sbuf("junkC%d" % l, [128, D], F32)
        wout = kb.sbuf("wout%d" % l, [128, 8, D], BF16)
        for c in range(8):
            kb.dma("pool", "w", wout[:, c, :], w_out_d[l, c * 128:(c + 1) * 128, :], w=[wout])
        x1 = kb.sbuf("x1_%d" % l, [128, NSC, D], F32)
        yTb = kb.sbuf("yTb%d" % l, [128, 8, TBC], BF16)
        h2T = yTb
        h2b = [kb.sbuf("h2b%d_%d" % (l, i), [128, D], BF16) for i in range(2)]
        stc = kb.sbuf("stc%d" % l, [128, 2 * NSC], F32)
        wgb = [kb.sbuf("wgb%d_%d" % (l, i), [128, 8, 512], BF16) for i in range(2)]
        wub = [kb.sbuf("wub%d_%d" % (l, i), [128, 8, 512], BF16) for i in range(2)]
        wdb = [kb.sbuf("wdb%d_%d" % (l, i), [128, 4, D], BF16) for i in range(2)]
        hact = kb.sbuf("hact%d" % l, [128, 4, TBC], BF16)
        sgt = [kb.sbuf("sgt%d_%d" % (l, i), [128, 512], BF16) for i in range(2)]
        moe = (l % 2 == 1)
        if moe:
            wrb = kb.sbuf("wrb%d" % l, [128, 8, NE], BF16)
            kb.dma("pool", "w", wrb[:, :, :], wr_d[0].rearrange("(c p) e -> p c e", p=128), w=[wrb])
            gates = kb.sbuf("gates%d" % l, [128, NSC, NE], F32)
            rt = kb.sbuf("rt%d" % l, [128, 64], F32)
        rc = {"a": 0, "g": 0, "u": 0, "w": 0, "h": 0, "sg": 0}

        def nxc(name, n):
            v = rc[name]
            rc[name] = (v + 1) % n
            return v

        for tbc in range(T // TBC):
            tok0 = tbc * TBC
            kb.dma("sp", "ld", x1[:, :, :], x_src[tok0:tok0 + TBC, :].rearrange("(s p) d -> p s d", p=128), r=[x_src], w=[x1])
            kb.dma("sp", "ld", yTb[:, :, :], yT_d[:, tok0:tok0 + TBC].rearrange("(c p) t -> p c t", p=128), r=[yT_d], w=[yTb])
            for s in range(NSC):
                for half in range(2):
                    p = ps[1 + nxc("a", 2)]
                    for c in range(8):
                        kb.op("pe", lambda e: e.matmul(p[:, :], lhsT=yTb[:, c, s * 128:(s + 1) * 128], rhs=wout[:, c, half * 512:(half + 1) * 512], start=(c == 0), stop=(c == 7)),
                              r=[yTb, wout], w=[p], inc=(c == 7))
                    kb.op("dve", lambda e: e.tensor_tensor(out=x1[:, s, half * 512:(half + 1) * 512], in0=x1[:, s, half * 512:(half + 1) * 512], in1=p[:, :], op=ALU.add), r=[x1, p], w=[x1])
            for s in range(NSC):
                kb.op("act", lambda e: e.activation(out=junk[:, :], in_=x1[:, s, :], func=AF.Square, accum_out=stc[:, s:s + 1]), r=[x1], w=[junk, stc])
            rstd_tm(kb, stc, epsc, NSC)
            for s in range(NSC):
                hbb = h2b[nxc("h", 2)]
                kb.op("act", lambda e: e.activation(out=hbb[:, :], in_=x1[:, s, :], func=AF.Identity, scale=stc[:, NSC + s:NSC + s + 1]), r=[x1, stc], w=[hbb])
                pT = ps[0]
                pTb = pT.t.ap().bitcast(BF16)
                for c in range(8):
                    kb.op("pe", lambda e: e.transpose(pTb[:, c * 128:(c + 1) * 128], hbb[:, c * 128:(c + 1) * 128], identb[:, :]), r=[hbb, identb], w=[pT], inc=(c == 7))
                kb.op("dve", lambda e: e.tensor_tensor(out=h2T[:, :, s * 128:(s + 1) * 128], in0=pTb[:, 0:1024].rearrange("p (c t) -> p c t", t=128),
                                                       in1=pl(P_G2, 8).unsqueeze(2).to_broadcast([128, 8, 128]), op=ALU.mult), r=[pT, par], w=[h2T])
            if moe:
                for s in range(NSC):
                    p = ps[1 + nxc("a", 2)]
                    for c in range(8):
                        kb.op("pe", lambda e: e.matmul(p[:, 0:NE], lhsT=h2T[:, c, s * 128:(s + 1) * 128], rhs=wrb[:, c, :], start=(c == 0), stop=(c == 7)), r=[h2T, wrb], w=[p], inc=(c == 7))
                    kb.op("dve", lambda e: e.tensor_tensor(out=rt[:, 0:8], in0=p[:, 0:NE], in1=pl(P_RB, 8), op=ALU.add), r=[p, par], w=[rt])
                    kb.op("dve", lambda e: e.max(out=rt[:, 8:16], in_=rt[:, 0:8]), r=[rt], w=[rt])
                    kb.op("dve", lambda e: e.tensor_tensor(out=rt[:, 16:17], in0=rt[:, 8:9], in1=rt[:, 9:10], op=ALU.subtract), r=[rt], w=[rt])
                    kb.op("act", lambda e: e.activation(out=rt[:, 17:18], in_=rt[:, 16:17], func=AF.Sigmoid), r=[rt], w=[rt])
                    kb.op("act", lambda e: e.activation(out=rt[:, 18:19], in_=rt[:, 16:17], func=AF.Sigmoid, scale=-1.0), r=[rt], w=[rt])
                    kb.op("dve", lambda e: e.tensor_scalar(out=rt[:, 24:32], in0=rt[:, 0:8], scalar1=rt[:, 8:9], scalar2=rt[:, 17:18], op0=ALU.is_equal, op1=ALU.mult), r=[rt], w=[rt])
                    kb.op("dve", lambda e: e.tensor_scalar(out=rt[:, 32:40], in0=rt[:, 0:8], scalar1=rt[:, 9:10], scalar2=rt[:, 18:19], op0=ALU.is_equal, op1=ALU.mult), r=[rt], w=[rt])
                    kb.op("dve", lambda e: e.tensor_tensor(out=gates[:, s, :], in0=rt[:, 24:32], in1=rt[:, 32:40], op=ALU.add), r=[rt], w=[gates])
            experts = range(NE) if moe else [None]
            for ex in experts:
                if ex is None:
                    Wg, Wu, Wd = ffn_g_d.t.ap()[0], ffn_u_d.t.ap()[0], ffn_d_d.t.ap()[0]
                    wbufs = [ffn_g_d, ffn_u_d, ffn_d_d]
                else:
                    Wg, Wu, Wd = moe_g_d.t.ap()[0, ex], moe_u_d.t.ap()[0, ex], moe_d_d.t.ap()[0, ex]
                    wbufs = [moe_g_d, moe_u_d, moe_d_d]
                for fg in range(DFF // 512):
                    wi = nxc("w", 2)
                    kb.dma("pool", "w", wgb[wi][:, :, :], Wg[:, fg * 512:(fg + 1) * 512].rearrange("(c p) f -> p c f", p=128), w=[wgb[wi]])
                    kb.dma("pool", "w", wub[wi][:, :, :], Wu[:, fg * 512:(fg + 1) * 512].rearrange("(c p) f -> p c f", p=128), w=[wub[wi]])
                    kb.dma("pool", "w", wdb[wi][:, :, :], Wd[fg * 512:(fg + 1) * 512, :].rearrange("(k p) d -> p k d", p=128), w=[wdb[wi]])
                    for t4 in range(TBC // 512):
                        tsl = slice(t4 * 512, (t4 + 1) * 512)
                        for k in range(4):
                            pg = ps[3 + nxc("g", 2)]
                            pu = ps[5 + nxc("u", 2)]
                            for c in range(8):
                                kb.op("pe", lambda e: e.matmul(pg[:, :], lhsT=wgb[wi][:, c, k * 128:(k + 1) * 128], rhs=h2T[:, c, tsl], start=(c == 0), stop=(c == 7)), r=[wgb[wi], h2T], w=[pg], inc=(c == 7))
                            for c in range(8):
                                kb.op("pe", lambda e: e.matmul(pu[:, :], lhsT=wub[wi][:, c, k * 128:(k + 1) * 128], rhs=h2T[:, c, tsl], start=(c == 0), stop=(c == 7)), r=[wub[wi], h2T], w=[pu], inc=(c == 7))
                            sg = sgt[nxc("sg", 2)]
                            kb.op("act", lambda e: e.activation(out=sg[:, :], in_=pg[:, :], func=AF.Silu), r=[pg], w=[sg])
                            kb.op("dve", lambda e: e.tensor_tensor(out=hact[:, k, tsl], in0=sg[:, :], in1=pu[:, :], op=ALU.mult), r=[sg, pu], w=[hact])
                    for s in range(NSC):
                        for half in range(2):
                            p = ps[1 + nxc("a", 2)]
                            for k in range(4):
                                kb.op("pe", lambda e: e.matmul(p[:, :], lhsT=hact[:, k, s * 128:(s + 1) * 128], rhs=wdb[wi][:, k, half * 512:(half + 1) * 512], start=(k == 0), stop=(k == 3)),
                                      r=[hact, wdb[wi]], w=[p], inc=(k == 3))
                            xs = x1[:, s, half * 512:(half + 1) * 512]
                            if ex is None:
                                kb.op("dve", lambda e: e.tensor_tensor(out=xs, in0=xs, in1=p[:, :], op=ALU.add), r=[x1, p], w=[x1])
                            else:
                                kb.op("dve", lambda e: e.scalar_tensor_tensor(out=xs, in0=p[:, :], scalar=gates[:, s, ex:ex + 1], in1=xs, op0=ALU.mult, op1=ALU.add), r=[x1, p, gates], w=[x1])
            kb.dma("sp", "st", x_dst[tok0:tok0 + TBC, :].rearrange("(s p) d -> p s d", p=128), x1[:, :, :], r=[x1], w=[x_dst])
        kb.barrier()
        stackC.close()
        kb.stack = None

    kb.finish()
    return nc, dbg


def rstd_tm(kb, st, epsc, n):
    kb.op("act", lambda e: e.activation(out=st[:, n:2 * n], in_=st[:, 0:n], func=AF.Ln, bias=epsc[:, 0:1], scale=1.0 / D), r=[st, epsc], w=[st])
    kb.op("act", lambda e: e.activation(out=st[:, n:2 * n], in_=st[:, n:2 * n], func=AF.Exp, scale=-0.5), r=[st], w=[st])


_CACHE = {}


def kernel(**inputs):
    x = np.ascontiguousarray(inputs["x"], dtype=np.float32)
    B, S, _ = x.shape
    key = S
    if key not in _CACHE:
        _CACHE[key] = build(S)[0]
    nc = _CACHE[key]
    consts = make_consts()
    params = make_params(inputs)
    shared = {k: np.ascontiguousarray(inputs[k], dtype=np.float32) for k in
              ("w_in", "w_out", "ffn_w_gate", "ffn_w_up", "ffn_w_down", "moe_router_w", "moe_w_gate", "moe_w_up", "moe_w_down")}
    n = 8
    in_maps = []
    for cid in range(n):
        m = dict(shared)
        m["x"] = x[cid % B]
        m["consts"] = consts
        m["params"] = params
        in_maps.append(m)
    res = run_bass_kernel_spmd(nc, in_maps, core_ids=list(range(n)))
    out = np.stack([res.results[b]["y"] for b in range(B)], axis=0)
    return out.astype(np.float32)
```

```python
import numpy as np
from contextlib import ExitStack
import concourse.bass as bass
import concourse.mybir as mybir
from concourse.bass_utils import run_bass_kernel_spmd

F32 = mybir.dt.float32
BF16 = mybir.dt.bfloat16
I32 = mybir.dt.int32
AF = mybir.ActivationFunctionType
ALU = mybir.AluOpType

D = 1024
DIN = 3336
DFF = 3584
NE = 8
EPS = 1e-6
TINY = 1e-30
C_Q, C_K, C_V, C_F = 0, 512, 1024, 1536
C_CX, C_CB, C_CC = 1544, 1800, 2056
C_RQ, C_RF, C_RI, C_RG = 2312, 2568, 2824, 3080

K_ID, K_BLK, K_TRI, K_ONES, K_SEL = 0, 128, 256, 384, 512
K_DM = 640
K_CM = K_DM + 2048
K_SEG = K_CM + 64
NCONST = K_SEG + 512

P_G1, P_G2, P_GQ, P_GK, P_CW, P_LB, P_MG, P_FB, P_RB, P_GQR, P_GKR = 0, 8, 16, 17, 18, 24, 28, 36, 44, 52, 116
NPAR = 180


def make_consts():
    c = np.zeros((128, NCONST), np.float32)
    p = np.arange(128)
    c[:, K_ID:K_ID + 128] = np.eye(128)
    c[:, K_BLK:K_BLK + 128] = ((p[:, None] // 64) == (p[None, :] // 64)) / 64.0
    c[:, K_TRI:K_TRI + 128] = (p[:, None] <= p[None, :])
    c[:, K_ONES:K_ONES + 128] = 1.0
    c[127, K_SEL:K_SEL + 128] = 1.0
    q = np.arange(512)
    for j in range(4):
        c[:, K_DM + j * 512:K_DM + (j + 1) * 512] = ((j * 128 + p[:, None]) <= q[None, :])
    t = np.arange(64)
    c[:, K_CM:K_CM + 64] = ((p[:, None] % 64) <= t[None, :])
    c[:, K_SEG:K_SEG + 512] = ((q % 64) != 0)[None, :]
    return c


def make_params(inp):
    L = 2
    P = np.zeros((L, 128, NPAR), np.float32)
    for l in range(L):
        P[l, :, P_G1:P_G1 + 8] = inp["norm_mix"][l].reshape(8, 128).T
        P[l, :, P_G2:P_G2 + 8] = inp["norm_ffn"][l].reshape(8, 128).T
        P[l, :, P_GQ] = np.tile(inp["q_norm_gain"][l], 2)
        P[l, :, P_GK] = np.tile(inp["k_norm_gain"][l], 2)
        P[l, :, P_CW:P_CW + 6] = inp["conv_w"][l].reshape(3, 2, 128).transpose(2, 0, 1).reshape(128, 6)
        P[l, :, P_LB:P_LB + 4] = inp["hgrn_lb_logits"].reshape(2, 2, 128).transpose(2, 0, 1).reshape(128, 4)
        P[l, :, P_MG:P_MG + 8] = inp["mix_out_gain"][l].reshape(8, 128).T
        P[l, :, P_FB:P_FB + 8] = inp["attn_f_bias"][l][None, :]
        P[l, :, P_RB:P_RB + 8] = inp["moe_router_b"][0][None, :]
        P[l, :, P_GQR:P_GQR + 64] = inp["q_norm_gain"][l][None, :]
        P[l, :, P_GKR:P_GKR + 64] = inp["k_norm_gain"][l][None, :]
    return P


class Buf:
    __slots__ = ("t", "w", "r", "nowaw")

    def __init__(self, t, nowaw=False):
        self.t = t
        self.w = {}
        self.r = {}
        self.nowaw = nowaw

    def __getitem__(self, idx):
        return self.t.ap()[idx]


class KB:
    def __init__(self, nc):
        self.nc = nc
        self.eng = {"pe": nc.tensor, "act": nc.scalar, "dve": nc.vector, "pool": nc.gpsimd, "sp": nc.sync}
        self.sems = {}
        self.cnt = {}
        self.waited = {e: {} for e in self.eng}
        self.nsem = 0
        self.stack = None
        self.ncall = 0
        import os as _os
        self.noself = _os.environ.get('NOSELF', '0') == '1'
        import os
        self.cut = int(os.environ['KCUT']) if 'KCUT' in os.environ else None
        for e in self.eng:
            self._newsem(e)

    def _newsem(self, key):
        self.nsem += 1
        self.sems[key] = self.nc.alloc_semaphore(name="s%d_%s" % (self.nsem, key.replace(":", "_")))
        self.cnt[key] = 0

    def sbuf(self, name, shape, dt):
        if self.stack is not None:
            return Buf(self.stack.enter_context(self.nc.sbuf_tensor(name, list(shape), dt)))
        return Buf(self.nc.alloc_sbuf_tensor(name, list(shape), dt))

    def psum(self, name, shape, dt):
        return Buf(self.nc.alloc_psum_tensor(name, list(shape), dt))

    def dram(self, name, shape, dt, kind=None):
        if kind is None:
            t = self.nc.dram_tensor(name, list(shape), dt)
        else:
            t = self.nc.dram_tensor(name, list(shape), dt, kind=kind)
        return Buf(t, nowaw=True)

    def _wait(self, en, toks):
        e = self.eng[en]
        wd = self.waited[en]
        for k, v in toks.items():
            if en == "pe" and k == "pe":
                continue
            if self.noself and k == en:
                continue
            if wd.get(k, 0) < v:
                e.wait_ge(self.sems[k], v)
                wd[k] = v

    def _deps(self, r, w):
        toks = {}
        for b in r:
            for k, v in b.w.items():
                if toks.get(k, 0) < v:
                    toks[k] = v
        for b in w:
            for k, v in b.r.items():
                if toks.get(k, 0) < v:
                    toks[k] = v
            if not b.nowaw:
                for k, v in b.w.items():
                    if toks.get(k, 0) < v:
                        toks[k] = v
        return toks

    def _mark(self, key, val, r, w):
        for b in r:
            if b.r.get(key, 0) < val:
                b.r[key] = val
        for b in w:
            if b.nowaw:
                if b.w.get(key, 0) < val:
                    b.w[key] = val
            else:
                b.w = {key: val}
            b.r = {}

    def op(self, en, fn, r=(), w=(), inc=True):
        self.ncall += 1
        if self.cut is not None and self.ncall > self.cut:
            return None
        self._wait(en, self._deps(r, w))
        inst = fn(self.eng[en])
        if inc:
            self.cnt[en] += 1
            inst.then_inc(self.sems[en], 1)
            val = self.cnt[en]
        else:
            val = self.cnt[en] + 1
        self._mark(en, val, r, w)
        return inst

    def dma(self, q, stream, out, in_, r=(), w=(), **kw):
        key = "d:" + stream
        self.ncall += 1
        if self.cut is not None and self.ncall > self.cut:
            return None
        if key not in self.sems:
            self._newsem(key)
        self._wait(q, self._deps(r, w))
        inst = self.eng[q].dma_start(out=out, in_=in_, **kw)
        self.cnt[key] += 16
        inst.then_inc(self.sems[key], 16)
        self._mark(key, self.cnt[key], r, w)
        return inst

    def gather(self, stream, out, in_full, idx_ap, r=(), w=()):
        key = "d:" + stream
        if key not in self.sems:
            self._newsem(key)
        self._wait("pool", self._deps(r, w))
        inst = self.nc.gpsimd.indirect_dma_start(out=out, out_offset=None, in_=in_full, in_offset=bass.IndirectOffsetOnAxis(idx_ap, 0))
        self.cnt[key] += 16
        inst.then_inc(self.sems[key], 16)
        self._mark(key, self.cnt[key], r, w)
        return inst

    def barrier(self):
        for en in self.eng:
            self._wait(en, dict(self.cnt))

    def finish(self):
        self._wait("sp", dict(self.cnt))


def build(T, L=2, debug=False, stop=None):
    assert T % 512 == 0
    NB = T // 512
    NS = T // 128
    TBC = min(1024, T)
    NSC = TBC // 128
    nc = bass.Bass("TRN2", target_bir_lowering=False)
    kb = KB(nc)

    def ext_in(name, shape):
        return Buf(nc.dram_tensor(name, list(shape), F32, kind="ExternalInput"), nowaw=True)

    xin = ext_in("x", [T, D])
    consts_d = ext_in("consts", [128, NCONST])
    params_d = ext_in("params", [L, 128, NPAR])
    w_in_d = ext_in("w_in", [L, D, DIN])
    w_out_d = ext_in("w_out", [L, D, D])
    ffn_g_d = ext_in("ffn_w_gate", [1, D, DFF])
    ffn_u_d = ext_in("ffn_w_up", [1, D, DFF])
    ffn_d_d = ext_in("ffn_w_down", [1, DFF, D])
    wr_d = ext_in("moe_router_w", [1, D, NE])
    moe_g_d = ext_in("moe_w_gate", [1, NE, D, DFF])
    moe_u_d = ext_in("moe_w_up", [1, NE, D, DFF])
    moe_d_d = ext_in("moe_w_down", [1, NE, DFF, D])
    TH = T // 2
    yout = Buf(nc.dram_tensor("y", [TH, D], F32, kind="ExternalOutput"), nowaw=True)
    tokidx_d = Buf(nc.dram_tensor("tokidx", [128, TH // 128], I32, kind="ExternalInput"), nowaw=True)

    dbg = {}

    def scratch(name, shape, dt):
        if debug:
            b = Buf(nc.dram_tensor(name, list(shape), dt, kind="ExternalOutput"), nowaw=True)
            dbg[name] = b
            return b
        return kb.dram(name, shape, dt)

    qT_d = scratch("qT_s", [512, T], BF16)
    kT_d = scratch("kT_s", [512, T], BF16)
    v_d = scratch("v_s", [T, 512], BF16)
    yT_d = scratch("yT_s", [1024, T], BF16)
    xmid_d = scratch("xmid_s", [T, D], F32)
    x1_d = kb.dram("x1_s", [T, D], F32)
    h2_d = kb.dram("h2_s", [T, D], BF16)

    cst = kb.sbuf("cst", [128, NCONST], F32)
    par = kb.sbuf("par", [128, L, NPAR], F32)
    identb = kb.sbuf("identb", [128, 128], BF16)
    blkb = kb.sbuf("blkb", [128, 128], BF16)
    onesb = kb.sbuf("onesb", [128, 128], BF16)
    dmaskb = kb.sbuf("dmaskb", [128, 4, 512], BF16)
    cmaskb = kb.sbuf("cmaskb", [128, 64], F32)
    epsc = kb.sbuf("epsc", [128, 1], F32)
    onec = kb.sbuf("onec", [128, 1], F32)
    lsig = kb.sbuf("lsig", [128, NS, 8], F32)
    negd = kb.sbuf("negd", [128, NS, 8], F32)
    cI = kb.sbuf("cI", [128, NS, 8], F32)
    lbt = kb.sbuf("lbt", [128, 2], F32)
    omlt = kb.sbuf("omlt", [128, 2], F32)
    nomlt = kb.sbuf("nomlt", [128, 2], F32)
    bshift = kb.sbuf("bshift", [128, 1], F32)
    small = kb.sbuf("small", [128, 64], F32)
    idxt = kb.sbuf("idxt", [128, TH // 128], I32)

    ps = [kb.psum("ps%d" % i, [128, 512], F32) for i in range(8)]

    kb.dma("sp", "ld", cst[:, :], consts_d[:, :], w=[cst])
    kb.dma("sp", "ld", idxt[:, :], tokidx_d[:, :], w=[idxt])
    kb.dma("sp", "ld", par[:, :, :], params_d.t.ap().rearrange("l p n -> p l n"), w=[par])
    kb.op("dve", lambda e: e.tensor_copy(out=identb[:, :], in_=cst[:, K_ID:K_ID + 128]), r=[cst], w=[identb])
    kb.op("dve", lambda e: e.tensor_copy(out=blkb[:, :], in_=cst[:, K_BLK:K_BLK + 128]), r=[cst], w=[blkb])
    kb.op("dve", lambda e: e.tensor_copy(out=onesb[:, :], in_=cst[:, K_ONES:K_ONES + 128]), r=[cst], w=[onesb])
    kb.op("dve", lambda e: e.tensor_copy(out=dmaskb[:, :, :], in_=cst[:, K_DM:K_DM + 2048].rearrange("p (j q) -> p j q", q=512)), r=[cst], w=[dmaskb])
    kb.op("dve", lambda e: e.tensor_copy(out=cmaskb[:, :], in_=cst[:, K_CM:K_CM + 64]), r=[cst], w=[cmaskb])
    kb.op("dve", lambda e: e.memset(epsc[:, :], EPS), w=[epsc])
    kb.op("dve", lambda e: e.memset(onec[:, :], 1.0), w=[onec])

    def rstd_from_ms(ms_ap, ms_bufs, out_b, out_ap, tmp_b, tmp_ap):
        kb.op("act", lambda e: e.activation(out=tmp_ap, in_=ms_ap, func=AF.Ln, bias=epsc[:, 0:1], scale=1.0), r=ms_bufs + [epsc], w=[tmp_b])
        kb.op("act", lambda e: e.activation(out=out_ap, in_=tmp_ap, func=AF.Exp, scale=-0.5), r=[tmp_b], w=[out_b])

    for l in range(L):
        x_src = xin if l == 0 else xmid_d
        x_dst = xmid_d if l == 0 else yout
        pl = lambda c0, n=1: par[:, l, c0:c0 + n]

        stackA = ExitStack()
        kb.stack = stackA
        win = kb.sbuf("win%d" % l, [128, 8, DIN], BF16)
        for c in range(8):
            kb.dma("pool", "w", win[:, c, :], w_in_d[l, c * 128:(c + 1) * 128, :], w=[win])
        xt = kb.sbuf("xt%d" % l, [128, 4, D], F32)
        hb = kb.sbuf("hb%d" % l, [128, 4, D], BF16)
        hT = kb.sbuf("hT%d" % l, [128, 8, 512], BF16)
        junk = kb.sbuf("junk%d" % l, [128, D], F32)
        st4 = kb.sbuf("st4%d" % l, [128, 8], F32)
        fa = [kb.sbuf("fa%d_%d" % (l, i), [128, 512], F32) for i in range(6)]
        fb = [kb.sbuf("fb%d_%d" % (l, i), [128, 512], BF16) for i in range(3)]
        ob = [kb.sbuf("ob%d_%d" % (l, i), [128, 512], BF16) for i in range(2)]
        vb = kb.sbuf("vb%d" % l, [128, 4, 512], BF16)
        vi = kb.sbuf("vi%d" % l, [128, 4, 256], BF16)
        ucv = [kb.sbuf("ucv%d_%d" % (l, j), [128, 514], F32) for j in range(2)]
        sig = kb.sbuf("sig%d" % l, [128, 512], F32)
        kk = kb.sbuf("kk%d" % l, [128, 512], F32)
        cc = kb.sbuf("cc%d" % l, [128, 512], F32)
        qs = kb.sbuf("qs%d" % l, [128, 512], F32)
        gs = [kb.sbuf("gs%d_%d" % (l, j), [128, 512], F32) for j in range(2)]
        qe = [kb.sbuf("qe%d_%d" % (l, j), [128, 512], BF16) for j in range(2)]
        ke = [kb.sbuf("ke%d_%d" % (l, j), [128, 512], BF16) for j in range(2)]
        qec = [kb.sbuf("qec%d_%d" % (l, j), [128, 512], BF16) for j in range(2)]
        kdT = kb.sbuf("kdT%d" % l, [128, 512], BF16)
        kd = kb.sbuf("kd%d" % l, [128, 4, 256], BF16)
        ecl = kb.sbuf("ecl%d" % l, [128, 2, 8], F32)
        state = kb.sbuf("state%d" % l, [128, 2, 64], F32)
        stateb = kb.sbuf("stateb%d" % l, [128, 2, 64], BF16)
        atsb = kb.sbuf("atsb%d" % l, [128, 128], BF16)
        osb = kb.sbuf("osb%d" % l, [128, 2, 512], F32)
        vi2 = kb.sbuf("vi2_%d" % l, [128, 8, 256], BF16)
        kd2 = kb.sbuf("kd2_%d" % l, [128, 8, 256], BF16)
        segm = kb.sbuf("segm%d" % l, [128, 512], F32)

        kb.op("dve", lambda e: e.tensor_copy(out=segm[:, :], in_=cst[:, K_SEG:K_SEG + 512]), r=[cst], w=[segm])
        kb.op("dve", lambda e: e.memset(state[:, :, :], 0.0), w=[state])
        kb.op("dve", lambda e: e.memset(stateb[:, :, :], 0.0), w=[stateb])
        for j in range(2):
            kb.op("dve", lambda e: e.memset(ucv[j][:, :], 0.0), w=[ucv[j]])
        if l == 0:
            kb.op("dve", lambda e: e.memset(lbt[:, :], 0.0), w=[lbt])
        else:
            kb.op("dve", lambda e: e.tensor_tensor(out=small[:, 0:2], in0=pl(P_LB + 2, 2), in1=pl(P_LB, 2), op=ALU.subtract), r=[par], w=[small])
            kb.op("act", lambda e: e.activation(out=lbt[:, :], in_=small[:, 0:2], func=AF.Sigmoid), r=[small], w=[lbt])
        kb.op("dve", lambda e: e.tensor_scalar(out=omlt[:, :], in0=lbt[:, :], scalar1=-1.0, scalar2=1.0, op0=ALU.mult, op1=ALU.add), r=[lbt], w=[omlt])
        kb.op("dve", lambda e: e.tensor_scalar(out=nomlt[:, :], in0=omlt[:, :], scalar1=-1.0, scalar2=None, op0=ALU.mult), r=[omlt], w=[nomlt])
        kb.op("dve", lambda e: e.tensor_reduce(out=small[:, 8:9], in_=pl(P_GQR, 64), axis=mybir.AxisListType.X, op=ALU.max, apply_absolute_value=True), r=[par], w=[small])
        kb.op("dve", lambda e: e.tensor_reduce(out=small[:, 9:10], in_=pl(P_GKR, 64), axis=mybir.AxisListType.X, op=ALU.max, apply_absolute_value=True), r=[par, small], w=[small])
        kb.op("dve", lambda e: e.scalar_tensor_tensor(out=bshift[:, :], in0=small[:, 8:9], scalar=8.0, in1=small[:, 9:10], op0=ALU.mult, op1=ALU.mult), r=[small], w=[bshift])

        rot = {"fm": 0, "tm": 0, "fa": 0, "fb": 0, "ob": 0}

        def nxt(name, n):
            v = rot[name]
            rot[name] = (v + 1) % n
            return v

        def fm_chunk(col0):
            p = ps[1 + nxt("fm", 2)]
            for c in range(8):
                kb.op("pe", lambda e: e.matmul(p[:, :], lhsT=win[:, c, col0:col0 + 128], rhs=hT[:, c, :], start=(c == 0), stop=(c == 7)),
                      r=[win, hT], w=[p], inc=(c == 7))
            return p

        def headnorm_store(src_ap, src_bufs, gain_ap, dst_d, row0, tok0, mul_b=None):
            sq = fb[nxt("fb", 3)]
            kb.op("act", lambda e: e.activation(out=sq[:, :], in_=src_ap, func=AF.Square), r=src_bufs, w=[sq])
            pm = ps[5]
            kb.op("pe", lambda e: e.matmul(pm[:, :], lhsT=blkb[:, :], rhs=sq[:, :], start=True, stop=True), r=[blkb, sq], w=[pm])
            t1 = fa[nxt("fa", 6)]
            rs = fa[nxt("fa", 6)]
            rstd_from_ms(pm[:, :], [pm], rs, rs[:, :], t1, t1[:, :])
            o = ob[nxt("ob", 2)]
            if mul_b is None:
                kb.op("dve", lambda e: e.scalar_tensor_tensor(out=o[:, :], in0=src_ap, scalar=gain_ap, in1=rs[:, :], op0=ALU.mult, op1=ALU.mult),
                      r=src_bufs + [rs, par], w=[o])
            else:
                kb.op("dve", lambda e: e.scalar_tensor_tensor(out=t1[:, :], in0=src_ap, scalar=gain_ap, in1=rs[:, :], op0=ALU.mult, op1=ALU.mult),
                      r=src_bufs + [rs, par], w=[t1])
                kb.op("dve", lambda e: e.tensor_tensor(out=o[:, :], in0=t1[:, :], in1=mul_b[:, :], op=ALU.mult), r=[t1, mul_b], w=[o])
            kb.dma("sp", "st", dst_d[row0:row0 + 128, tok0:tok0 + 512], o[:, :], r=[o], w=[dst_d])

        for tb in range(NB):
            tok0 = tb * 512
            kb.dma("sp", "ld", xt[:, :, :], x_src[tok0:tok0 + 512, :].rearrange("(s p) d -> p s d", p=128), r=[x_src], w=[xt])
            for s in range(4):
                kb.op("act", lambda e: e.activation(out=junk[:, :], in_=xt[:, s, :], func=AF.Square, accum_out=st4[:, s:s + 1]), r=[xt], w=[junk, st4])
            rstd_tm(kb, st4, epsc, 4)
            for s in range(4):
                kb.op("act", lambda e: e.activation(out=hb[:, s, :], in_=xt[:, s, :], func=AF.Identity, scale=st4[:, 4 + s:5 + s]), r=[xt, st4], w=[hb])
            pT = ps[0]
            pTb = pT.t.ap().bitcast(BF16)
            for c in range(8):
                half = (c % 2) * 512
                for s in range(4):
                    kb.op("pe", lambda e: e.transpose(pTb[:, half + s * 128:half + (s + 1) * 128], hb[:, s, c * 128:(c + 1) * 128], identb[:, :]),
                          r=[hb, identb], w=[pT], inc=(s == 3))
                en = "dve" if c % 2 == 0 else "act"
                if en == "dve":
                    kb.op("dve", lambda e: e.tensor_scalar(out=hT[:, c, :], in0=pTb[:, half:half + 512], scalar1=pl(P_G1 + c), scalar2=None, op0=ALU.mult), r=[pT, par], w=[hT])
                else:
                    kb.op("act", lambda e: e.activation(out=hT[:, c, :], in_=pTb[:, half:half + 512], func=AF.Identity, scale=pl(P_G1 + c)), r=[pT, par], w=[hT])

            for which, col0, dst, gcol in (("q", C_Q, qT_d, P_GQ), ("k", C_K, kT_d, P_GK)):
                for c in range(4):
                    p = fm_chunk(col0 + c * 128)
                    headnorm_store(p[:, :], [p], pl(gcol), dst, c * 128, tok0)

            for s in range(4):
                p = ps[3 + nxt("tm", 2)]
                for c in range(8):
                    kb.op("pe", lambda e: e.matmul(p[:, :], lhsT=hT[:, c, s * 128:(s + 1) * 128], rhs=win[:, c, C_V:C_V + 512], start=(c == 0), stop=(c == 7)),
                          r=[win, hT], w=[p], inc=(c == 7))
                kb.op("act", lambda e: e.activation(out=vb[:, s, :], in_=p[:, :], func=AF.Copy), r=[p], w=[vb])
                p2 = ps[3 + nxt("tm", 2)]
                for c in range(8):
                    kb.op("pe", lambda e: e.matmul(p2[:, 0:256], lhsT=hT[:, c, s * 128:(s + 1) * 128], rhs=win[:, c, C_RI:C_RI + 256], start=(c == 0), stop=(c == 7)),
                          r=[win, hT], w=[p2], inc=False)
                for c in range(8):
                    kb.op("pe", lambda e: e.matmul(p2[:, 256:264], lhsT=hT[:, c, s * 128:(s + 1) * 128], rhs=win[:, c, C_F:C_F + 8], start=(c == 0), stop=(c == 7)),
                          r=[win, hT], w=[p2], inc=(c == 7))
                kb.op("act", lambda e: e.activation(out=vi[:, s, :], in_=p2[:, 0:256], func=AF.Copy), r=[p2], w=[vi])
                blk = tb * 4 + s
                kb.op("dve", lambda e: e.tensor_tensor(out=small[:, 16:24], in0=p2[:, 256:264], in1=pl(P_FB, 8), op=ALU.add), r=[p2, par, vi], w=[small])
                kb.op("act", lambda e: e.activation(out=small[:, 24:32], in_=small[:, 16:24], func=AF.Abs), r=[small], w=[small])
                kb.op("act", lambda e: e.activation(out=small[:, 32:40], in_=small[:, 24:32], func=AF.Exp, scale=-1.0), r=[small], w=[small])
                kb.op("act", lambda e: e.activation(out=small[:, 40:48], in_=small[:, 32:40], func=AF.Ln, bias=onec[:, 0:1], scale=1.0), r=[small, onec], w=[small])
                kb.op("dve", lambda e: e.tensor_single_scalar(out=small[:, 48:56], in_=small[:, 16:24], scalar=0.0, op=ALU.min), r=[small], w=[small])
                kb.op("dve", lambda e: e.tensor_tensor(out=lsig[:, blk, :], in0=small[:, 48:56], in1=small[:, 40:48], op=ALU.subtract), r=[small], w=[lsig])
            kb.dma("sp", "st", v_d[tok0:tok0 + 512, :].rearrange("(s p) f -> p s f", p=128), vb[:, :, :], r=[vb], w=[v_d])

            for j in range(2):
                px = fm_chunk(C_CX + j * 128)
                t0 = fa[nxt("fa", 6)]
                kb.op("act", lambda e: e.activation(out=t0[:, :], in_=px[:, :], func=AF.Copy), r=[px], w=[t0])
                pc = fm_chunk(C_CC + j * 128)
                u = ucv[j]
                kb.op("dve", lambda e: e.tensor_tensor(out=u[:, 2:514], in0=t0[:, :], in1=pc[:, :], op=ALU.mult), r=[t0, pc], w=[u])
                t1 = fa[nxt("fa", 6)]
                kb.op("dve", lambda e: e.tensor_scalar(out=t1[:, :], in0=u[:, 0:512], scalar1=pl(P_CW + 0 * 2 + j), scalar2=None, op0=ALU.mult), r=[u, par], w=[t1])
                kb.op("dve", lambda e: e.scalar_tensor_tensor(out=t1[:, :], in0=u[:, 1:513], scalar=pl(P_CW + 1 * 2 + j), in1=t1[:, :], op0=ALU.mult, op1=ALU.add), r=[u, par, t1], w=[t1])
                kb.op("dve", lambda e: e.scalar_tensor_tensor(out=t1[:, :], in0=u[:, 2:514], scalar=pl(P_CW + 2 * 2 + j), in1=t1[:, :], op0=ALU.mult, op1=ALU.add), r=[u, par, t1], w=[t1])
                pbg = fm_chunk(C_CB + j * 128)
                kb.op("dve", lambda e: e.tensor_tensor(out=t0[:, :], in0=t1[:, :], in1=pbg[:, :], op=ALU.mult), r=[t1, pbg], w=[t0])
                kb.op("dve", lambda e: e.tensor_copy(out=small[:, 56:58], in_=u[:, 512:514]), r=[u], w=[small])
                kb.op("dve", lambda e: e.tensor_copy(out=u[:, 0:2], in_=small[:, 56:58]), r=[small], w=[u])
                headnorm_store(t0[:, :], [t0], pl(P_MG + 4 + j), yT_d, 512 + j * 128, tok0)

            for j in range(2):
                pf = fm_chunk(C_RF + j * 128)
                kb.op("act", lambda e: e.activation(out=sig[:, :], in_=pf[:, :], func=AF.Sigmoid), r=[pf], w=[sig])
                tf = fa[nxt("fa", 6)]
                kb.op("dve", lambda e: e.tensor_scalar(out=tf[:, :], in0=sig[:, :], scalar1=omlt[:, j:j + 1], scalar2=lbt[:, j:j + 1], op0=ALU.mult, op1=ALU.add), r=[sig, omlt, lbt], w=[tf])
                kb.op("dve", lambda e: e.tensor_single_scalar(out=tf[:, :], in_=tf[:, :], scalar=TINY, op=ALU.max), r=[tf], w=[tf])
                kb.op("act", lambda e: e.activation(out=tf[:, :], in_=tf[:, :], func=AF.Ln), r=[tf], w=[tf])
                kb.op("dve", lambda e: e.tensor_scalar(out=kk[:, :], in0=sig[:, :], scalar1=nomlt[:, j:j + 1], scalar2=omlt[:, j:j + 1], op0=ALU.mult, op1=ALU.add), r=[sig, omlt, nomlt], w=[kk])
                kb.op("dve", lambda e: e.tensor_tensor_scan(out=cc[:, :], data0=segm[:, :], data1=tf[:, :], initial=0.0, op0=ALU.mult, op1=ALU.add), r=[segm, tf], w=[cc])
                c3 = cc.t.ap().rearrange("p (n t) -> p n t", t=64)
                pq = fm_chunk(C_RQ + j * 128)
                kb.op("act", lambda e: e.activation(out=qs[:, :], in_=pq[:, :], func=AF.Silu), r=[pq], w=[qs])
                pg = fm_chunk(C_RG + j * 128)
                kb.op("act", lambda e: e.activation(out=gs[j][:, :], in_=pg[:, :], func=AF.Silu), r=[pg], w=[gs[j]])
                d1 = fa[nxt("fa", 6)]
                kb.op("dve", lambda e: e.tensor_tensor(out=d1.t.ap().rearrange("p (n t) -> p n t", t=64), in0=c3, in1=c3[:, :, 31:32].to_broadcast([128, 8, 64]), op=ALU.subtract), r=[cc], w=[d1])
                ex = fa[nxt("fa", 6)]
                kb.op("act", lambda e: e.activation(out=ex[:, :], in_=d1[:, :], func=AF.Exp), r=[d1], w=[ex])
                kb.op("dve", lambda e: e.tensor_tensor(out=qe[j][:, :], in0=qs[:, :], in1=ex[:, :], op=ALU.mult), r=[qs, ex], w=[qe[j]])
                kb.op("act", lambda e: e.activation(out=ex[:, :], in_=d1[:, :], func=AF.Exp, scale=-1.0), r=[d1], w=[ex])
                kb.op("dve", lambda e: e.tensor_tensor(out=ke[j][:, :], in0=kk[:, :], in1=ex[:, :], op=ALU.mult), r=[kk, ex], w=[ke[j]])
                kb.op("act", lambda e: e.activation(out=ex[:, :], in_=cc[:, :], func=AF.Exp), r=[cc], w=[ex])
                kb.op("dve", lambda e: e.tensor_tensor(out=qec[j][:, :], in0=qs[:, :], in1=ex[:, :], op=ALU.mult), r=[qs, ex], w=[qec[j]])
                kb.op("dve", lambda e: e.tensor_tensor(out=d1.t.ap().rearrange("p (n t) -> p n t", t=64), in0=c3[:, :, 63:64].to_broadcast([128, 8, 64]), in1=c3, op=ALU.subtract), r=[cc], w=[d1])
                kb.op("act", lambda e: e.activation(out=ex[:, :], in_=d1[:, :], func=AF.Exp), r=[d1], w=[ex])
                kb.op("dve", lambda e: e.tensor_tensor(out=kdT[:, :], in0=kk[:, :], in1=ex[:, :], op=ALU.mult), r=[kk, ex], w=[kdT])
                kb.op("act", lambda e: e.activation(out=ecl[:, j, :], in_=c3[:, :, 63], func=AF.Exp), r=[cc], w=[ecl])
                pT = ps[0]
                pTb = pT.t.ap().bitcast(BF16)
                for s in range(4):
                    kb.op("pe", lambda e: e.transpose(pTb[:, s * 128:(s + 1) * 128], kdT[:, s * 128:(s + 1) * 128], identb[:, :]), r=[kdT, identb], w=[pT], inc=(s == 3))
                kb.op("dve", lambda e: e.tensor_copy(out=kd[:, :, j * 128:(j + 1) * 128], in_=pTb[:, 0:512].rearrange("p (s f) -> p s f", f=128)), r=[pT], w=[kd])

            for pr in range(2):
                for dst in range(2):
                    kb.dma("sp", "cp", vi2.t.ap().rearrange("p (s two) f -> p s two f", two=2)[dst * 64:(dst + 1) * 64, :, pr, :], vi[pr * 64:(pr + 1) * 64, :, :], r=[vi], w=[vi2])
                    kb.dma("sp", "cp", kd2.t.ap().rearrange("p (s two) f -> p s two f", two=2)[dst * 64:(dst + 1) * 64, :, pr, :], kd[pr * 64:(pr + 1) * 64, :, :], r=[kd], w=[kd2])
            for ch in range(8):
                cols = slice(ch * 64, (ch + 1) * 64)
                for hh in range(2):
                    pb = hh * 64
                    ph = ps[6 + hh]
                    for j in range(2):
                        kb.op("pe", lambda e: e.matmul(ph[pb:pb + 64, j * 64:(j + 1) * 64], lhsT=ke[j][pb:pb + 64, cols], rhs=qe[j][pb:pb + 64, cols], start=True, stop=True, tile_position=(pb, pb)),
                              r=[ke[j], qe[j]], w=[ph], inc=(j == 1))
                for hh in range(2):
                    pb = hh * 64
                    ph = ps[6 + hh]
                    kb.op("dve", lambda e: e.tensor_tensor(out=atsb[pb:pb + 64, 0:128].rearrange("p (h t) -> p h t", t=64), in0=ph[pb:pb + 64, 0:128].rearrange("p (h t) -> p h t", t=64),
                                                           in1=cmaskb[pb:pb + 64, :].unsqueeze(1).to_broadcast([64, 2, 64]), op=ALU.mult), r=[ph, cmaskb], w=[atsb])
                for hh in range(2):
                    pb = hh * 64
                    ph = ps[6 + hh]
                    for j in range(2):
                        hd = j * 2 + hh
                        oslc = ph[pb:pb + 64, 128 + j * 64:128 + (j + 1) * 64]
                        kb.op("pe", lambda e: e.matmul(oslc, lhsT=vi2[pb:pb + 64, ch, hd * 64:(hd + 1) * 64], rhs=atsb[pb:pb + 64, j * 64:(j + 1) * 64], start=True, stop=False, tile_position=(pb, pb)),
                              r=[vi2, atsb], w=[ph], inc=False)
                        kb.op("pe", lambda e: e.matmul(oslc, lhsT=stateb[pb:pb + 64, j, :], rhs=qec[j][pb:pb + 64, cols], start=False, stop=True, tile_position=(pb, pb)),
                              r=[stateb, qec[j]], w=[ph], inc=False)
                        kb.op("pe", lambda e: e.matmul(ph[pb:pb + 64, 256 + j * 64:256 + (j + 1) * 64], lhsT=kd2[pb:pb + 64, ch, hd * 64:(hd + 1) * 64], rhs=vi2[pb:pb + 64, ch, hd * 64:(hd + 1) * 64], start=True, stop=True, tile_position=(pb, pb)),
                              r=[kd2, vi2], w=[ph], inc=(j == 1))
                kb.op("dve", lambda e: e.tensor_tensor(out=state[:, :, :], in0=state[:, :, :], in1=ecl[:, :, ch:ch + 1].to_broadcast([128, 2, 64]), op=ALU.mult), r=[state, ecl], w=[state])
                for hh in range(2):
                    pb = hh * 64
                    ph = ps[6 + hh]
                    kb.op("act", lambda e: e.activation(out=osb[pb:pb + 64, :, cols], in_=ph[pb:pb + 64, 128:256].rearrange("p (j t) -> p j t", t=64), func=AF.Copy), r=[ph], w=[osb])
                    kb.op("dve", lambda e: e.tensor_tensor(out=state[pb:pb + 64, :, :], in0=state[pb:pb + 64, :, :], in1=ph[pb:pb + 64, 256:384].rearrange("p (j v) -> p j v", v=64), op=ALU.add), r=[state, ph, osb], w=[state])
                kb.op("dve", lambda e: e.tensor_copy(out=stateb[:, :, :], in_=state[:, :, :]), r=[state], w=[stateb])
            for j in range(2):
                headnorm_store(osb[:, j, :], [osb], pl(P_MG + 6 + j), yT_d, 768 + j * 128, tok0, mul_b=gs[j])

        lflat = lsig.t.ap().rearrange("p n h -> p (n h)")
        NW = NS * 8
        tri = cst[:, K_TRI:K_TRI + 128]
        onesf = cst[:, K_ONES:K_ONES + 128]
        sel = cst[:, K_SEL:K_SEL + 128]
        pcs, ptot = ps[1], ps[2]
        kb.op("pe", lambda e: e.matmul(pcs[:, 0:NW], lhsT=tri, rhs=lflat, start=True, stop=True), r=[cst, lsig], w=[pcs])
        kb.op("pe", lambda e: e.matmul(ptot[:, 0:NW], lhsT=onesf, rhs=lflat, start=True, stop=True), r=[cst, lsig], w=[ptot])
        tot = fa[0]
        kb.op("act", lambda e: e.activation(out=tot[:, 0:NW], in_=ptot[:, 0:NW], func=AF.Copy), r=[ptot], w=[tot])
        kb.op("dve", lambda e: e.memset(cI[:, 0, :], 0.0), w=[cI])
        for b in range(1, NS):
            kb.op("dve", lambda e: e.tensor_tensor(out=cI[:, b, :], in0=cI[:, b - 1, :], in1=tot[:, (b - 1) * 8:b * 8], op=ALU.add), r=[cI, tot], w=[cI])
        dcum = fa[1]
        kb.op("dve", lambda e: e.tensor_tensor(out=dcum[:, 0:NW], in0=pcs[:, 0:NW], in1=cI.t.ap().rearrange("p n h -> p (n h)"), op=ALU.add), r=[pcs, cI], w=[dcum])
        kb.op("dve", lambda e: e.tensor_scalar(out=negd.t.ap().rearrange("p n h -> p (n h)"), in0=dcum[:, 0:NW], scalar1=-1.0, scalar2=None, op0=ALU.mult), r=[dcum], w=[negd])
        pbc = ps[3]
        kb.op("pe", lambda e: e.matmul(pbc[:, 0:NW], lhsT=sel, rhs=dcum[:, 0:NW], start=True, stop=True), r=[cst, dcum], w=[pbc])
        kb.op("dve", lambda e: e.tensor_scalar(out=cI.t.ap().rearrange("p n h -> p (n h)"), in0=pbc[:, 0:NW], scalar1=bshift[:, 0:1], scalar2=None, op0=ALU.subtract), r=[pbc, bshift], w=[cI])

        kb.barrier()
        if stop == ("A", l):
            kb.finish()
            return nc, dbg
        stackA.close()
        stackB = ExitStack()
        kb.stack = stackB
        obB = [kb.sbuf("obB%d_%d" % (l, i), [128, 512], BF16) for i in range(2)]
        kTc = kb.sbuf("kTc%d" % l, [128, T], BF16)
        qTc = kb.sbuf("qTc%d" % l, [128, T], BF16)
        vh = kb.sbuf("vh%d" % l, [128, NS, 128], BF16)
        pTt = [kb.sbuf("pTt%d_%d" % (l, i), [128, 512], BF16) for i in range(3)]
        biasb = [kb.sbuf("biasb%d_%d" % (l, i), [128, NS], F32) for i in range(2)]
        rden = kb.sbuf("rden%d" % l, [128, 512], F32)
        on = kb.sbuf("on%d" % l, [128, 512], F32)
        rotb = {"s": 0, "p": 0, "b": 0, "o": 0, "ob": 0}

        def nxb(name, n):
            v = rotb[name]
            rotb[name] = (v + 1) % n
            return v

        for c in range(4):
            kb.dma("sp", "ld", kTc[:, :], kT_d[c * 128:(c + 1) * 128, :], r=[kT_d], w=[kTc])
            kb.dma("sp", "ld", qTc[:, :], qT_d[c * 128:(c + 1) * 128, :], r=[qT_d], w=[qTc])
            kb.dma("sp", "ld", vh[:, :, :], v_d[:, c * 128:(c + 1) * 128].rearrange("(n p) f -> p n f", p=128), r=[v_d], w=[vh])
            for I in range(NB):
                oi = nxb("o", 2)
                pO, pD = ps[3 + oi], ps[5 + oi]
                nJ = 4 * (I + 1)
                for hh in range(2):
                    pb = hh * 64
                    h = 2 * c + hh
                    bb = biasb[nxb("b", 2)]
                    kb.op("dve", lambda e: e.tensor_scalar(out=bb[:, 0:nJ], in0=negd[:, 0:nJ, h], scalar1=cI[:, 4 * I + 1, h:h + 1], scalar2=None, op0=ALU.add), r=[negd, cI], w=[bb])
                    for J in range(nJ):
                        pS = ps[nxb("s", 3)]
                        kb.op("pe", lambda e: e.matmul(pS[:, :], lhsT=kTc[pb:pb + 64, J * 128:(J + 1) * 128], rhs=qTc[pb:pb + 64, I * 512:(I + 1) * 512], start=True, stop=True, tile_position=(pb, 0)),
                              r=[kTc, qTc], w=[pS])
                        pt = pTt[nxb("p", 3)]
                        kb.op("act", lambda e: e.activation(out=pt[:, :], in_=pS[:, :], func=AF.Exp, bias=bb[:, J:J + 1], scale=0.125), r=[pS, bb], w=[pt])
                        if J >= 4 * I:
                            kb.op("dve", lambda e: e.tensor_tensor(out=pt[:, :], in0=pt[:, :], in1=dmaskb[:, J - 4 * I, :], op=ALU.mult), r=[pt, dmaskb], w=[pt])
                        kb.op("pe", lambda e: e.matmul(pO[pb:pb + 64, :], lhsT=vh[:, J, pb:pb + 64], rhs=pt[:, :], start=(J == 0), stop=(J == nJ - 1), tile_position=(0, pb)), r=[vh, pt], w=[pO], inc=False)
                        kb.op("pe", lambda e: e.matmul(pD[pb:pb + 64, :], lhsT=onesb[:, 0:64], rhs=pt[:, :], start=(J == 0), stop=(J == nJ - 1), tile_position=(0, pb)), r=[onesb, pt], w=[pD])
                kb.op("dve", lambda e: e.reciprocal(out=rden[:, :], in_=pD[:, :]), r=[pD], w=[rden])
                kb.op("dve", lambda e: e.tensor_tensor(out=on[:, :], in0=pO[:, :], in1=rden[:, :], op=ALU.mult), r=[pO, rden], w=[on])
                sq = pTt[nxb("p", 3)]
                kb.op("act", lambda e: e.activation(out=sq[:, :], in_=on[:, :], func=AF.Square), r=[on], w=[sq])
                pm = ps[7]
                kb.op("pe", lambda e: e.matmul(pm[:, :], lhsT=blkb[:, :], rhs=sq[:, :], start=True, stop=True), r=[blkb, sq], w=[pm])
                rstd_from_ms(pm[:, :], [pm], rden, rden[:, :], rden, rden[:, :])
                o = obB[nxb("ob", 2)]
                kb.op("dve", lambda e: e.scalar_tensor_tensor(out=o[:, :], in0=on[:, :], scalar=pl(P_MG + c), in1=rden[:, :], op0=ALU.mult, op1=ALU.mult), r=[on, rden, par], w=[o])
                kb.dma("sp", "st", yT_d[c * 128:(c + 1) * 128, I * 512:(I + 1) * 512], o[:, :], r=[o], w=[yT_d])

        kb.barrier()
        if stop == ("B", l):
            kb.finish()
            return nc, dbg
        stackB.close()
        stackC = ExitStack()
        kb.stack = stackC
        junk = kb.sbuf("junkC%d" % l, [128, D], F32)
        wout = kb.sbuf("wout%d" % l, [128, 8, D], BF16)
        for c in range(8):
            kb.dma("pool", "w", wout[:, c, :], w_out_d[l, c * 128:(c + 1) * 128, :], w=[wout])
        x1 = kb.sbuf("x1_%d" % l, [128, NSC, D], F32)
        yTb = kb.sbuf("yTb%d" % l, [128, 8, TBC], BF16)
        h2T = yTb
        h2b = [kb.sbuf("h2b%d_%d" % (l, i), [128, D], BF16) for i in range(2)]
        stc = kb.sbuf("stc%d" % l, [128, 2 * NSC], F32)
        wgb = [kb.sbuf("wgb%d_%d" % (l, i), [128, 8, 512], BF16) for i in range(2)]
        wub = [kb.sbuf("wub%d_%d" % (l, i), [128, 8, 512], BF16) for i in range(2)]
        wdb = [kb.sbuf("wdb%d_%d" % (l, i), [128, 4, D], BF16) for i in range(2)]
        hact = kb.sbuf("hact%d" % l, [128, 4, TBC], BF16)
        sgt = [kb.sbuf("sgt%d_%d" % (l, i), [128, 512], BF16) for i in range(2)]
        moe = (l % 2 == 1)
        if moe:
            wrb = kb.sbuf("wrb%d" % l, [128, 8, NE], BF16)
            kb.dma("pool", "w", wrb[:, :, :], wr_d[0].rearrange("(c p) e -> p c e", p=128), w=[wrb])
            gates = kb.sbuf("gates%d" % l, [128, NSC, NE], F32)
            rt = kb.sbuf("rt%d" % l, [128, 64], F32)
        rc = {"a": 0, "g": 0, "u": 0, "w": 0, "h": 0, "sg": 0}

        def nxc(name, n):
            v = rc[name]
            rc[name] = (v + 1) % n
            return v

        passes = [("full", T // TBC)] if not moe else [("c1", T // TBC), ("c2", TH // TBC)]
        for mode, tbc in [(m_, t_) for m_, n_ in passes for t_ in range(n_)]:
            tok0 = tbc * TBC
            if mode != "c2":
                kb.dma("sp", "ld", x1[:, :, :], x_src[tok0:tok0 + TBC, :].rearrange("(s p) d -> p s d", p=128), r=[x_src], w=[x1])
                kb.dma("sp", "ld", yTb[:, :, :], yT_d[:, tok0:tok0 + TBC].rearrange("(c p) t -> p c t", p=128), r=[yT_d], w=[yTb])
            for s in (range(NSC) if mode != "c2" else []):
                for half in range(2):
                    p = ps[1 + nxc("a", 2)]
                    for c in range(8):
                        kb.op("pe", lambda e: e.matmul(p[:, :], lhsT=yTb[:, c, s * 128:(s + 1) * 128], rhs=wout[:, c, half * 512:(half + 1) * 512], start=(c == 0), stop=(c == 7)),
                              r=[yTb, wout], w=[p], inc=(c == 7))
                    kb.op("dve", lambda e: e.tensor_tensor(out=x1[:, s, half * 512:(half + 1) * 512], in0=x1[:, s, half * 512:(half + 1) * 512], in1=p[:, :], op=ALU.add), r=[x1, p], w=[x1])
            if mode != "c2":
                for s in range(NSC):
                    kb.op("act", lambda e: e.activation(out=junk[:, :], in_=x1[:, s, :], func=AF.Square, accum_out=stc[:, s:s + 1]), r=[x1], w=[junk, stc])
                rstd_tm(kb, stc, epsc, NSC)
            for s in range(NSC):
                hbb = h2b[nxc("h", 2)]
                if mode != "c2":
                    kb.op("act", lambda e: e.activation(out=hbb[:, :], in_=x1[:, s, :], func=AF.Identity, scale=stc[:, NSC + s:NSC + s + 1]), r=[x1, stc], w=[hbb])
                if mode == "c1":
                    kb.dma("sp", "st", h2_d[tok0 + s * 128:tok0 + (s + 1) * 128, :], hbb[:, :], r=[hbb], w=[h2_d])
                    continue
                if mode == "c2":
                    ic = tbc * NSC + s
                    kb.gather("g", x1[:, s, :], x1_d.t.ap(), idxt[:, ic:ic + 1], r=[x1_d, idxt], w=[x1])
                    kb.gather("g", hbb[:, :], h2_d.t.ap(), idxt[:, ic:ic + 1], r=[h2_d, idxt], w=[hbb])
                pT = ps[0]
                pTb = pT.t.ap().bitcast(BF16)
                for c in range(8):
                    kb.op("pe", lambda e: e.transpose(pTb[:, c * 128:(c + 1) * 128], hbb[:, c * 128:(c + 1) * 128], identb[:, :]), r=[hbb, identb], w=[pT], inc=(c == 7))
                kb.op("dve", lambda e: e.tensor_tensor(out=h2T[:, :, s * 128:(s + 1) * 128], in0=pTb[:, 0:1024].rearrange("p (c t) -> p c t", t=128),
                                                       in1=pl(P_G2, 8).unsqueeze(2).to_broadcast([128, 8, 128]), op=ALU.mult), r=[pT, par], w=[h2T])
            if mode == "c1":
                kb.dma("sp", "st", x1_d[tok0:tok0 + TBC, :].rearrange("(s p) d -> p s d", p=128), x1[:, :, :], r=[x1], w=[x1_d])
                continue
            if moe:
                for s in range(NSC):
                    p = ps[1 + nxc("a", 2)]
                    for c in range(8):
                        kb.op("pe", lambda e: e.matmul(p[:, 0:NE], lhsT=h2T[:, c, s * 128:(s + 1) * 128], rhs=wrb[:, c, :], start=(c == 0), stop=(c == 7)), r=[h2T, wrb], w=[p], inc=(c == 7))
                    kb.op("dve", lambda e: e.tensor_tensor(out=rt[:, 0:8], in0=p[:, 0:NE], in1=pl(P_RB, 8), op=ALU.add), r=[p, par], w=[rt])
                    kb.op("dve", lambda e: e.max(out=rt[:, 8:16], in_=rt[:, 0:8]), r=[rt], w=[rt])
                    kb.op("dve", lambda e: e.tensor_tensor(out=rt[:, 16:17], in0=rt[:, 8:9], in1=rt[:, 9:10], op=ALU.subtract), r=[rt], w=[rt])
                    kb.op("act", lambda e: e.activation(out=rt[:, 17:18], in_=rt[:, 16:17], func=AF.Sigmoid), r=[rt], w=[rt])
                    kb.op("act", lambda e: e.activation(out=rt[:, 18:19], in_=rt[:, 16:17], func=AF.Sigmoid, scale=-1.0), r=[rt], w=[rt])
                    kb.op("dve", lambda e: e.tensor_scalar(out=rt[:, 24:32], in0=rt[:, 0:8], scalar1=rt[:, 8:9], scalar2=rt[:, 17:18], op0=ALU.is_equal, op1=ALU.mult), r=[rt], w=[rt])
                    kb.op("dve", lambda e: e.tensor_scalar(out=rt[:, 32:40], in0=rt[:, 0:8], scalar1=rt[:, 9:10], scalar2=rt[:, 18:19], op0=ALU.is_equal, op1=ALU.mult), r=[rt], w=[rt])
                    kb.op("dve", lambda e: e.tensor_tensor(out=gates[:, s, :], in0=rt[:, 24:32], in1=rt[:, 32:40], op=ALU.add), r=[rt], w=[gates])
            experts = range(NE) if moe else [None]
            for ex in experts:
                if ex is None:
                    Wg, Wu, Wd = ffn_g_d.t.ap()[0], ffn_u_d.t.ap()[0], ffn_d_d.t.ap()[0]
                    wbufs = [ffn_g_d, ffn_u_d, ffn_d_d]
                else:
                    Wg, Wu, Wd = moe_g_d.t.ap()[0, ex], moe_u_d.t.ap()[0, ex], moe_d_d.t.ap()[0, ex]
                    wbufs = [moe_g_d, moe_u_d, moe_d_d]
                for fg in range(DFF // 512):
                    wi = nxc("w", 2)
                    kb.dma("pool", "w", wgb[wi][:, :, :], Wg[:, fg * 512:(fg + 1) * 512].rearrange("(c p) f -> p c f", p=128), w=[wgb[wi]])
                    kb.dma("pool", "w", wub[wi][:, :, :], Wu[:, fg * 512:(fg + 1) * 512].rearrange("(c p) f -> p c f", p=128), w=[wub[wi]])
                    kb.dma("pool", "w", wdb[wi][:, :, :], Wd[fg * 512:(fg + 1) * 512, :].rearrange("(k p) d -> p k d", p=128), w=[wdb[wi]])
                    for t4 in range(TBC // 512):
                        tsl = slice(t4 * 512, (t4 + 1) * 512)
                        for k in range(4):
                            pg = ps[3 + nxc("g", 2)]
                            pu = ps[5 + nxc("u", 2)]
                            for c in range(8):
                                kb.op("pe", lambda e: e.matmul(pg[:, :], lhsT=wgb[wi][:, c, k * 128:(k + 1) * 128], rhs=h2T[:, c, tsl], start=(c == 0), stop=(c == 7)), r=[wgb[wi], h2T], w=[pg], inc=(c == 7))
                            for c in range(8):
                                kb.op("pe", lambda e: e.matmul(pu[:, :], lhsT=wub[wi][:, c, k * 128:(k + 1) * 128], rhs=h2T[:, c, tsl], start=(c == 0), stop=(c == 7)), r=[wub[wi], h2T], w=[pu], inc=(c == 7))
                            sg = sgt[nxc("sg", 2)]
                            kb.op("act", lambda e: e.activation(out=sg[:, :], in_=pg[:, :], func=AF.Silu), r=[pg], w=[sg])
                            kb.op("dve", lambda e: e.tensor_tensor(out=hact[:, k, tsl], in0=sg[:, :], in1=pu[:, :], op=ALU.mult), r=[sg, pu], w=[hact])
                    for s in range(NSC):
                        for half in range(2):
                            p = ps[1 + nxc("a", 2)]
                            for k in range(4):
                                kb.op("pe", lambda e: e.matmul(p[:, :], lhsT=hact[:, k, s * 128:(s + 1) * 128], rhs=wdb[wi][:, k, half * 512:(half + 1) * 512], start=(k == 0), stop=(k == 3)),
                                      r=[hact, wdb[wi]], w=[p], inc=(k == 3))
                            xs = x1[:, s, half * 512:(half + 1) * 512]
                            if ex is None:
                                kb.op("dve", lambda e: e.tensor_tensor(out=xs, in0=xs, in1=p[:, :], op=ALU.add), r=[x1, p], w=[x1])
                            else:
                                kb.op("dve", lambda e: e.scalar_tensor_tensor(out=xs, in0=p[:, :], scalar=gates[:, s, ex:ex + 1], in1=xs, op0=ALU.mult, op1=ALU.add), r=[x1, p, gates], w=[x1])
            kb.dma("sp", "st", x_dst[tok0:tok0 + TBC, :].rearrange("(s p) d -> p s d", p=128), x1[:, :, :], r=[x1], w=[x_dst])
        kb.barrier()
        stackC.close()
        kb.stack = None

    kb.finish()
    return nc, dbg


def rstd_tm(kb, st, epsc, n):
    kb.op("act", lambda e: e.activation(out=st[:, n:2 * n], in_=st[:, 0:n], func=AF.Ln, bias=epsc[:, 0:1], scale=1.0 / D), r=[st, epsc], w=[st])
    kb.op("act", lambda e: e.activation(out=st[:, n:2 * n], in_=st[:, n:2 * n], func=AF.Exp, scale=-0.5), r=[st], w=[st])


_CACHE = {}


def kernel(**inputs):
    x = np.ascontiguousarray(inputs["x"], dtype=np.float32)
    B, S, _ = x.shape
    key = S
    if key not in _CACHE:
        _CACHE[key] = build(S)[0]
    nc = _CACHE[key]
    consts = make_consts()
    params = make_params(inputs)
    shared = {k: np.ascontiguousarray(inputs[k], dtype=np.float32) for k in
              ("w_in", "w_out", "ffn_w_gate", "ffn_w_up", "ffn_w_down", "moe_router_w", "moe_w_gate", "moe_w_up", "moe_w_down")}
    n = 8
    TH = S // 2
    in_maps = []
    for cid in range(n):
        m = dict(shared)
        m["x"] = x[cid % B]
        m["consts"] = consts
        m["params"] = params
        rank = cid // B
        m["tokidx"] = (rank * TH + np.arange(TH, dtype=np.int32)).reshape(TH // 128, 128).T.copy()
        in_maps.append(m)
    res = run_bass_kernel_spmd(nc, in_maps, core_ids=list(range(n)))
    out = np.stack([np.concatenate([res.results[b]["y"], res.results[b + B]["y"]], axis=0) for b in range(B)], axis=0)
    return out.astype(np.float32)
```

```python
import numpy as np
from contextlib import ExitStack
import concourse.bass as bass
import concourse.mybir as mybir
from concourse.bass_utils import run_bass_kernel_spmd

F32 = mybir.dt.float32
BF16 = mybir.dt.bfloat16
I32 = mybir.dt.int32
AF = mybir.ActivationFunctionType
ALU = mybir.AluOpType

D = 1024
DIN = 3336
DFF = 3584
NE = 8
EPS = 1e-6
TINY = 1e-30
C_Q, C_K, C_V, C_F = 0, 512, 1024, 1536
C_CX, C_CB, C_CC = 1544, 1800, 2056
C_RQ, C_RF, C_RI, C_RG = 2312, 2568, 2824, 3080

K_ID, K_BLK, K_TRI, K_ONES, K_SEL = 0, 128, 256, 384, 512
K_DM = 640
K_CM = K_DM + 2048
K_SEG = K_CM + 64
NCONST = K_SEG + 512

P_G1, P_G2, P_GQ, P_GK, P_CW, P_LB, P_MG, P_FB, P_RB, P_GQR, P_GKR = 0, 8, 16, 17, 18, 24, 28, 36, 44, 52, 116
NPAR = 180


def make_consts():
    c = np.zeros((128, NCONST), np.float32)
    p = np.arange(128)
    c[:, K_ID:K_ID + 128] = np.eye(128)
    c[:, K_BLK:K_BLK + 128] = ((p[:, None] // 64) == (p[None, :] // 64)) / 64.0
    c[:, K_TRI:K_TRI + 128] = (p[:, None] <= p[None, :])
    c[:, K_ONES:K_ONES + 128] = 1.0
    c[127, K_SEL:K_SEL + 128] = 1.0
    q = np.arange(512)
    for j in range(4):
        c[:, K_DM + j * 512:K_DM + (j + 1) * 512] = ((j * 128 + p[:, None]) <= q[None, :])
    t = np.arange(64)
    c[:, K_CM:K_CM + 64] = ((p[:, None] % 64) <= t[None, :])
    c[:, K_SEG:K_SEG + 512] = ((q % 64) != 0)[None, :]
    return c


def make_params(inp):
    L = 2
    P = np.zeros((L, 128, NPAR), np.float32)
    for l in range(L):
        P[l, :, P_G1:P_G1 + 8] = inp["norm_mix"][l].reshape(8, 128).T
        P[l, :, P_G2:P_G2 + 8] = inp["norm_ffn"][l].reshape(8, 128).T
        P[l, :, P_GQ] = np.tile(inp["q_norm_gain"][l], 2)
        P[l, :, P_GK] = np.tile(inp["k_norm_gain"][l], 2)
        P[l, :, P_CW:P_CW + 6] = inp["conv_w"][l].reshape(3, 2, 128).transpose(2, 0, 1).reshape(128, 6)
        P[l, :, P_LB:P_LB + 4] = inp["hgrn_lb_logits"].reshape(2, 2, 128).transpose(2, 0, 1).reshape(128, 4)
        P[l, :, P_MG:P_MG + 8] = inp["mix_out_gain"][l].reshape(8, 128).T
        P[l, :, P_FB:P_FB + 8] = inp["attn_f_bias"][l][None, :]
        P[l, :, P_RB:P_RB + 8] = inp["moe_router_b"][0][None, :]
        P[l, :, P_GQR:P_GQR + 64] = inp["q_norm_gain"][l][None, :]
        P[l, :, P_GKR:P_GKR + 64] = inp["k_norm_gain"][l][None, :]
    return P


class Buf:
    __slots__ = ("t", "w", "r", "nowaw")

    def __init__(self, t, nowaw=False):
        self.t = t
        self.w = {}
        self.r = {}
        self.nowaw = nowaw

    def __getitem__(self, idx):
        return self.t.ap()[idx]


class KB:
    def __init__(self, nc):
        self.nc = nc
        self.eng = {"pe": nc.tensor, "act": nc.scalar, "dve": nc.vector, "pool": nc.gpsimd, "sp": nc.sync}
        self.sems = {}
        self.cnt = {}
        self.waited = {e: {} for e in self.eng}
        self.nsem = 0
        self.stack = None
        self.ncall = 0
        import os as _os
        self.noself = _os.environ.get('NOSELF', '0') == '1'
        import os
        self.cut = int(os.environ['KCUT']) if 'KCUT' in os.environ else None
        for e in self.eng:
            self._newsem(e)

    def _newsem(self, key):
        self.nsem += 1
        self.sems[key] = self.nc.alloc_semaphore(name="s%d_%s" % (self.nsem, key.replace(":", "_")))
        self.cnt[key] = 0

    def sbuf(self, name, shape, dt):
        if self.stack is not None:
            return Buf(self.stack.enter_context(self.nc.sbuf_tensor(name, list(shape), dt)))
        return Buf(self.nc.alloc_sbuf_tensor(name, list(shape), dt))

    def psum(self, name, shape, dt):
        return Buf(self.nc.alloc_psum_tensor(name, list(shape), dt))

    def dram(self, name, shape, dt, kind=None):
        if kind is None:
            t = self.nc.dram_tensor(name, list(shape), dt)
        else:
            t = self.nc.dram_tensor(name, list(shape), dt, kind=kind)
        return Buf(t, nowaw=True)

    def _wait(self, en, toks):
        e = self.eng[en]
        wd = self.waited[en]
        for k, v in toks.items():
            if en == "pe" and k == "pe":
                continue
            if self.noself and k == en:
                continue
            if wd.get(k, 0) < v:
                e.wait_ge(self.sems[k], v)
                wd[k] = v

    def _deps(self, r, w):
        toks = {}
        for b in r:
            for k, v in b.w.items():
                if toks.get(k, 0) < v:
                    toks[k] = v
        for b in w:
            for k, v in b.r.items():
                if toks.get(k, 0) < v:
                    toks[k] = v
            if not b.nowaw:
                for k, v in b.w.items():
                    if toks.get(k, 0) < v:
                        toks[k] = v
        return toks

    def _mark(self, key, val, r, w):
        for b in r:
            if b.r.get(key, 0) < val:
                b.r[key] = val
        for b in w:
            if b.nowaw:
                if b.w.get(key, 0) < val:
                    b.w[key] = val
            else:
                b.w = {key: val}
            b.r = {}

    def op(self, en, fn, r=(), w=(), inc=True):
        self.ncall += 1
        if self.cut is not None and self.ncall > self.cut:
            return None
        self._wait(en, self._deps(r, w))
        inst = fn(self.eng[en])
        if inc:
            self.cnt[en] += 1
            inst.then_inc(self.sems[en], 1)
            val = self.cnt[en]
        else:
            val = self.cnt[en] + 1
        self._mark(en, val, r, w)
        return inst

    def dma(self, q, stream, out, in_, r=(), w=(), **kw):
        key = "d:" + stream
        self.ncall += 1
        if self.cut is not None and self.ncall > self.cut:
            return None
        if key not in self.sems:
            self._newsem(key)
        self._wait(q, self._deps(r, w))
        inst = self.eng[q].dma_start(out=out, in_=in_, **kw)
        self.cnt[key] += 16
        inst.then_inc(self.sems[key], 16)
        self._mark(key, self.cnt[key], r, w)
        return inst

    def gather(self, stream, out, in_full, idx_ap, r=(), w=()):
        key = "d:" + stream
        if key not in self.sems:
            self._newsem(key)
        self._wait("pool", self._deps(r, w))
        inst = self.nc.gpsimd.indirect_dma_start(out=out, out_offset=None, in_=in_full, in_offset=bass.IndirectOffsetOnAxis(idx_ap, 0))
        self.cnt[key] += 16
        inst.then_inc(self.sems[key], 16)
        self._mark(key, self.cnt[key], r, w)
        return inst

    def barrier(self):
        for en in self.eng:
            self._wait(en, dict(self.cnt))

    def finish(self):
        self._wait("sp", dict(self.cnt))


def build(T, L=2, debug=False, stop=None):
    assert T % 512 == 0
    NB = T // 512
    NS = T // 128
    TBC = min(1024, T)
    NSC = TBC // 128
    nc = bass.Bass("TRN2", target_bir_lowering=False)
    kb = KB(nc)

    def ext_in(name, shape):
        return Buf(nc.dram_tensor(name, list(shape), F32, kind="ExternalInput"), nowaw=True)

    xin = ext_in("x", [T, D])
    consts_d = ext_in("consts", [128, NCONST])
    params_d = ext_in("params", [L, 128, NPAR])
    w_in_d = ext_in("w_in", [L, D, DIN])
    w_out_d = ext_in("w_out", [L, D, D])
    ffn_g_d = ext_in("ffn_w_gate", [1, D, DFF])
    ffn_u_d = ext_in("ffn_w_up", [1, D, DFF])
    ffn_d_d = ext_in("ffn_w_down", [1, DFF, D])
    wr_d = ext_in("moe_router_w", [1, D, NE])
    moe_g_d = ext_in("moe_w_gate", [1, NE, D, DFF])
    moe_u_d = ext_in("moe_w_up", [1, NE, D, DFF])
    moe_d_d = ext_in("moe_w_down", [1, NE, DFF, D])
    TH = T // 2
    yout = Buf(nc.dram_tensor("y", [TH, D], F32, kind="ExternalOutput"), nowaw=True)
    tokidx_d = Buf(nc.dram_tensor("tokidx", [128, TH // 128], I32, kind="ExternalInput"), nowaw=True)

    dbg = {}

    def scratch(name, shape, dt):
        if debug:
            b = Buf(nc.dram_tensor(name, list(shape), dt, kind="ExternalOutput"), nowaw=True)
            dbg[name] = b
            return b
        return kb.dram(name, shape, dt)

    qT_d = scratch("qT_s", [512, T], BF16)
    kT_d = scratch("kT_s", [512, T], BF16)
    v_d = scratch("v_s", [T, 512], BF16)
    yT_d = scratch("yT_s", [1024, T], BF16)
    xmid_d = scratch("xmid_s", [T, D], F32)
    x1_d = kb.dram("x1_s", [T, D], F32)
    h2_d = kb.dram("h2_s", [T, D], BF16)

    cst = kb.sbuf("cst", [128, NCONST], F32)
    par = kb.sbuf("par", [128, L, NPAR], F32)
    identb = kb.sbuf("identb", [128, 128], BF16)
    blkb = kb.sbuf("blkb", [128, 128], BF16)
    onesb = kb.sbuf("onesb", [128, 128], BF16)
    dmaskb = kb.sbuf("dmaskb", [128, 4, 512], BF16)
    cmaskb = kb.sbuf("cmaskb", [128, 64], F32)
    epsc = kb.sbuf("epsc", [128, 1], F32)
    onec = kb.sbuf("onec", [128, 1], F32)
    lsig = kb.sbuf("lsig", [128, NS, 8], F32)
    negd = kb.sbuf("negd", [128, NS, 8], F32)
    cI = kb.sbuf("cI", [128, NS, 8], F32)
    lbt = kb.sbuf("lbt", [128, 2], F32)
    omlt = kb.sbuf("omlt", [128, 2], F32)
    nomlt = kb.sbuf("nomlt", [128, 2], F32)
    bshift = kb.sbuf("bshift", [128, 1], F32)
    small = kb.sbuf("small", [128, 64], F32)
    idxt = kb.sbuf("idxt", [128, TH // 128], I32)

    ps = [kb.psum("ps%d" % i, [128, 512], F32) for i in range(8)]

    kb.dma("sp", "ld", cst[:, :], consts_d[:, :], w=[cst])
    kb.dma("sp", "ld", idxt[:, :], tokidx_d[:, :], w=[idxt])
    kb.dma("sp", "ld", par[:, :, :], params_d.t.ap().rearrange("l p n -> p l n"), w=[par])
    kb.op("dve", lambda e: e.tensor_copy(out=identb[:, :], in_=cst[:, K_ID:K_ID + 128]), r=[cst], w=[identb])
    kb.op("dve", lambda e: e.tensor_copy(out=blkb[:, :], in_=cst[:, K_BLK:K_BLK + 128]), r=[cst], w=[blkb])
    kb.op("dve", lambda e: e.tensor_copy(out=onesb[:, :], in_=cst[:, K_ONES:K_ONES + 128]), r=[cst], w=[onesb])
    kb.op("dve", lambda e: e.tensor_copy(out=dmaskb[:, :, :], in_=cst[:, K_DM:K_DM + 2048].rearrange("p (j q) -> p j q", q=512)), r=[cst], w=[dmaskb])
    kb.op("dve", lambda e: e.tensor_copy(out=cmaskb[:, :], in_=cst[:, K_CM:K_CM + 64]), r=[cst], w=[cmaskb])
    kb.op("dve", lambda e: e.memset(epsc[:, :], EPS), w=[epsc])
    kb.op("dve", lambda e: e.memset(onec[:, :], 1.0), w=[onec])

    def rstd_from_ms(ms_ap, ms_bufs, out_b, out_ap, tmp_b, tmp_ap):
        kb.op("act", lambda e: e.activation(out=tmp_ap, in_=ms_ap, func=AF.Ln, bias=epsc[:, 0:1], scale=1.0), r=ms_bufs + [epsc], w=[tmp_b])
        kb.op("act", lambda e: e.activation(out=out_ap, in_=tmp_ap, func=AF.Exp, scale=-0.5), r=[tmp_b], w=[out_b])

    for l in range(L):
        x_src = xin if l == 0 else xmid_d
        x_dst = xmid_d if l == 0 else yout
        pl = lambda c0, n=1: par[:, l, c0:c0 + n]

        stackA = ExitStack()
        kb.stack = stackA
        win = kb.sbuf("win%d" % l, [128, 8, DIN], BF16)
        for c in range(8):
            kb.dma("pool", "w", win[:, c, :], w_in_d[l, c * 128:(c + 1) * 128, :], w=[win])
        xt = kb.sbuf("xt%d" % l, [128, 4, D], F32)
        hb = kb.sbuf("hb%d" % l, [128, 4, D], BF16)
        hT = kb.sbuf("hT%d" % l, [128, 8, 512], BF16)
        junk = kb.sbuf("junk%d" % l, [128, D], F32)
        st4 = kb.sbuf("st4%d" % l, [128, 8], F32)
        fa = [kb.sbuf("fa%d_%d" % (l, i), [128, 512], F32) for i in range(6)]
        fb = [kb.sbuf("fb%d_%d" % (l, i), [128, 512], BF16) for i in range(3)]
        ob = [kb.sbuf("ob%d_%d" % (l, i), [128, 512], BF16) for i in range(2)]
        vb = kb.sbuf("vb%d" % l, [128, 4, 512], BF16)
        vi = kb.sbuf("vi%d" % l, [128, 4, 256], BF16)
        ucv = [kb.sbuf("ucv%d_%d" % (l, j), [128, 514], F32) for j in range(2)]
        sig = kb.sbuf("sig%d" % l, [128, 512], F32)
        kk = kb.sbuf("kk%d" % l, [128, 512], F32)
        cc = kb.sbuf("cc%d" % l, [128, 512], F32)
        qs = kb.sbuf("qs%d" % l, [128, 512], F32)
        gs = [kb.sbuf("gs%d_%d" % (l, j), [128, 512], F32) for j in range(2)]
        qe = [kb.sbuf("qe%d_%d" % (l, j), [128, 512], BF16) for j in range(2)]
        ke = [kb.sbuf("ke%d_%d" % (l, j), [128, 512], BF16) for j in range(2)]
        qec = [kb.sbuf("qec%d_%d" % (l, j), [128, 512], BF16) for j in range(2)]
        kdT = kb.sbuf("kdT%d" % l, [128, 512], BF16)
        kd = kb.sbuf("kd%d" % l, [128, 4, 256], BF16)
        ecl = kb.sbuf("ecl%d" % l, [128, 2, 8], F32)
        state = kb.sbuf("state%d" % l, [128, 2, 64], F32)
        stateb = kb.sbuf("stateb%d" % l, [128, 2, 64], BF16)
        atsb = kb.sbuf("atsb%d" % l, [128, 128], BF16)
        osb = kb.sbuf("osb%d" % l, [128, 2, 512], F32)
        vi2 = kb.sbuf("vi2_%d" % l, [128, 8, 256], BF16)
        kd2 = kb.sbuf("kd2_%d" % l, [128, 8, 256], BF16)
        segm = kb.sbuf("segm%d" % l, [128, 512], F32)

        kb.op("dve", lambda e: e.tensor_copy(out=segm[:, :], in_=cst[:, K_SEG:K_SEG + 512]), r=[cst], w=[segm])
        kb.op("dve", lambda e: e.memset(state[:, :, :], 0.0), w=[state])
        kb.op("dve", lambda e: e.memset(stateb[:, :, :], 0.0), w=[stateb])
        for j in range(2):
            kb.op("dve", lambda e: e.memset(ucv[j][:, :], 0.0), w=[ucv[j]])
        if l == 0:
            kb.op("dve", lambda e: e.memset(lbt[:, :], 0.0), w=[lbt])
        else:
            kb.op("dve", lambda e: e.tensor_tensor(out=small[:, 0:2], in0=pl(P_LB + 2, 2), in1=pl(P_LB, 2), op=ALU.subtract), r=[par], w=[small])
            kb.op("act", lambda e: e.activation(out=lbt[:, :], in_=small[:, 0:2], func=AF.Sigmoid), r=[small], w=[lbt])
        kb.op("dve", lambda e: e.tensor_scalar(out=omlt[:, :], in0=lbt[:, :], scalar1=-1.0, scalar2=1.0, op0=ALU.mult, op1=ALU.add), r=[lbt], w=[omlt])
        kb.op("dve", lambda e: e.tensor_scalar(out=nomlt[:, :], in0=omlt[:, :], scalar1=-1.0, scalar2=None, op0=ALU.mult), r=[omlt], w=[nomlt])
        kb.op("dve", lambda e: e.tensor_reduce(out=small[:, 8:9], in_=pl(P_GQR, 64), axis=mybir.AxisListType.X, op=ALU.max, apply_absolute_value=True), r=[par], w=[small])
        kb.op("dve", lambda e: e.tensor_reduce(out=small[:, 9:10], in_=pl(P_GKR, 64), axis=mybir.AxisListType.X, op=ALU.max, apply_absolute_value=True), r=[par, small], w=[small])
        kb.op("dve", lambda e: e.scalar_tensor_tensor(out=bshift[:, :], in0=small[:, 8:9], scalar=8.0, in1=small[:, 9:10], op0=ALU.mult, op1=ALU.mult), r=[small], w=[bshift])

        rot = {"fm": 0, "tm": 0, "fa": 0, "fb": 0, "ob": 0}

        def nxt(name, n):
            v = rot[name]
            rot[name] = (v + 1) % n
            return v

        def fm_chunk(col0):
            p = ps[1 + nxt("fm", 2)]
            for c in range(8):
                kb.op("pe", lambda e: e.matmul(p[:, :], lhsT=win[:, c, col0:col0 + 128], rhs=hT[:, c, :], start=(c == 0), stop=(c == 7)),
                      r=[win, hT], w=[p], inc=(c == 7))
            return p

        def headnorm_store(src_ap, src_bufs, gain_ap, dst_d, row0, tok0, mul_b=None):
            sq = fb[nxt("fb", 3)]
            kb.op("act", lambda e: e.activation(out=sq[:, :], in_=src_ap, func=AF.Square), r=src_bufs, w=[sq])
            pm = ps[5]
            kb.op("pe", lambda e: e.matmul(pm[:, :], lhsT=blkb[:, :], rhs=sq[:, :], start=True, stop=True), r=[blkb, sq], w=[pm])
            t1 = fa[nxt("fa", 6)]
            rs = fa[nxt("fa", 6)]
            rstd_from_ms(pm[:, :], [pm], rs, rs[:, :], t1, t1[:, :])
            o = ob[nxt("ob", 2)]
            if mul_b is None:
                kb.op("dve", lambda e: e.scalar_tensor_tensor(out=o[:, :], in0=src_ap, scalar=gain_ap, in1=rs[:, :], op0=ALU.mult, op1=ALU.mult),
                      r=src_bufs + [rs, par], w=[o])
            else:
                kb.op("dve", lambda e: e.scalar_tensor_tensor(out=t1[:, :], in0=src_ap, scalar=gain_ap, in1=rs[:, :], op0=ALU.mult, op1=ALU.mult),
                      r=src_bufs + [rs, par], w=[t1])
                kb.op("dve", lambda e: e.tensor_tensor(out=o[:, :], in0=t1[:, :], in1=mul_b[:, :], op=ALU.mult), r=[t1, mul_b], w=[o])
            kb.dma("sp", "st", dst_d[row0:row0 + 128, tok0:tok0 + 512], o[:, :], r=[o], w=[dst_d])

        for tb in range(NB):
            tok0 = tb * 512
            kb.dma("sp", "ld", xt[:, :, :], x_src[tok0:tok0 + 512, :].rearrange("(s p) d -> p s d", p=128), r=[x_src], w=[xt])
            for s in range(4):
                kb.op("act", lambda e: e.activation(out=junk[:, :], in_=xt[:, s, :], func=AF.Square, accum_out=st4[:, s:s + 1]), r=[xt], w=[junk, st4])
            rstd_tm(kb, st4, epsc, 4)
            for s in range(4):
                kb.op("act", lambda e: e.activation(out=hb[:, s, :], in_=xt[:, s, :], func=AF.Identity, scale=st4[:, 4 + s:5 + s]), r=[xt, st4], w=[hb])
            pT = ps[0]
            pTb = pT.t.ap().bitcast(BF16)
            for c in range(8):
                half = (c % 2) * 512
                for s in range(4):
                    kb.op("pe", lambda e: e.transpose(pTb[:, half + s * 128:half + (s + 1) * 128], hb[:, s, c * 128:(c + 1) * 128], identb[:, :]),
                          r=[hb, identb], w=[pT], inc=(s == 3))
                en = "dve" if c % 2 == 0 else "act"
                if en == "dve":
                    kb.op("dve", lambda e: e.tensor_scalar(out=hT[:, c, :], in0=pTb[:, half:half + 512], scalar1=pl(P_G1 + c), scalar2=None, op0=ALU.mult), r=[pT, par], w=[hT])
                else:
                    kb.op("act", lambda e: e.activation(out=hT[:, c, :], in_=pTb[:, half:half + 512], func=AF.Identity, scale=pl(P_G1 + c)), r=[pT, par], w=[hT])

            for which, col0, dst, gcol in (("q", C_Q, qT_d, P_GQ), ("k", C_K, kT_d, P_GK)):
                for c in range(4):
                    p = fm_chunk(col0 + c * 128)
                    headnorm_store(p[:, :], [p], pl(gcol), dst, c * 128, tok0)

            for s in range(4):
                p = ps[3 + nxt("tm", 2)]
                for c in range(8):
                    kb.op("pe", lambda e: e.matmul(p[:, :], lhsT=hT[:, c, s * 128:(s + 1) * 128], rhs=win[:, c, C_V:C_V + 512], start=(c == 0), stop=(c == 7)),
                          r=[win, hT], w=[p], inc=(c == 7))
                kb.op("act", lambda e: e.activation(out=vb[:, s, :], in_=p[:, :], func=AF.Copy), r=[p], w=[vb])
                p2 = ps[3 + nxt("tm", 2)]
                for c in range(8):
                    kb.op("pe", lambda e: e.matmul(p2[:, 0:256], lhsT=hT[:, c, s * 128:(s + 1) * 128], rhs=win[:, c, C_RI:C_RI + 256], start=(c == 0), stop=(c == 7)),
                          r=[win, hT], w=[p2], inc=False)
                for c in range(8):
                    kb.op("pe", lambda e: e.matmul(p2[:, 256:264], lhsT=hT[:, c, s * 128:(s + 1) * 128], rhs=win[:, c, C_F:C_F + 8], start=(c == 0), stop=(c == 7)),
                          r=[win, hT], w=[p2], inc=(c == 7))
                kb.op("act", lambda e: e.activation(out=vi[:, s, :], in_=p2[:, 0:256], func=AF.Copy), r=[p2], w=[vi])
                blk = tb * 4 + s
                kb.op("dve", lambda e: e.tensor_tensor(out=small[:, 16:24], in0=p2[:, 256:264], in1=pl(P_FB, 8), op=ALU.add), r=[p2, par, vi], w=[small])
                kb.op("act", lambda e: e.activation(out=small[:, 24:32], in_=small[:, 16:24], func=AF.Abs), r=[small], w=[small])
                kb.op("act", lambda e: e.activation(out=small[:, 32:40], in_=small[:, 24:32], func=AF.Exp, scale=-1.0), r=[small], w=[small])
                kb.op("act", lambda e: e.activation(out=small[:, 40:48], in_=small[:, 32:40], func=AF.Ln, bias=onec[:, 0:1], scale=1.0), r=[small, onec], w=[small])
                kb.op("dve", lambda e: e.tensor_single_scalar(out=small[:, 48:56], in_=small[:, 16:24], scalar=0.0, op=ALU.min), r=[small], w=[small])
                kb.op("dve", lambda e: e.tensor_tensor(out=lsig[:, blk, :], in0=small[:, 48:56], in1=small[:, 40:48], op=ALU.subtract), r=[small], w=[lsig])
            kb.dma("sp", "st", v_d[tok0:tok0 + 512, :].rearrange("(s p) f -> p s f", p=128), vb[:, :, :], r=[vb], w=[v_d])

            for j in range(2):
                px = fm_chunk(C_CX + j * 128)
                t0 = fa[nxt("fa", 6)]
                kb.op("act", lambda e: e.activation(out=t0[:, :], in_=px[:, :], func=AF.Copy), r=[px], w=[t0])
                pc = fm_chunk(C_CC + j * 128)
                u = ucv[j]
                kb.op("dve", lambda e: e.tensor_tensor(out=u[:, 2:514], in0=t0[:, :], in1=pc[:, :], op=ALU.mult), r=[t0, pc], w=[u])
                t1 = fa[nxt("fa", 6)]
                kb.op("dve", lambda e: e.tensor_scalar(out=t1[:, :], in0=u[:, 0:512], scalar1=pl(P_CW + 0 * 2 + j), scalar2=None, op0=ALU.mult), r=[u, par], w=[t1])
                kb.op("dve", lambda e: e.scalar_tensor_tensor(out=t1[:, :], in0=u[:, 1:513], scalar=pl(P_CW + 1 * 2 + j), in1=t1[:, :], op0=ALU.mult, op1=ALU.add), r=[u, par, t1], w=[t1])
                kb.op("dve", lambda e: e.scalar_tensor_tensor(out=t1[:, :], in0=u[:, 2:514], scalar=pl(P_CW + 2 * 2 + j), in1=t1[:, :], op0=ALU.mult, op1=ALU.add), r=[u, par, t1], w=[t1])
                pbg = fm_chunk(C_CB + j * 128)
                kb.op("dve", lambda e: e.tensor_tensor(out=t0[:, :], in0=t1[:, :], in1=pbg[:, :], op=ALU.mult), r=[t1, pbg], w=[t0])
                kb.op("dve", lambda e: e.tensor_copy(out=small[:, 56:58], in_=u[:, 512:514]), r=[u], w=[small])
                kb.op("dve", lambda e: e.tensor_copy(out=u[:, 0:2], in_=small[:, 56:58]), r=[small], w=[u])
                headnorm_store(t0[:, :], [t0], pl(P_MG + 4 + j), yT_d, 512 + j * 128, tok0)

            for j in range(2):
                pf = fm_chunk(C_RF + j * 128)
                kb.op("act", lambda e: e.activation(out=sig[:, :], in_=pf[:, :], func=AF.Sigmoid), r=[pf], w=[sig])
                tf = fa[nxt("fa", 6)]
                kb.op("dve", lambda e: e.tensor_scalar(out=tf[:, :], in0=sig[:, :], scalar1=omlt[:, j:j + 1], scalar2=lbt[:, j:j + 1], op0=ALU.mult, op1=ALU.add), r=[sig, omlt, lbt], w=[tf])
                kb.op("dve", lambda e: e.tensor_single_scalar(out=tf[:, :], in_=tf[:, :], scalar=TINY, op=ALU.max), r=[tf], w=[tf])
                kb.op("act", lambda e: e.activation(out=tf[:, :], in_=tf[:, :], func=AF.Ln), r=[tf], w=[tf])
                kb.op("dve", lambda e: e.tensor_scalar(out=kk[:, :], in0=sig[:, :], scalar1=nomlt[:, j:j + 1], scalar2=omlt[:, j:j + 1], op0=ALU.mult, op1=ALU.add), r=[sig, omlt, nomlt], w=[kk])
                kb.op("dve", lambda e: e.tensor_tensor_scan(out=cc[:, :], data0=segm[:, :], data1=tf[:, :], initial=0.0, op0=ALU.mult, op1=ALU.add), r=[segm, tf], w=[cc])
                c3 = cc.t.ap().rearrange("p (n t) -> p n t", t=64)
                pq = fm_chunk(C_RQ + j * 128)
                kb.op("act", lambda e: e.activation(out=qs[:, :], in_=pq[:, :], func=AF.Silu), r=[pq], w=[qs])
                pg = fm_chunk(C_RG + j * 128)
                kb.op("act", lambda e: e.activation(out=gs[j][:, :], in_=pg[:, :], func=AF.Silu), r=[pg], w=[gs[j]])
                d1 = fa[nxt("fa", 6)]
                kb.op("dve", lambda e: e.tensor_tensor(out=d1.t.ap().rearrange("p (n t) -> p n t", t=64), in0=c3, in1=c3[:, :, 31:32].to_broadcast([128, 8, 64]), op=ALU.subtract), r=[cc], w=[d1])
                ex = fa[nxt("fa", 6)]
                kb.op("act", lambda e: e.activation(out=ex[:, :], in_=d1[:, :], func=AF.Exp), r=[d1], w=[ex])
                kb.op("dve", lambda e: e.tensor_tensor(out=qe[j][:, :], in0=qs[:, :], in1=ex[:, :], op=ALU.mult), r=[qs, ex], w=[qe[j]])
                kb.op("act", lambda e: e.activation(out=ex[:, :], in_=d1[:, :], func=AF.Exp, scale=-1.0), r=[d1], w=[ex])
                kb.op("dve", lambda e: e.tensor_tensor(out=ke[j][:, :], in0=kk[:, :], in1=ex[:, :], op=ALU.mult), r=[kk, ex], w=[ke[j]])
                kb.op("act", lambda e: e.activation(out=ex[:, :], in_=cc[:, :], func=AF.Exp), r=[cc], w=[ex])
                kb.op("dve", lambda e: e.tensor_tensor(out=qec[j][:, :], in0=qs[:, :], in1=ex[:, :], op=ALU.mult), r=[qs, ex], w=[qec[j]])
                kb.op("dve", lambda e: e.tensor_tensor(out=d1.t.ap().rearrange("p (n t) -> p n t", t=64), in0=c3[:, :, 63:64].to_broadcast([128, 8, 64]), in1=c3, op=ALU.subtract), r=[cc], w=[d1])
                kb.op("act", lambda e: e.activation(out=ex[:, :], in_=d1[:, :], func=AF.Exp), r=[d1], w=[ex])
                kb.op("dve", lambda e: e.tensor_tensor(out=kdT[:, :], in0=kk[:, :], in1=ex[:, :], op=ALU.mult), r=[kk, ex], w=[kdT])
                kb.op("act", lambda e: e.activation(out=ecl[:, j, :], in_=c3[:, :, 63], func=AF.Exp), r=[cc], w=[ecl])
                pT = ps[0]
                pTb = pT.t.ap().bitcast(BF16)
                for s in range(4):
                    kb.op("pe", lambda e: e.transpose(pTb[:, s * 128:(s + 1) * 128], kdT[:, s * 128:(s + 1) * 128], identb[:, :]), r=[kdT, identb], w=[pT], inc=(s == 3))
                kb.op("dve", lambda e: e.tensor_copy(out=kd[:, :, j * 128:(j + 1) * 128], in_=pTb[:, 0:512].rearrange("p (s f) -> p s f", f=128)), r=[pT], w=[kd])

            for pr in range(2):
                for dst in range(2):
                    kb.dma("sp", "cp", vi2.t.ap().rearrange("p (s two) f -> p s two f", two=2)[dst * 64:(dst + 1) * 64, :, pr, :], vi[pr * 64:(pr + 1) * 64, :, :], r=[vi], w=[vi2])
                    kb.dma("sp", "cp", kd2.t.ap().rearrange("p (s two) f -> p s two f", two=2)[dst * 64:(dst + 1) * 64, :, pr, :], kd[pr * 64:(pr + 1) * 64, :, :], r=[kd], w=[kd2])
            for ch in range(8):
                cols = slice(ch * 64, (ch + 1) * 64)
                for hh in range(2):
                    pb = hh * 64
                    ph = ps[6 + hh]
                    for j in range(2):
                        kb.op("pe", lambda e: e.matmul(ph[pb:pb + 64, j * 64:(j + 1) * 64], lhsT=ke[j][pb:pb + 64, cols], rhs=qe[j][pb:pb + 64, cols], start=True, stop=True, tile_position=(pb, pb)),
                              r=[ke[j], qe[j]], w=[ph], inc=(j == 1))
                for hh in range(2):
                    pb = hh * 64
                    ph = ps[6 + hh]
                    kb.op("dve", lambda e: e.tensor_tensor(out=atsb[pb:pb + 64, 0:128].rearrange("p (h t) -> p h t", t=64), in0=ph[pb:pb + 64, 0:128].rearrange("p (h t) -> p h t", t=64),
                                                           in1=cmaskb[pb:pb + 64, :].unsqueeze(1).to_broadcast([64, 2, 64]), op=ALU.mult), r=[ph, cmaskb], w=[atsb])
                for hh in range(2):
                    pb = hh * 64
                    ph = ps[6 + hh]
                    for j in range(2):
                        hd = j * 2 + hh
                        oslc = ph[pb:pb + 64, 128 + j * 64:128 + (j + 1) * 64]
                        kb.op("pe", lambda e: e.matmul(oslc, lhsT=vi2[pb:pb + 64, ch, hd * 64:(hd + 1) * 64], rhs=atsb[pb:pb + 64, j * 64:(j + 1) * 64], start=True, stop=False, tile_position=(pb, pb)),
                              r=[vi2, atsb], w=[ph], inc=False)
                        kb.op("pe", lambda e: e.matmul(oslc, lhsT=stateb[pb:pb + 64, j, :], rhs=qec[j][pb:pb + 64, cols], start=False, stop=True, tile_position=(pb, pb)),
                              r=[stateb, qec[j]], w=[ph], inc=False)
                        kb.op("pe", lambda e: e.matmul(ph[pb:pb + 64, 256 + j * 64:256 + (j + 1) * 64], lhsT=kd2[pb:pb + 64, ch, hd * 64:(hd + 1) * 64], rhs=vi2[pb:pb + 64, ch, hd * 64:(hd + 1) * 64], start=True, stop=True, tile_position=(pb, pb)),
                              r=[kd2, vi2], w=[ph], inc=(j == 1))
                kb.op("dve", lambda e: e.tensor_tensor(out=state[:, :, :], in0=state[:, :, :], in1=ecl[:, :, ch:ch + 1].to_broadcast([128, 2, 64]), op=ALU.mult), r=[state, ecl], w=[state])
                for hh in range(2):
                    pb = hh * 64
                    ph = ps[6 + hh]
                    kb.op("act", lambda e: e.activation(out=osb[pb:pb + 64, :, cols], in_=ph[pb:pb + 64, 128:256].rearrange("p (j t) -> p j t", t=64), func=AF.Copy), r=[ph], w=[osb])
                    kb.op("dve", lambda e: e.tensor_tensor(out=state[pb:pb + 64, :, :], in0=state[pb:pb + 64, :, :], in1=ph[pb:pb + 64, 256:384].rearrange("p (j v) -> p j v", v=64), op=ALU.add), r=[state, ph, osb], w=[state])
                kb.op("dve", lambda e: e.tensor_copy(out=stateb[:, :, :], in_=state[:, :, :]), r=[state], w=[stateb])
            for j in range(2):
                headnorm_store(osb[:, j, :], [osb], pl(P_MG + 6 + j), yT_d, 768 + j * 128, tok0, mul_b=gs[j])

        lflat = lsig.t.ap().rearrange("p n h -> p (n h)")
        NW = NS * 8
        tri = cst[:, K_TRI:K_TRI + 128]
        onesf = cst[:, K_ONES:K_ONES + 128]
        sel = cst[:, K_SEL:K_SEL + 128]
        pcs, ptot = ps[1], ps[2]
        kb.op("pe", lambda e: e.matmul(pcs[:, 0:NW], lhsT=tri, rhs=lflat, start=True, stop=True), r=[cst, lsig], w=[pcs])
        kb.op("pe", lambda e: e.matmul(ptot[:, 0:NW], lhsT=onesf, rhs=lflat, start=True, stop=True), r=[cst, lsig], w=[ptot])
        tot = fa[0]
        kb.op("act", lambda e: e.activation(out=tot[:, 0:NW], in_=ptot[:, 0:NW], func=AF.Copy), r=[ptot], w=[tot])
        kb.op("dve", lambda e: e.memset(cI[:, 0, :], 0.0), w=[cI])
        for b in range(1, NS):
            kb.op("dve", lambda e: e.tensor_tensor(out=cI[:, b, :], in0=cI[:, b - 1, :], in1=tot[:, (b - 1) * 8:b * 8], op=ALU.add), r=[cI, tot], w=[cI])
        dcum = fa[1]
        kb.op("dve", lambda e: e.tensor_tensor(out=dcum[:, 0:NW], in0=pcs[:, 0:NW], in1=cI.t.ap().rearrange("p n h -> p (n h)"), op=ALU.add), r=[pcs, cI], w=[dcum])
        kb.op("dve", lambda e: e.tensor_scalar(out=negd.t.ap().rearrange("p n h -> p (n h)"), in0=dcum[:, 0:NW], scalar1=-1.0, scalar2=None, op0=ALU.mult), r=[dcum], w=[negd])
        pbc = ps[3]
        kb.op("pe", lambda e: e.matmul(pbc[:, 0:NW], lhsT=sel, rhs=dcum[:, 0:NW], start=True, stop=True), r=[cst, dcum], w=[pbc])
        kb.op("dve", lambda e: e.tensor_scalar(out=cI.t.ap().rearrange("p n h -> p (n h)"), in0=pbc[:, 0:NW], scalar1=bshift[:, 0:1], scalar2=None, op0=ALU.subtract), r=[pbc, bshift], w=[cI])

        kb.barrier()
        if stop == ("A", l):
            kb.finish()
            return nc, dbg
        stackA.close()
        stackB = ExitStack()
        kb.stack = stackB
        obB = [kb.sbuf("obB%d_%d" % (l, i), [128, 512], BF16) for i in range(2)]
        kTc = kb.sbuf("kTc%d" % l, [128, T], BF16)
        qTc = kb.sbuf("qTc%d" % l, [128, T], BF16)
        vh = kb.sbuf("vh%d" % l, [128, NS, 128], BF16)
        pTt = [kb.sbuf("pTt%d_%d" % (l, i), [128, 512], BF16) for i in range(3)]
        biasb = [kb.sbuf("biasb%d_%d" % (l, i), [128, NS], F32) for i in range(2)]
        rden = kb.sbuf("rden%d" % l, [128, 512], F32)
        on = kb.sbuf("on%d" % l, [128, 512], F32)
        rotb = {"s": 0, "p": 0, "b": 0, "o": 0, "ob": 0}

        def nxb(name, n):
            v = rotb[name]
            rotb[name] = (v + 1) % n
            return v

        for c in range(4):
            kb.dma("sp", "ld", kTc[:, :], kT_d[c * 128:(c + 1) * 128, :], r=[kT_d], w=[kTc])
            kb.dma("sp", "ld", qTc[:, :], qT_d[c * 128:(c + 1) * 128, :], r=[qT_d], w=[qTc])
            kb.dma("sp", "ld", vh[:, :, :], v_d[:, c * 128:(c + 1) * 128].rearrange("(n p) f -> p n f", p=128), r=[v_d], w=[vh])
            for I in range(NB):
                oi = nxb("o", 2)
                pO, pD = ps[3 + oi], ps[5 + oi]
                nJ = 4 * (I + 1)
                for hh in range(2):
                    pb = hh * 64
                    h = 2 * c + hh
                    bb = biasb[nxb("b", 2)]
                    kb.op("dve", lambda e: e.tensor_scalar(out=bb[:, 0:nJ], in0=negd[:, 0:nJ, h], scalar1=cI[:, 4 * I + 1, h:h + 1], scalar2=None, op0=ALU.add), r=[negd, cI], w=[bb])
                    for J in range(nJ):
                        pS = ps[nxb("s", 3)]
                        kb.op("pe", lambda e: e.matmul(pS[:, :], lhsT=kTc[pb:pb + 64, J * 128:(J + 1) * 128], rhs=qTc[pb:pb + 64, I * 512:(I + 1) * 512], start=True, stop=True, tile_position=(pb, 0)),
                              r=[kTc, qTc], w=[pS])
                        pt = pTt[nxb("p", 3)]
                        kb.op("act", lambda e: e.activation(out=pt[:, :], in_=pS[:, :], func=AF.Exp, bias=bb[:, J:J + 1], scale=0.125), r=[pS, bb], w=[pt])
                        if J >= 4 * I:
                            kb.op("dve", lambda e: e.tensor_tensor(out=pt[:, :], in0=pt[:, :], in1=dmaskb[:, J - 4 * I, :], op=ALU.mult), r=[pt, dmaskb], w=[pt])
                        kb.op("pe", lambda e: e.matmul(pO[pb:pb + 64, :], lhsT=vh[:, J, pb:pb + 64], rhs=pt[:, :], start=(J == 0), stop=(J == nJ - 1), tile_position=(0, pb)), r=[vh, pt], w=[pO], inc=False)
                        kb.op("pe", lambda e: e.matmul(pD[pb:pb + 64, :], lhsT=onesb[:, 0:64], rhs=pt[:, :], start=(J == 0), stop=(J == nJ - 1), tile_position=(0, pb)), r=[onesb, pt], w=[pD])
                kb.op("dve", lambda e: e.reciprocal(out=rden[:, :], in_=pD[:, :]), r=[pD], w=[rden])
                kb.op("dve", lambda e: e.tensor_tensor(out=on[:, :], in0=pO[:, :], in1=rden[:, :], op=ALU.mult), r=[pO, rden], w=[on])
                sq = pTt[nxb("p", 3)]
                kb.op("act", lambda e: e.activation(out=sq[:, :], in_=on[:, :], func=AF.Square), r=[on], w=[sq])
                pm = ps[7]
                kb.op("pe", lambda e: e.matmul(pm[:, :], lhsT=blkb[:, :], rhs=sq[:, :], start=True, stop=True), r=[blkb, sq], w=[pm])
                rstd_from_ms(pm[:, :], [pm], rden, rden[:, :], rden, rden[:, :])
                o = obB[nxb("ob", 2)]
                kb.op("dve", lambda e: e.scalar_tensor_tensor(out=o[:, :], in0=on[:, :], scalar=pl(P_MG + c), in1=rden[:, :], op0=ALU.mult, op1=ALU.mult), r=[on, rden, par], w=[o])
                kb.dma("sp", "st", yT_d[c * 128:(c + 1) * 128, I * 512:(I + 1) * 512], o[:, :], r=[o], w=[yT_d])

        kb.barrier()
        if stop == ("B", l):
            kb.finish()
            return nc, dbg
        stackB.close()
        stackC = ExitStack()
        kb.stack = stackC
        moe = (l % 2 == 1)
        junk = kb.sbuf("junkC%d" % l, [128, D], F32)
        h2b = [kb.sbuf("h2b%d_%d" % (l, i), [128, D], BF16) for i in range(2)]
        stc = kb.sbuf("stc%d" % l, [128, 2 * NSC], F32)
        wgb = [kb.sbuf("wgb%d_%d" % (l, i), [128, 8, 512], BF16) for i in range(2)]
        wub = [kb.sbuf("wub%d_%d" % (l, i), [128, 8, 512], BF16) for i in range(2)]
        wdb = [kb.sbuf("wdb%d_%d" % (l, i), [128, 4, D], BF16) for i in range(2)]
        sgt = [kb.sbuf("sgt%d_%d" % (l, i), [128, 512], BF16) for i in range(2)]
        if moe:
            wrb = kb.sbuf("wrb%d" % l, [128, 8, NE], BF16)
            kb.dma("pool", "w", wrb[:, :, :], wr_d[0].rearrange("(c p) e -> p c e", p=128), w=[wrb])
            rt = kb.sbuf("rt%d" % l, [128, 64], F32)
            trib = kb.sbuf("trib%d" % l, [128, 128], BF16)
            kb.op("dve", lambda e: e.tensor_copy(out=trib[:, :], in_=cst[:, K_TRI:K_TRI + 128]), r=[cst], w=[trib])
        stackC1 = ExitStack()
        kb.stack = stackC1
        wout = kb.sbuf("wout%d" % l, [128, 8, D], BF16)
        for c in range(8):
            kb.dma("pool", "w", wout[:, c, :], w_out_d[l, c * 128:(c + 1) * 128, :], w=[wout])
        x1 = kb.sbuf("x1_%d" % l, [128, NSC, D], F32)
        yTb = kb.sbuf("yTb%d" % l, [128, 8, TBC], BF16)
        h2T = yTb
        hact = kb.sbuf("hact%d" % l, [128, 4, TBC], BF16)
        rc = {"a": 0, "g": 0, "u": 0, "w": 0, "h": 0, "sg": 0}

        def nxc(name, n):
            v = rc[name]
            rc[name] = (v + 1) % n
            return v

        passes = [("full", T // TBC)] if not moe else [("c1", T // TBC)]
        for mode, tbc in [(m_, t_) for m_, n_ in passes for t_ in range(n_)]:
            tok0 = tbc * TBC
            if mode != "c2":
                kb.dma("sp", "ld", x1[:, :, :], x_src[tok0:tok0 + TBC, :].rearrange("(s p) d -> p s d", p=128), r=[x_src], w=[x1])
                kb.dma("sp", "ld", yTb[:, :, :], yT_d[:, tok0:tok0 + TBC].rearrange("(c p) t -> p c t", p=128), r=[yT_d], w=[yTb])
            for s in (range(NSC) if mode != "c2" else []):
                for half in range(2):
                    p = ps[1 + nxc("a", 2)]
                    for c in range(8):
                        kb.op("pe", lambda e: e.matmul(p[:, :], lhsT=yTb[:, c, s * 128:(s + 1) * 128], rhs=wout[:, c, half * 512:(half + 1) * 512], start=(c == 0), stop=(c == 7)),
                              r=[yTb, wout], w=[p], inc=(c == 7))
                    kb.op("dve", lambda e: e.tensor_tensor(out=x1[:, s, half * 512:(half + 1) * 512], in0=x1[:, s, half * 512:(half + 1) * 512], in1=p[:, :], op=ALU.add), r=[x1, p], w=[x1])
            if mode != "c2":
                for s in range(NSC):
                    kb.op("act", lambda e: e.activation(out=junk[:, :], in_=x1[:, s, :], func=AF.Square, accum_out=stc[:, s:s + 1]), r=[x1], w=[junk, stc])
                rstd_tm(kb, stc, epsc, NSC)
            for s in range(NSC):
                hbb = h2b[nxc("h", 2)]
                if mode != "c2":
                    kb.op("act", lambda e: e.activation(out=hbb[:, :], in_=x1[:, s, :], func=AF.Identity, scale=stc[:, NSC + s:NSC + s + 1]), r=[x1, stc], w=[hbb])
                if mode == "c1":
                    kb.dma("sp", "st", h2_d[tok0 + s * 128:tok0 + (s + 1) * 128, :], hbb[:, :], r=[hbb], w=[h2_d])
                    continue
                if mode == "c2":
                    ic = tbc * NSC + s
                    kb.gather("g", x1[:, s, :], x1_d.t.ap(), idxt[:, ic:ic + 1], r=[x1_d, idxt], w=[x1])
                    kb.gather("g", hbb[:, :], h2_d.t.ap(), idxt[:, ic:ic + 1], r=[h2_d, idxt], w=[hbb])
                pT = ps[0]
                pTb = pT.t.ap().bitcast(BF16)
                for c in range(8):
                    kb.op("pe", lambda e: e.transpose(pTb[:, c * 128:(c + 1) * 128], hbb[:, c * 128:(c + 1) * 128], identb[:, :]), r=[hbb, identb], w=[pT], inc=(c == 7))
                kb.op("dve", lambda e: e.tensor_tensor(out=h2T[:, :, s * 128:(s + 1) * 128], in0=pTb[:, 0:1024].rearrange("p (c t) -> p c t", t=128),
                                                       in1=pl(P_G2, 8).unsqueeze(2).to_broadcast([128, 8, 128]), op=ALU.mult), r=[pT, par], w=[h2T])
            if mode == "c1":
                kb.dma("sp", "st", x1_d[tok0:tok0 + TBC, :].rearrange("(s p) d -> p s d", p=128), x1[:, :, :], r=[x1], w=[x1_d])
                continue
            if moe:
                for s in range(NSC):
                    p = ps[1 + nxc("a", 2)]
                    for c in range(8):
                        kb.op("pe", lambda e: e.matmul(p[:, 0:NE], lhsT=h2T[:, c, s * 128:(s + 1) * 128], rhs=wrb[:, c, :], start=(c == 0), stop=(c == 7)), r=[h2T, wrb], w=[p], inc=(c == 7))
                    kb.op("dve", lambda e: e.tensor_tensor(out=rt[:, 0:8], in0=p[:, 0:NE], in1=pl(P_RB, 8), op=ALU.add), r=[p, par], w=[rt])
                    kb.op("dve", lambda e: e.max(out=rt[:, 8:16], in_=rt[:, 0:8]), r=[rt], w=[rt])
                    kb.op("dve", lambda e: e.tensor_tensor(out=rt[:, 16:17], in0=rt[:, 8:9], in1=rt[:, 9:10], op=ALU.subtract), r=[rt], w=[rt])
                    kb.op("act", lambda e: e.activation(out=rt[:, 17:18], in_=rt[:, 16:17], func=AF.Sigmoid), r=[rt], w=[rt])
                    kb.op("act", lambda e: e.activation(out=rt[:, 18:19], in_=rt[:, 16:17], func=AF.Sigmoid, scale=-1.0), r=[rt], w=[rt])
                    kb.op("dve", lambda e: e.tensor_scalar(out=rt[:, 24:32], in0=rt[:, 0:8], scalar1=rt[:, 8:9], scalar2=rt[:, 17:18], op0=ALU.is_equal, op1=ALU.mult), r=[rt], w=[rt])
                    kb.op("dve", lambda e: e.tensor_scalar(out=rt[:, 32:40], in0=rt[:, 0:8], scalar1=rt[:, 9:10], scalar2=rt[:, 18:19], op0=ALU.is_equal, op1=ALU.mult), r=[rt], w=[rt])
                    kb.op("dve", lambda e: e.tensor_tensor(out=gates[:, s, :], in0=rt[:, 24:32], in1=rt[:, 32:40], op=ALU.add), r=[rt], w=[gates])
            experts = range(NE) if moe else [None]
            for ex in experts:
                if ex is None:
                    Wg, Wu, Wd = ffn_g_d.t.ap()[0], ffn_u_d.t.ap()[0], ffn_d_d.t.ap()[0]
                    wbufs = [ffn_g_d, ffn_u_d, ffn_d_d]
                else:
                    Wg, Wu, Wd = moe_g_d.t.ap()[0, ex], moe_u_d.t.ap()[0, ex], moe_d_d.t.ap()[0, ex]
                    wbufs = [moe_g_d, moe_u_d, moe_d_d]
                for fg in range(DFF // 512):
                    wi = nxc("w", 2)
                    kb.dma("pool", "w", wgb[wi][:, :, :], Wg[:, fg * 512:(fg + 1) * 512].rearrange("(c p) f -> p c f", p=128), w=[wgb[wi]])
                    kb.dma("pool", "w", wub[wi][:, :, :], Wu[:, fg * 512:(fg + 1) * 512].rearrange("(c p) f -> p c f", p=128), w=[wub[wi]])
                    kb.dma("pool", "w", wdb[wi][:, :, :], Wd[fg * 512:(fg + 1) * 512, :].rearrange("(k p) d -> p k d", p=128), w=[wdb[wi]])
                    for t4 in range(TBC // 512):
                        tsl = slice(t4 * 512, (t4 + 1) * 512)
                        for k in range(4):
                            pg = ps[3 + nxc("g", 2)]
                            pu = ps[5 + nxc("u", 2)]
                            for c in range(8):
                                kb.op("pe", lambda e: e.matmul(pg[:, :], lhsT=wgb[wi][:, c, k * 128:(k + 1) * 128], rhs=h2T[:, c, tsl], start=(c == 0), stop=(c == 7)), r=[wgb[wi], h2T], w=[pg], inc=(c == 7))
                            for c in range(8):
                                kb.op("pe", lambda e: e.matmul(pu[:, :], lhsT=wub[wi][:, c, k * 128:(k + 1) * 128], rhs=h2T[:, c, tsl], start=(c == 0), stop=(c == 7)), r=[wub[wi], h2T], w=[pu], inc=(c == 7))
                            sg = sgt[nxc("sg", 2)]
                            kb.op("act", lambda e: e.activation(out=sg[:, :], in_=pg[:, :], func=AF.Silu), r=[pg], w=[sg])
                            kb.op("dve", lambda e: e.tensor_tensor(out=hact[:, k, tsl], in0=sg[:, :], in1=pu[:, :], op=ALU.mult), r=[sg, pu], w=[hact])
                    for s in range(NSC):
                        for half in range(2):
                            p = ps[1 + nxc("a", 2)]
                            for k in range(4):
                                kb.op("pe", lambda e: e.matmul(p[:, :], lhsT=hact[:, k, s * 128:(s + 1) * 128], rhs=wdb[wi][:, k, half * 512:(half + 1) * 512], start=(k == 0), stop=(k == 3)),
                                      r=[hact, wdb[wi]], w=[p], inc=(k == 3))
                            xs = x1[:, s, half * 512:(half + 1) * 512]
                            if ex is None:
                                kb.op("dve", lambda e: e.tensor_tensor(out=xs, in0=xs, in1=p[:, :], op=ALU.add), r=[x1, p], w=[x1])
                            else:
                                kb.op("dve", lambda e: e.scalar_tensor_tensor(out=xs, in0=p[:, :], scalar=gates[:, s, ex:ex + 1], in1=xs, op0=ALU.mult, op1=ALU.add), r=[x1, p, gates], w=[x1])
            kb.dma("sp", "st", x_dst[tok0:tok0 + TBC, :].rearrange("(s p) d -> p s d", p=128), x1[:, :, :], r=[x1], w=[x_dst])
        if moe:
            kb.barrier()
            stackC1.close()
            stackR = ExitStack()
            kb.stack = stackR
            NJ = TH // 128
            CAP = ((TH * 2 // NE) * 3 // 2 + 511) // 512 * 512
            NSR = CAP // 128
            xdisp_d = kb.dram("xdisp_s", [NE * CAP, D], BF16)
            ydisp_d = kb.dram("ydisp_s", [NE * CAP, D], F32)
            h2R = kb.sbuf("h2R", [128, 8, CAP], BF16)
            hactR = kb.sbuf("hactR", [128, 4, CAP], BF16)
            yacc = kb.sbuf("yacc", [128, NSR, D], F32)
            slots = kb.sbuf("slots", [128, NJ, 2], I32)
            wts = kb.sbuf("wts", [128, NJ, 2], F32)
            basec = kb.sbuf("basec", [128, NE], F32)
            eoff = kb.sbuf("eoff", [128, NE], F32)
            selb = kb.sbuf("selb", [128, NE], BF16)
            xa = kb.sbuf("xa", [128, D], F32)
            y1 = kb.sbuf("y1", [128, D], F32)
            y2 = kb.sbuf("y2", [128, D], F32)
            kb.op("dve", lambda e: e.memset(basec[:, :], 0.0), w=[basec])
            for ex in range(NE):
                kb.op("dve", lambda e: e.memset(eoff[:, ex:ex + 1], float(ex * CAP)), w=[eoff])

            def transposed(hbb, dst_ap):
                pT = ps[0]
                pTb = pT.t.ap().bitcast(BF16)
                for c in range(8):
                    kb.op("pe", lambda e: e.transpose(pTb[:, c * 128:(c + 1) * 128], hbb[:, c * 128:(c + 1) * 128], identb[:, :]), r=[hbb, identb], w=[pT], inc=(c == 7))
                kb.op("dve", lambda e: e.tensor_tensor(out=dst_ap, in0=pTb[:, 0:1024].rearrange("p (c t) -> p c t", t=128),
                                                       in1=pl(P_G2, 8).unsqueeze(2).to_broadcast([128, 8, 128]), op=ALU.mult), r=[pT, par], w=[h2R])

            for j in range(NJ):
                hi = nxc("h", 2)
                hbb = h2b[hi]
                kb.gather("gh%d" % hi, hbb[:, :], h2_d.t.ap(), idxt[:, j:j + 1], r=[h2_d, idxt], w=[hbb])
                transposed(hbb, h2R[:, :, 0:128])
                p = ps[1 + nxc("a", 2)]
                for c in range(8):
                    kb.op("pe", lambda e: e.matmul(p[:, 0:NE], lhsT=h2R[:, c, 0:128], rhs=wrb[:, c, :], start=(c == 0), stop=(c == 7)), r=[h2R, wrb], w=[p], inc=(c == 7))
                kb.op("dve", lambda e: e.tensor_tensor(out=rt[:, 0:8], in0=p[:, 0:NE], in1=pl(P_RB, 8), op=ALU.add), r=[p, par], w=[rt])
                kb.op("dve", lambda e: e.max(out=rt[:, 8:16], in_=rt[:, 0:8]), r=[rt], w=[rt])
                kb.op("dve", lambda e: e.tensor_tensor(out=rt[:, 16:17], in0=rt[:, 8:9], in1=rt[:, 9:10], op=ALU.subtract), r=[rt], w=[rt])
                kb.op("act", lambda e: e.activation(out=wts[:, j, 0:1], in_=rt[:, 16:17], func=AF.Sigmoid), r=[rt], w=[wts])
                kb.op("act", lambda e: e.activation(out=wts[:, j, 1:2], in_=rt[:, 16:17], func=AF.Sigmoid, scale=-1.0), r=[rt], w=[wts])
                kb.op("dve", lambda e: e.tensor_scalar(out=rt[:, 24:32], in0=rt[:, 0:8], scalar1=rt[:, 8:9], scalar2=None, op0=ALU.is_equal), r=[rt], w=[rt])
                kb.op("dve", lambda e: e.tensor_scalar(out=rt[:, 32:40], in0=rt[:, 0:8], scalar1=rt[:, 9:10], scalar2=None, op0=ALU.is_equal), r=[rt], w=[rt])
                kb.op("dve", lambda e: e.tensor_tensor(out=rt[:, 40:48], in0=rt[:, 24:32], in1=rt[:, 32:40], op=ALU.add), r=[rt], w=[rt])
                kb.op("dve", lambda e: e.tensor_copy(out=selb[:, :], in_=rt[:, 40:48]), r=[rt], w=[selb])
                pp, ptot = ps[3], ps[5]
                kb.op("pe", lambda e: e.matmul(pp[:, 0:NE], lhsT=trib[:, :], rhs=selb[:, :], start=True, stop=True), r=[trib, selb], w=[pp])
                kb.op("pe", lambda e: e.matmul(ptot[:, 0:NE], lhsT=onesb[:, :], rhs=selb[:, :], start=True, stop=True), r=[onesb, selb], w=[ptot])
                kb.op("dve", lambda e: e.tensor_tensor(out=rt[:, 48:56], in0=pp[:, 0:NE], in1=rt[:, 40:48], op=ALU.subtract), r=[pp, rt], w=[rt])
                kb.op("dve", lambda e: e.tensor_tensor(out=rt[:, 48:56], in0=rt[:, 48:56], in1=basec[:, :], op=ALU.add), r=[rt, basec], w=[rt])
                kb.op("dve", lambda e: e.tensor_tensor(out=rt[:, 48:56], in0=rt[:, 48:56], in1=eoff[:, :], op=ALU.add), r=[rt, eoff], w=[rt])
                kb.op("dve", lambda e: e.tensor_tensor(out=basec[:, :], in0=basec[:, :], in1=ptot[:, 0:NE], op=ALU.add), r=[basec, ptot], w=[basec])
                for k in range(2):
                    kb.op("dve", lambda e: e.tensor_tensor(out=rt[:, 56:64], in0=rt[:, 24 + 8 * k:32 + 8 * k], in1=rt[:, 48:56], op=ALU.mult), r=[rt], w=[rt])
                    kb.op("dve", lambda e: e.tensor_reduce(out=rt[:, 20 + k:21 + k], in_=rt[:, 56:64], axis=mybir.AxisListType.X, op=ALU.add), r=[rt], w=[rt])
                kb.op("dve", lambda e: e.tensor_copy(out=slots[:, j, :], in_=rt[:, 20:22]), r=[rt], w=[slots])
                for k in range(2):
                    kb.ncall += 1
                    kb._wait("pool", kb._deps([hbb, slots], [xdisp_d]))
                    inst = nc.gpsimd.indirect_dma_start(out=xdisp_d.t.ap(), out_offset=bass.IndirectOffsetOnAxis(slots[:, j, k:k + 1], 0), in_=hbb[:, :], in_offset=None)
                    sk = "d:sc%d" % hi
                    if sk not in kb.sems:
                        kb._newsem(sk)
                    kb.cnt[sk] += 16
                    inst.then_inc(kb.sems[sk], 16)
                    kb._mark(sk, kb.cnt[sk], [hbb, slots], [xdisp_d])

            for ex in range(NE):
                for s in range(NSR):
                    hi = nxc("h", 2)
                    hbb = h2b[hi]
                    kb.dma("sp", "lh%d" % hi, hbb[:, :], xdisp_d[ex * CAP + s * 128:ex * CAP + (s + 1) * 128, :], r=[xdisp_d], w=[hbb])
                    transposed(hbb, h2R[:, :, s * 128:(s + 1) * 128])
                Wg, Wu, Wd = moe_g_d.t.ap()[0, ex], moe_u_d.t.ap()[0, ex], moe_d_d.t.ap()[0, ex]
                for fg in range(DFF // 512):
                    wi = nxc("w", 2)
                    kb.dma("pool", "w", wgb[wi][:, :, :], Wg[:, fg * 512:(fg + 1) * 512].rearrange("(c p) f -> p c f", p=128), w=[wgb[wi]])
                    kb.dma("pool", "w", wub[wi][:, :, :], Wu[:, fg * 512:(fg + 1) * 512].rearrange("(c p) f -> p c f", p=128), w=[wub[wi]])
                    kb.dma("pool", "w", wdb[wi][:, :, :], Wd[fg * 512:(fg + 1) * 512, :].rearrange("(k p) d -> p k d", p=128), w=[wdb[wi]])
                    for t4 in range(CAP // 512):
                        tsl = slice(t4 * 512, (t4 + 1) * 512)
                        for k in range(4):
                            pg = ps[3 + nxc("g", 2)]
                            pu = ps[5 + nxc("u", 2)]
                            for c in range(8):
                                kb.op("pe", lambda e: e.matmul(pg[:, :], lhsT=wgb[wi][:, c, k * 128:(k + 1) * 128], rhs=h2R[:, c, tsl], start=(c == 0), stop=(c == 7)), r=[wgb[wi], h2R], w=[pg], inc=(c == 7))
                            for c in range(8):
                                kb.op("pe", lambda e: e.matmul(pu[:, :], lhsT=wub[wi][:, c, k * 128:(k + 1) * 128], rhs=h2R[:, c, tsl], start=(c == 0), stop=(c == 7)), r=[wub[wi], h2R], w=[pu], inc=(c == 7))
                            sg = sgt[nxc("sg", 2)]
                            kb.op("act", lambda e: e.activation(out=sg[:, :], in_=pg[:, :], func=AF.Silu), r=[pg], w=[sg])
                            kb.op("dve", lambda e: e.tensor_tensor(out=hactR[:, k, tsl], in0=sg[:, :], in1=pu[:, :], op=ALU.mult), r=[sg, pu], w=[hactR])
                    for s in range(NSR):
                        for half in range(2):
                            p = ps[1 + nxc("a", 2)]
                            for k in range(4):
                                kb.op("pe", lambda e: e.matmul(p[:, :], lhsT=hactR[:, k, s * 128:(s + 1) * 128], rhs=wdb[wi][:, k, half * 512:(half + 1) * 512], start=(k == 0), stop=(k == 3)),
                                      r=[hactR, wdb[wi]], w=[p], inc=(k == 3))
                            ys = yacc[:, s, half * 512:(half + 1) * 512]
                            if fg == 0:
                                kb.op("dve", lambda e: e.tensor_copy(out=ys, in_=p[:, :]), r=[p], w=[yacc])
                            else:
                                kb.op("dve", lambda e: e.tensor_tensor(out=ys, in0=ys, in1=p[:, :], op=ALU.add), r=[yacc, p], w=[yacc])
                kb.dma("sp", "st", ydisp_d[ex * CAP:(ex + 1) * CAP, :].rearrange("(s p) d -> p s d", p=128), yacc[:, :, :], r=[yacc], w=[ydisp_d])

            for j in range(NJ):
                kb.gather("gx", xa[:, :], x1_d.t.ap(), idxt[:, j:j + 1], r=[x1_d, idxt], w=[xa])
                kb.gather("gy1", y1[:, :], ydisp_d.t.ap(), slots[:, j, 0:1], r=[ydisp_d, slots], w=[y1])
                kb.gather("gy2", y2[:, :], ydisp_d.t.ap(), slots[:, j, 1:2], r=[ydisp_d, slots], w=[y2])
                kb.op("dve", lambda e: e.scalar_tensor_tensor(out=xa[:, :], in0=y1[:, :], scalar=wts[:, j, 0:1], in1=xa[:, :], op0=ALU.mult, op1=ALU.add), r=[xa, y1, wts], w=[xa])
                kb.op("dve", lambda e: e.scalar_tensor_tensor(out=xa[:, :], in0=y2[:, :], scalar=wts[:, j, 1:2], in1=xa[:, :], op0=ALU.mult, op1=ALU.add), r=[xa, y2, wts], w=[xa])
                kb.dma("sp", "st", yout[j * 128:(j + 1) * 128, :], xa[:, :], r=[xa], w=[yout])
            kb.barrier()
            stackR.close()
        else:
            kb.barrier()
            stackC1.close()
        kb.stack = stackC
        kb.barrier()
        stackC.close()
        kb.stack = None

    kb.finish()
    return nc, dbg


def rstd_tm(kb, st, epsc, n):
    kb.op("act", lambda e: e.activation(out=st[:, n:2 * n], in_=st[:, 0:n], func=AF.Ln, bias=epsc[:, 0:1], scale=1.0 / D), r=[st, epsc], w=[st])
    kb.op("act", lambda e: e.activation(out=st[:, n:2 * n], in_=st[:, n:2 * n], func=AF.Exp, scale=-0.5), r=[st], w=[st])


_CACHE = {}


def kernel(**inputs):
    x = np.ascontiguousarray(inputs["x"], dtype=np.float32)
    B, S, _ = x.shape
    key = S
    if key not in _CACHE:
        _CACHE[key] = build(S)[0]
    nc = _CACHE[key]
    consts = make_consts()
    params = make_params(inputs)
    shared = {k: np.ascontiguousarray(inputs[k], dtype=np.float32) for k in
              ("w_in", "w_out", "ffn_w_gate", "ffn_w_up", "ffn_w_down", "moe_router_w", "moe_w_gate", "moe_w_up", "moe_w_down")}
    n = 8
    TH = S // 2
    in_maps = []
    for cid in range(n):
        m = dict(shared)
        m["x"] = x[cid % B]
        m["consts"] = consts
        m["params"] = params
        rank = cid // B
        m["tokidx"] = (rank * TH + np.arange(TH, dtype=np.int32)).reshape(TH // 128, 128).T.copy()
        in_maps.append(m)
    res = run_bass_kernel_spmd(nc, in_maps, core_ids=list(range(n)))
    out = np.stack([np.concatenate([res.results[b]["y"], res.results[b + B]["y"]], axis=0) for b in range(B)], axis=0)
    return out.astype(np.float32)
```

```python
import numpy as np
from contextlib import ExitStack
import concourse.bass as bass
import concourse.mybir as mybir
from concourse.bass_utils import run_bass_kernel_spmd

F32 = mybir.dt.float32
BF16 = mybir.dt.bfloat16
I32 = mybir.dt.int32
AF = mybir.ActivationFunctionType
ALU = mybir.AluOpType

D = 1024
DIN = 3336
DFF = 3584
NE = 8
EPS = 1e-6
TINY = 1e-30
C_Q, C_K, C_V, C_F = 0, 512, 1024, 1536
C_CX, C_CB, C_CC = 1544, 1800, 2056
C_RQ, C_RF, C_RI, C_RG = 2312, 2568, 2824, 3080

K_ID, K_BLK, K_TRI, K_ONES, K_SEL = 0, 128, 256, 384, 512
K_DM = 640
K_CM = K_DM + 2048
K_SEG = K_CM + 64
NCONST = K_SEG + 512

P_G1, P_G2, P_GQ, P_GK, P_CW, P_LB, P_MG, P_FB, P_RB, P_GQR, P_GKR = 0, 8, 16, 17, 18, 24, 28, 36, 44, 52, 116
NPAR = 180


def make_consts():
    c = np.zeros((128, NCONST), np.float32)
    p = np.arange(128)
    c[:, K_ID:K_ID + 128] = np.eye(128)
    c[:, K_BLK:K_BLK + 128] = ((p[:, None] // 64) == (p[None, :] // 64)) / 64.0
    c[:, K_TRI:K_TRI + 128] = (p[:, None] <= p[None, :])
    c[:, K_ONES:K_ONES + 128] = 1.0
    c[127, K_SEL:K_SEL + 128] = 1.0
    q = np.arange(512)
    for j in range(4):
        c[:, K_DM + j * 512:K_DM + (j + 1) * 512] = ((j * 128 + p[:, None]) <= q[None, :])
    t = np.arange(64)
    c[:, K_CM:K_CM + 64] = ((p[:, None] % 64) <= t[None, :])
    c[:, K_SEG:K_SEG + 512] = ((q % 64) != 0)[None, :]
    return c


def make_params(inp):
    L = 2
    P = np.zeros((L, 128, NPAR), np.float32)
    for l in range(L):
        P[l, :, P_G1:P_G1 + 8] = inp["norm_mix"][l].reshape(8, 128).T
        P[l, :, P_G2:P_G2 + 8] = inp["norm_ffn"][l].reshape(8, 128).T
        P[l, :, P_GQ] = np.tile(inp["q_norm_gain"][l], 2)
        P[l, :, P_GK] = np.tile(inp["k_norm_gain"][l], 2)
        P[l, :, P_CW:P_CW + 6] = inp["conv_w"][l].reshape(3, 2, 128).transpose(2, 0, 1).reshape(128, 6)
        P[l, :, P_LB:P_LB + 4] = inp["hgrn_lb_logits"].reshape(2, 2, 128).transpose(2, 0, 1).reshape(128, 4)
        P[l, :, P_MG:P_MG + 8] = inp["mix_out_gain"][l].reshape(8, 128).T
        P[l, :, P_FB:P_FB + 8] = inp["attn_f_bias"][l][None, :]
        P[l, :, P_RB:P_RB + 8] = inp["moe_router_b"][0][None, :]
        P[l, :, P_GQR:P_GQR + 64] = inp["q_norm_gain"][l][None, :]
        P[l, :, P_GKR:P_GKR + 64] = inp["k_norm_gain"][l][None, :]
    return P


class Buf:
    __slots__ = ("t", "w", "r", "nowaw")

    def __init__(self, t, nowaw=False):
        self.t = t
        self.w = {}
        self.r = {}
        self.nowaw = nowaw

    def __getitem__(self, idx):
        return self.t.ap()[idx]


class KB:
    def __init__(self, nc):
        self.nc = nc
        self.eng = {"pe": nc.tensor, "act": nc.scalar, "dve": nc.vector, "pool": nc.gpsimd, "sp": nc.sync}
        self.sems = {}
        self.cnt = {}
        self.waited = {e: {} for e in self.eng}
        self.nsem = 0
        self.stack = None
        self.ncall = 0
        import os as _os
        self.noself = _os.environ.get('NOSELF', '0') == '1'
        import os
        self.cut = int(os.environ['KCUT']) if 'KCUT' in os.environ else None
        for e in self.eng:
            self._newsem(e)

    def _newsem(self, key):
        self.nsem += 1
        self.sems[key] = self.nc.alloc_semaphore(name="s%d_%s" % (self.nsem, key.replace(":", "_")))
        self.cnt[key] = 0

    def sbuf(self, name, shape, dt):
        if self.stack is not None:
            return Buf(self.stack.enter_context(self.nc.sbuf_tensor(name, list(shape), dt)))
        return Buf(self.nc.alloc_sbuf_tensor(name, list(shape), dt))

    def psum(self, name, shape, dt):
        return Buf(self.nc.alloc_psum_tensor(name, list(shape), dt))

    def dram(self, name, shape, dt, kind=None):
        if kind is None:
            t = self.nc.dram_tensor(name, list(shape), dt)
        else:
            t = self.nc.dram_tensor(name, list(shape), dt, kind=kind)
        return Buf(t, nowaw=True)

    def _wait(self, en, toks):
        e = self.eng[en]
        wd = self.waited[en]
        for k, v in toks.items():
            if en == "pe" and k == "pe":
                continue
            if self.noself and k == en:
                continue
            if wd.get(k, 0) < v:
                e.wait_ge(self.sems[k], v)
                wd[k] = v

    def _deps(self, r, w):
        toks = {}
        for b in r:
            for k, v in b.w.items():
                if toks.get(k, 0) < v:
                    toks[k] = v
        for b in w:
            for k, v in b.r.items():
                if toks.get(k, 0) < v:
                    toks[k] = v
            if not b.nowaw:
                for k, v in b.w.items():
                    if toks.get(k, 0) < v:
                        toks[k] = v
        return toks

    def _mark(self, key, val, r, w):
        for b in r:
            if b.r.get(key, 0) < val:
                b.r[key] = val
        for b in w:
            if b.nowaw:
                if b.w.get(key, 0) < val:
                    b.w[key] = val
            else:
                b.w = {key: val}
            b.r = {}

    def op(self, en, fn, r=(), w=(), inc=True):
        self.ncall += 1
        if self.cut is not None and self.ncall > self.cut:
            return None
        self._wait(en, self._deps(r, w))
        inst = fn(self.eng[en])
        if inc:
            self.cnt[en] += 1
            inst.then_inc(self.sems[en], 1)
            val = self.cnt[en]
        else:
            val = self.cnt[en] + 1
        self._mark(en, val, r, w)
        return inst

    def dma(self, q, stream, out, in_, r=(), w=(), **kw):
        key = "d:" + stream
        self.ncall += 1
        if self.cut is not None and self.ncall > self.cut:
            return None
        if key not in self.sems:
            self._newsem(key)
        self._wait(q, self._deps(r, w))
        inst = self.eng[q].dma_start(out=out, in_=in_, **kw)
        self.cnt[key] += 16
        inst.then_inc(self.sems[key], 16)
        self._mark(key, self.cnt[key], r, w)
        return inst

    def gather(self, stream, out, in_full, idx_ap, r=(), w=()):
        key = "d:" + stream
        if key not in self.sems:
            self._newsem(key)
        self._wait("pool", self._deps(r, w))
        inst = self.nc.gpsimd.indirect_dma_start(out=out, out_offset=None, in_=in_full, in_offset=bass.IndirectOffsetOnAxis(idx_ap, 0))
        self.cnt[key] += 16
        inst.then_inc(self.sems[key], 16)
        self._mark(key, self.cnt[key], r, w)
        return inst

    def barrier(self):
        for en in self.eng:
            self._wait(en, dict(self.cnt))

    def finish(self):
        self._wait("sp", dict(self.cnt))


def build(T, L=2, debug=False, stop=None):
    assert T % 512 == 0
    NB = T // 512
    NS = T // 128
    TBC = min(1024, T)
    NSC = TBC // 128
    nc = bass.Bass("TRN2", target_bir_lowering=False)
    kb = KB(nc)

    def ext_in(name, shape):
        return Buf(nc.dram_tensor(name, list(shape), F32, kind="ExternalInput"), nowaw=True)

    xin = ext_in("x", [T, D])
    consts_d = ext_in("consts", [128, NCONST])
    params_d = ext_in("params", [L, 128, NPAR])
    w_in_d = ext_in("w_in", [L, D, DIN])
    w_out_d = ext_in("w_out", [L, D, D])
    ffn_g_d = ext_in("ffn_w_gate", [1, D, DFF])
    ffn_u_d = ext_in("ffn_w_up", [1, D, DFF])
    ffn_d_d = ext_in("ffn_w_down", [1, DFF, D])
    wr_d = ext_in("moe_router_w", [1, D, NE])
    moe_g_d = ext_in("moe_w_gate", [1, NE, D, DFF])
    moe_u_d = ext_in("moe_w_up", [1, NE, D, DFF])
    moe_d_d = ext_in("moe_w_down", [1, NE, DFF, D])
    TH = T // 2
    yout = Buf(nc.dram_tensor("y", [TH, D], F32, kind="ExternalOutput"), nowaw=True)
    tokidx_d = Buf(nc.dram_tensor("tokidx", [128, TH // 128], I32, kind="ExternalInput"), nowaw=True)

    dbg = {}

    def scratch(name, shape, dt):
        if debug:
            b = Buf(nc.dram_tensor(name, list(shape), dt, kind="ExternalOutput"), nowaw=True)
            dbg[name] = b
            return b
        return kb.dram(name, shape, dt)

    qT_d = scratch("qT_s", [512, T], BF16)
    kT_d = scratch("kT_s", [512, T], BF16)
    v_d = scratch("v_s", [T, 512], BF16)
    yT_d = scratch("yT_s", [1024, T], BF16)
    xmid_d = scratch("xmid_s", [T, D], F32)
    x1_d = kb.dram("x1_s", [T, D], F32)
    h2_d = kb.dram("h2_s", [T, D], BF16)

    cst = kb.sbuf("cst", [128, NCONST], F32)
    par = kb.sbuf("par", [128, L, NPAR], F32)
    identb = kb.sbuf("identb", [128, 128], BF16)
    blkb = kb.sbuf("blkb", [128, 128], BF16)
    onesb = kb.sbuf("onesb", [128, 128], BF16)
    dmaskb = kb.sbuf("dmaskb", [128, 4, 512], BF16)
    cmaskb = kb.sbuf("cmaskb", [128, 64], F32)
    epsc = kb.sbuf("epsc", [128, 1], F32)
    onec = kb.sbuf("onec", [128, 1], F32)
    lsig = kb.sbuf("lsig", [128, NS, 8], F32)
    negd = kb.sbuf("negd", [128, NS, 8], F32)
    cI = kb.sbuf("cI", [128, NS, 8], F32)
    lbt = kb.sbuf("lbt", [128, 2], F32)
    omlt = kb.sbuf("omlt", [128, 2], F32)
    nomlt = kb.sbuf("nomlt", [128, 2], F32)
    bshift = kb.sbuf("bshift", [128, 1], F32)
    small = kb.sbuf("small", [128, 64], F32)
    idxt = kb.sbuf("idxt", [128, TH // 128], I32)

    ps = [kb.psum("ps%d" % i, [128, 512], F32) for i in range(8)]

    kb.dma("sp", "ld", cst[:, :], consts_d[:, :], w=[cst])
    kb.dma("sp", "ld", idxt[:, :], tokidx_d[:, :], w=[idxt])
    kb.dma("sp", "ld", par[:, :, :], params_d.t.ap().rearrange("l p n -> p l n"), w=[par])
    kb.op("dve", lambda e: e.tensor_copy(out=identb[:, :], in_=cst[:, K_ID:K_ID + 128]), r=[cst], w=[identb])
    kb.op("dve", lambda e: e.tensor_copy(out=blkb[:, :], in_=cst[:, K_BLK:K_BLK + 128]), r=[cst], w=[blkb])
    kb.op("dve", lambda e: e.tensor_copy(out=onesb[:, :], in_=cst[:, K_ONES:K_ONES + 128]), r=[cst], w=[onesb])
    kb.op("dve", lambda e: e.tensor_copy(out=dmaskb[:, :, :], in_=cst[:, K_DM:K_DM + 2048].rearrange("p (j q) -> p j q", q=512)), r=[cst], w=[dmaskb])
    kb.op("dve", lambda e: e.tensor_copy(out=cmaskb[:, :], in_=cst[:, K_CM:K_CM + 64]), r=[cst], w=[cmaskb])
    kb.op("dve", lambda e: e.memset(epsc[:, :], EPS), w=[epsc])
    kb.op("dve", lambda e: e.memset(onec[:, :], 1.0), w=[onec])

    def rstd_from_ms(ms_ap, ms_bufs, out_b, out_ap, tmp_b, tmp_ap):
        kb.op("act", lambda e: e.activation(out=tmp_ap, in_=ms_ap, func=AF.Ln, bias=epsc[:, 0:1], scale=1.0), r=ms_bufs + [epsc], w=[tmp_b])
        kb.op("act", lambda e: e.activation(out=out_ap, in_=tmp_ap, func=AF.Exp, scale=-0.5), r=[tmp_b], w=[out_b])

    for l in range(L):
        x_src = xin if l == 0 else xmid_d
        x_dst = xmid_d if l == 0 else yout
        pl = lambda c0, n=1: par[:, l, c0:c0 + n]

        stackA = ExitStack()
        kb.stack = stackA
        win = kb.sbuf("win%d" % l, [128, 8, DIN], BF16)
        for c in range(8):
            kb.dma("pool", "w", win[:, c, :], w_in_d[l, c * 128:(c + 1) * 128, :], w=[win])
        xt = kb.sbuf("xt%d" % l, [128, 4, D], F32)
        hb = kb.sbuf("hb%d" % l, [128, 4, D], BF16)
        hT = kb.sbuf("hT%d" % l, [128, 8, 512], BF16)
        junk = kb.sbuf("junk%d" % l, [128, D], F32)
        st4 = kb.sbuf("st4%d" % l, [128, 8], F32)
        fa = [kb.sbuf("fa%d_%d" % (l, i), [128, 512], F32) for i in range(6)]
        fb = [kb.sbuf("fb%d_%d" % (l, i), [128, 512], BF16) for i in range(3)]
        ob = [kb.sbuf("ob%d_%d" % (l, i), [128, 512], BF16) for i in range(2)]
        vb = kb.sbuf("vb%d" % l, [128, 4, 512], BF16)
        vi = kb.sbuf("vi%d" % l, [128, 4, 256], BF16)
        ucv = [kb.sbuf("ucv%d_%d" % (l, j), [128, 514], F32) for j in range(2)]
        sig = kb.sbuf("sig%d" % l, [128, 512], F32)
        kk = kb.sbuf("kk%d" % l, [128, 512], F32)
        cc = kb.sbuf("cc%d" % l, [128, 512], F32)
        qs = kb.sbuf("qs%d" % l, [128, 512], F32)
        gs = [kb.sbuf("gs%d_%d" % (l, j), [128, 512], F32) for j in range(2)]
        qe = [kb.sbuf("qe%d_%d" % (l, j), [128, 512], BF16) for j in range(2)]
        ke = [kb.sbuf("ke%d_%d" % (l, j), [128, 512], BF16) for j in range(2)]
        qec = [kb.sbuf("qec%d_%d" % (l, j), [128, 512], BF16) for j in range(2)]
        kdT = kb.sbuf("kdT%d" % l, [128, 512], BF16)
        kd = kb.sbuf("kd%d" % l, [128, 4, 256], BF16)
        ecl = kb.sbuf("ecl%d" % l, [128, 2, 8], F32)
        state = kb.sbuf("state%d" % l, [128, 2, 64], F32)
        stateb = kb.sbuf("stateb%d" % l, [128, 2, 64], BF16)
        atsb = kb.sbuf("atsb%d" % l, [128, 128], BF16)
        osb = kb.sbuf("osb%d" % l, [128, 2, 512], F32)
        vi2 = kb.sbuf("vi2_%d" % l, [128, 8, 256], BF16)
        kd2 = kb.sbuf("kd2_%d" % l, [128, 8, 256], BF16)
        segm = kb.sbuf("segm%d" % l, [128, 512], F32)

        kb.op("dve", lambda e: e.tensor_copy(out=segm[:, :], in_=cst[:, K_SEG:K_SEG + 512]), r=[cst], w=[segm])
        kb.op("dve", lambda e: e.memset(state[:, :, :], 0.0), w=[state])
        kb.op("dve", lambda e: e.memset(stateb[:, :, :], 0.0), w=[stateb])
        for j in range(2):
            kb.op("dve", lambda e: e.memset(ucv[j][:, :], 0.0), w=[ucv[j]])
        if l == 0:
            kb.op("dve", lambda e: e.memset(lbt[:, :], 0.0), w=[lbt])
        else:
            kb.op("dve", lambda e: e.tensor_tensor(out=small[:, 0:2], in0=pl(P_LB + 2, 2), in1=pl(P_LB, 2), op=ALU.subtract), r=[par], w=[small])
            kb.op("act", lambda e: e.activation(out=lbt[:, :], in_=small[:, 0:2], func=AF.Sigmoid), r=[small], w=[lbt])
        kb.op("dve", lambda e: e.tensor_scalar(out=omlt[:, :], in0=lbt[:, :], scalar1=-1.0, scalar2=1.0, op0=ALU.mult, op1=ALU.add), r=[lbt], w=[omlt])
        kb.op("dve", lambda e: e.tensor_scalar(out=nomlt[:, :], in0=omlt[:, :], scalar1=-1.0, scalar2=None, op0=ALU.mult), r=[omlt], w=[nomlt])
        kb.op("dve", lambda e: e.tensor_reduce(out=small[:, 8:9], in_=pl(P_GQR, 64), axis=mybir.AxisListType.X, op=ALU.max, apply_absolute_value=True), r=[par], w=[small])
        kb.op("dve", lambda e: e.tensor_reduce(out=small[:, 9:10], in_=pl(P_GKR, 64), axis=mybir.AxisListType.X, op=ALU.max, apply_absolute_value=True), r=[par, small], w=[small])
        kb.op("dve", lambda e: e.scalar_tensor_tensor(out=bshift[:, :], in0=small[:, 8:9], scalar=8.0, in1=small[:, 9:10], op0=ALU.mult, op1=ALU.mult), r=[small], w=[bshift])

        rot = {"fm": 0, "tm": 0, "fa": 0, "fb": 0, "ob": 0}

        def nxt(name, n):
            v = rot[name]
            rot[name] = (v + 1) % n
            return v

        def fm_chunk(col0):
            p = ps[1 + nxt("fm", 2)]
            for c in range(8):
                kb.op("pe", lambda e: e.matmul(p[:, :], lhsT=win[:, c, col0:col0 + 128], rhs=hT[:, c, :], start=(c == 0), stop=(c == 7)),
                      r=[win, hT], w=[p], inc=(c == 7))
            return p

        def headnorm_store(src_ap, src_bufs, gain_ap, dst_d, row0, tok0, mul_b=None):
            sq = fb[nxt("fb", 3)]
            kb.op("act", lambda e: e.activation(out=sq[:, :], in_=src_ap, func=AF.Square), r=src_bufs, w=[sq])
            pm = ps[5]
            kb.op("pe", lambda e: e.matmul(pm[:, :], lhsT=blkb[:, :], rhs=sq[:, :], start=True, stop=True), r=[blkb, sq], w=[pm])
            t1 = fa[nxt("fa", 6)]
            rs = fa[nxt("fa", 6)]
            rstd_from_ms(pm[:, :], [pm], rs, rs[:, :], t1, t1[:, :])
            o = ob[nxt("ob", 2)]
            if mul_b is None:
                kb.op("dve", lambda e: e.scalar_tensor_tensor(out=o[:, :], in0=src_ap, scalar=gain_ap, in1=rs[:, :], op0=ALU.mult, op1=ALU.mult),
                      r=src_bufs + [rs, par], w=[o])
            else:
                kb.op("dve", lambda e: e.scalar_tensor_tensor(out=t1[:, :], in0=src_ap, scalar=gain_ap, in1=rs[:, :], op0=ALU.mult, op1=ALU.mult),
                      r=src_bufs + [rs, par], w=[t1])
                kb.op("dve", lambda e: e.tensor_tensor(out=o[:, :], in0=t1[:, :], in1=mul_b[:, :], op=ALU.mult), r=[t1, mul_b], w=[o])
            kb.dma("sp", "st", dst_d[row0:row0 + 128, tok0:tok0 + 512], o[:, :], r=[o], w=[dst_d])

        for tb in range(NB):
            tok0 = tb * 512
            kb.dma("sp", "ld", xt[:, :, :], x_src[tok0:tok0 + 512, :].rearrange("(s p) d -> p s d", p=128), r=[x_src], w=[xt])
            for s in range(4):
                kb.op("act", lambda e: e.activation(out=junk[:, :], in_=xt[:, s, :], func=AF.Square, accum_out=st4[:, s:s + 1]), r=[xt], w=[junk, st4])
            rstd_tm(kb, st4, epsc, 4)
            for s in range(4):
                kb.op("act", lambda e: e.activation(out=hb[:, s, :], in_=xt[:, s, :], func=AF.Identity, scale=st4[:, 4 + s:5 + s]), r=[xt, st4], w=[hb])
            pT = ps[0]
            pTb = pT.t.ap().bitcast(BF16)
            for c in range(8):
                half = (c % 2) * 512
                for s in range(4):
                    kb.op("pe", lambda e: e.transpose(pTb[:, half + s * 128:half + (s + 1) * 128], hb[:, s, c * 128:(c + 1) * 128], identb[:, :]),
                          r=[hb, identb], w=[pT], inc=(s == 3))
                en = "dve" if c % 2 == 0 else "act"
                if en == "dve":
                    kb.op("dve", lambda e: e.tensor_scalar(out=hT[:, c, :], in0=pTb[:, half:half + 512], scalar1=pl(P_G1 + c), scalar2=None, op0=ALU.mult), r=[pT, par], w=[hT])
                else:
                    kb.op("act", lambda e: e.activation(out=hT[:, c, :], in_=pTb[:, half:half + 512], func=AF.Identity, scale=pl(P_G1 + c)), r=[pT, par], w=[hT])

            for which, col0, dst, gcol in (("q", C_Q, qT_d, P_GQ), ("k", C_K, kT_d, P_GK)):
                for c in range(4):
                    p = fm_chunk(col0 + c * 128)
                    headnorm_store(p[:, :], [p], pl(gcol), dst, c * 128, tok0)

            for s in range(4):
                p = ps[3 + nxt("tm", 2)]
                for c in range(8):
                    kb.op("pe", lambda e: e.matmul(p[:, :], lhsT=hT[:, c, s * 128:(s + 1) * 128], rhs=win[:, c, C_V:C_V + 512], start=(c == 0), stop=(c == 7)),
                          r=[win, hT], w=[p], inc=(c == 7))
                kb.op("act", lambda e: e.activation(out=vb[:, s, :], in_=p[:, :], func=AF.Copy), r=[p], w=[vb])
                p2 = ps[3 + nxt("tm", 2)]
                for c in range(8):
                    kb.op("pe", lambda e: e.matmul(p2[:, 0:256], lhsT=hT[:, c, s * 128:(s + 1) * 128], rhs=win[:, c, C_RI:C_RI + 256], start=(c == 0), stop=(c == 7)),
                          r=[win, hT], w=[p2], inc=False)
                for c in range(8):
                    kb.op("pe", lambda e: e.matmul(p2[:, 256:264], lhsT=hT[:, c, s * 128:(s + 1) * 128], rhs=win[:, c, C_F:C_F + 8], start=(c == 0), stop=(c == 7)),
                          r=[win, hT], w=[p2], inc=(c == 7))
                kb.op("act", lambda e: e.activation(out=vi[:, s, :], in_=p2[:, 0:256], func=AF.Copy), r=[p2], w=[vi])
                blk = tb * 4 + s
                kb.op("dve", lambda e: e.tensor_tensor(out=small[:, 16:24], in0=p2[:, 256:264], in1=pl(P_FB, 8), op=ALU.add), r=[p2, par, vi], w=[small])
                kb.op("act", lambda e: e.activation(out=small[:, 24:32], in_=small[:, 16:24], func=AF.Abs), r=[small], w=[small])
                kb.op("act", lambda e: e.activation(out=small[:, 32:40], in_=small[:, 24:32], func=AF.Exp, scale=-1.0), r=[small], w=[small])
                kb.op("act", lambda e: e.activation(out=small[:, 40:48], in_=small[:, 32:40], func=AF.Ln, bias=onec[:, 0:1], scale=1.0), r=[small, onec], w=[small])
                kb.op("dve", lambda e: e.tensor_single_scalar(out=small[:, 48:56], in_=small[:, 16:24], scalar=0.0, op=ALU.min), r=[small], w=[small])
                kb.op("dve", lambda e: e.tensor_tensor(out=lsig[:, blk, :], in0=small[:, 48:56], in1=small[:, 40:48], op=ALU.subtract), r=[small], w=[lsig])
            kb.dma("sp", "st", v_d[tok0:tok0 + 512, :].rearrange("(s p) f -> p s f", p=128), vb[:, :, :], r=[vb], w=[v_d])

            for j in range(2):
                px = fm_chunk(C_CX + j * 128)
                t0 = fa[nxt("fa", 6)]
                kb.op("act", lambda e: e.activation(out=t0[:, :], in_=px[:, :], func=AF.Copy), r=[px], w=[t0])
                pc = fm_chunk(C_CC + j * 128)
                u = ucv[j]
                kb.op("dve", lambda e: e.tensor_tensor(out=u[:, 2:514], in0=t0[:, :], in1=pc[:, :], op=ALU.mult), r=[t0, pc], w=[u])
                t1 = fa[nxt("fa", 6)]
                kb.op("dve", lambda e: e.tensor_scalar(out=t1[:, :], in0=u[:, 0:512], scalar1=pl(P_CW + 0 * 2 + j), scalar2=None, op0=ALU.mult), r=[u, par], w=[t1])
                kb.op("dve", lambda e: e.scalar_tensor_tensor(out=t1[:, :], in0=u[:, 1:513], scalar=pl(P_CW + 1 * 2 + j), in1=t1[:, :], op0=ALU.mult, op1=ALU.add), r=[u, par, t1], w=[t1])
                kb.op("dve", lambda e: e.scalar_tensor_tensor(out=t1[:, :], in0=u[:, 2:514], scalar=pl(P_CW + 2 * 2 + j), in1=t1[:, :], op0=ALU.mult, op1=ALU.add), r=[u, par, t1], w=[t1])
                pbg = fm_chunk(C_CB + j * 128)
                kb.op("dve", lambda e: e.tensor_tensor(out=t0[:, :], in0=t1[:, :], in1=pbg[:, :], op=ALU.mult), r=[t1, pbg], w=[t0])
                kb.op("dve", lambda e: e.tensor_copy(out=small[:, 56:58], in_=u[:, 512:514]), r=[u], w=[small])
                kb.op("dve", lambda e: e.tensor_copy(out=u[:, 0:2], in_=small[:, 56:58]), r=[small], w=[u])
                headnorm_store(t0[:, :], [t0], pl(P_MG + 4 + j), yT_d, 512 + j * 128, tok0)

            for j in range(2):
                pf = fm_chunk(C_RF + j * 128)
                kb.op("act", lambda e: e.activation(out=sig[:, :], in_=pf[:, :], func=AF.Sigmoid), r=[pf], w=[sig])
                tf = fa[nxt("fa", 6)]
                kb.op("dve", lambda e: e.tensor_scalar(out=tf[:, :], in0=sig[:, :], scalar1=omlt[:, j:j + 1], scalar2=lbt[:, j:j + 1], op0=ALU.mult, op1=ALU.add), r=[sig, omlt, lbt], w=[tf])
                kb.op("dve", lambda e: e.tensor_single_scalar(out=tf[:, :], in_=tf[:, :], scalar=TINY, op=ALU.max), r=[tf], w=[tf])
                kb.op("act", lambda e: e.activation(out=tf[:, :], in_=tf[:, :], func=AF.Ln), r=[tf], w=[tf])
                kb.op("dve", lambda e: e.tensor_scalar(out=kk[:, :], in0=sig[:, :], scalar1=nomlt[:, j:j + 1], scalar2=omlt[:, j:j + 1], op0=ALU.mult, op1=ALU.add), r=[sig, omlt, nomlt], w=[kk])
                kb.op("dve", lambda e: e.tensor_tensor_scan(out=cc[:, :], data0=segm[:, :], data1=tf[:, :], initial=0.0, op0=ALU.mult, op1=ALU.add), r=[segm, tf], w=[cc])
                c3 = cc.t.ap().rearrange("p (n t) -> p n t", t=64)
                pq = fm_chunk(C_RQ + j * 128)
                kb.op("act", lambda e: e.activation(out=qs[:, :], in_=pq[:, :], func=AF.Silu), r=[pq], w=[qs])
                pg = fm_chunk(C_RG + j * 128)
                kb.op("act", lambda e: e.activation(out=gs[j][:, :], in_=pg[:, :], func=AF.Silu), r=[pg], w=[gs[j]])
                d1 = fa[nxt("fa", 6)]
                kb.op("dve", lambda e: e.tensor_tensor(out=d1.t.ap().rearrange("p (n t) -> p n t", t=64), in0=c3, in1=c3[:, :, 31:32].to_broadcast([128, 8, 64]), op=ALU.subtract), r=[cc], w=[d1])
                ex = fa[nxt("fa", 6)]
                kb.op("act", lambda e: e.activation(out=ex[:, :], in_=d1[:, :], func=AF.Exp), r=[d1], w=[ex])
                kb.op("dve", lambda e: e.tensor_tensor(out=qe[j][:, :], in0=qs[:, :], in1=ex[:, :], op=ALU.mult), r=[qs, ex], w=[qe[j]])
                kb.op("act", lambda e: e.activation(out=ex[:, :], in_=d1[:, :], func=AF.Exp, scale=-1.0), r=[d1], w=[ex])
                kb.op("dve", lambda e: e.tensor_tensor(out=ke[j][:, :], in0=kk[:, :], in1=ex[:, :], op=ALU.mult), r=[kk, ex], w=[ke[j]])
                kb.op("act", lambda e: e.activation(out=ex[:, :], in_=cc[:, :], func=AF.Exp), r=[cc], w=[ex])
                kb.op("dve", lambda e: e.tensor_tensor(out=qec[j][:, :], in0=qs[:, :], in1=ex[:, :], op=ALU.mult), r=[qs, ex], w=[qec[j]])
                kb.op("dve", lambda e: e.tensor_tensor(out=d1.t.ap().rearrange("p (n t) -> p n t", t=64), in0=c3[:, :, 63:64].to_broadcast([128, 8, 64]), in1=c3, op=ALU.subtract), r=[cc], w=[d1])
                kb.op("act", lambda e: e.activation(out=ex[:, :], in_=d1[:, :], func=AF.Exp), r=[d1], w=[ex])
                kb.op("dve", lambda e: e.tensor_tensor(out=kdT[:, :], in0=kk[:, :], in1=ex[:, :], op=ALU.mult), r=[kk, ex], w=[kdT])
                kb.op("act", lambda e: e.activation(out=ecl[:, j, :], in_=c3[:, :, 63], func=AF.Exp), r=[cc], w=[ecl])
                pT = ps[0]
                pTb = pT.t.ap().bitcast(BF16)
                for s in range(4):
                    kb.op("pe", lambda e: e.transpose(pTb[:, s * 128:(s + 1) * 128], kdT[:, s * 128:(s + 1) * 128], identb[:, :]), r=[kdT, identb], w=[pT], inc=(s == 3))
                kb.op("dve", lambda e: e.tensor_copy(out=kd[:, :, j * 128:(j + 1) * 128], in_=pTb[:, 0:512].rearrange("p (s f) -> p s f", f=128)), r=[pT], w=[kd])

            for pr in range(2):
                for dst in range(2):
                    kb.dma("sp", "cp", vi2.t.ap().rearrange("p (s two) f -> p s two f", two=2)[dst * 64:(dst + 1) * 64, :, pr, :], vi[pr * 64:(pr + 1) * 64, :, :], r=[vi], w=[vi2])
                    kb.dma("sp", "cp", kd2.t.ap().rearrange("p (s two) f -> p s two f", two=2)[dst * 64:(dst + 1) * 64, :, pr, :], kd[pr * 64:(pr + 1) * 64, :, :], r=[kd], w=[kd2])
            for ch in range(8):
                cols = slice(ch * 64, (ch + 1) * 64)
                for hh in range(2):
                    pb = hh * 64
                    ph = ps[6 + hh]
                    for j in range(2):
                        kb.op("pe", lambda e: e.matmul(ph[pb:pb + 64, j * 64:(j + 1) * 64], lhsT=ke[j][pb:pb + 64, cols], rhs=qe[j][pb:pb + 64, cols], start=True, stop=True, tile_position=(pb, pb)),
                              r=[ke[j], qe[j]], w=[ph], inc=(j == 1))
                for hh in range(2):
                    pb = hh * 64
                    ph = ps[6 + hh]
                    kb.op("dve", lambda e: e.tensor_tensor(out=atsb[pb:pb + 64, 0:128].rearrange("p (h t) -> p h t", t=64), in0=ph[pb:pb + 64, 0:128].rearrange("p (h t) -> p h t", t=64),
                                                           in1=cmaskb[pb:pb + 64, :].unsqueeze(1).to_broadcast([64, 2, 64]), op=ALU.mult), r=[ph, cmaskb], w=[atsb])
                for hh in range(2):
                    pb = hh * 64
                    ph = ps[6 + hh]
                    for j in range(2):
                        hd = j * 2 + hh
                        oslc = ph[pb:pb + 64, 128 + j * 64:128 + (j + 1) * 64]
                        kb.op("pe", lambda e: e.matmul(oslc, lhsT=vi2[pb:pb + 64, ch, hd * 64:(hd + 1) * 64], rhs=atsb[pb:pb + 64, j * 64:(j + 1) * 64], start=True, stop=False, tile_position=(pb, pb)),
                              r=[vi2, atsb], w=[ph], inc=False)
                        kb.op("pe", lambda e: e.matmul(oslc, lhsT=stateb[pb:pb + 64, j, :], rhs=qec[j][pb:pb + 64, cols], start=False, stop=True, tile_position=(pb, pb)),
                              r=[stateb, qec[j]], w=[ph], inc=False)
                        kb.op("pe", lambda e: e.matmul(ph[pb:pb + 64, 256 + j * 64:256 + (j + 1) * 64], lhsT=kd2[pb:pb + 64, ch, hd * 64:(hd + 1) * 64], rhs=vi2[pb:pb + 64, ch, hd * 64:(hd + 1) * 64], start=True, stop=True, tile_position=(pb, pb)),
                              r=[kd2, vi2], w=[ph], inc=(j == 1))
                kb.op("dve", lambda e: e.tensor_tensor(out=state[:, :, :], in0=state[:, :, :], in1=ecl[:, :, ch:ch + 1].to_broadcast([128, 2, 64]), op=ALU.mult), r=[state, ecl], w=[state])
                for hh in range(2):
                    pb = hh * 64
                    ph = ps[6 + hh]
                    kb.op("act", lambda e: e.activation(out=osb[pb:pb + 64, :, cols], in_=ph[pb:pb + 64, 128:256].rearrange("p (j t) -> p j t", t=64), func=AF.Copy), r=[ph], w=[osb])
                    kb.op("dve", lambda e: e.tensor_tensor(out=state[pb:pb + 64, :, :], in0=state[pb:pb + 64, :, :], in1=ph[pb:pb + 64, 256:384].rearrange("p (j v) -> p j v", v=64), op=ALU.add), r=[state, ph, osb], w=[state])
                kb.op("dve", lambda e: e.tensor_copy(out=stateb[:, :, :], in_=state[:, :, :]), r=[state], w=[stateb])
            for j in range(2):
                headnorm_store(osb[:, j, :], [osb], pl(P_MG + 6 + j), yT_d, 768 + j * 128, tok0, mul_b=gs[j])

        lflat = lsig.t.ap().rearrange("p n h -> p (n h)")
        NW = NS * 8
        tri = cst[:, K_TRI:K_TRI + 128]
        onesf = cst[:, K_ONES:K_ONES + 128]
        sel = cst[:, K_SEL:K_SEL + 128]
        pcs, ptot = ps[1], ps[2]
        kb.op("pe", lambda e: e.matmul(pcs[:, 0:NW], lhsT=tri, rhs=lflat, start=True, stop=True), r=[cst, lsig], w=[pcs])
        kb.op("pe", lambda e: e.matmul(ptot[:, 0:NW], lhsT=onesf, rhs=lflat, start=True, stop=True), r=[cst, lsig], w=[ptot])
        tot = fa[0]
        kb.op("act", lambda e: e.activation(out=tot[:, 0:NW], in_=ptot[:, 0:NW], func=AF.Copy), r=[ptot], w=[tot])
        kb.op("dve", lambda e: e.memset(cI[:, 0, :], 0.0), w=[cI])
        for b in range(1, NS):
            kb.op("dve", lambda e: e.tensor_tensor(out=cI[:, b, :], in0=cI[:, b - 1, :], in1=tot[:, (b - 1) * 8:b * 8], op=ALU.add), r=[cI, tot], w=[cI])
        dcum = fa[1]
        kb.op("dve", lambda e: e.tensor_tensor(out=dcum[:, 0:NW], in0=pcs[:, 0:NW], in1=cI.t.ap().rearrange("p n h -> p (n h)"), op=ALU.add), r=[pcs, cI], w=[dcum])
        kb.op("dve", lambda e: e.tensor_scalar(out=negd.t.ap().rearrange("p n h -> p (n h)"), in0=dcum[:, 0:NW], scalar1=-1.0, scalar2=None, op0=ALU.mult), r=[dcum], w=[negd])
        pbc = ps[3]
        kb.op("pe", lambda e: e.matmul(pbc[:, 0:NW], lhsT=sel, rhs=dcum[:, 0:NW], start=True, stop=True), r=[cst, dcum], w=[pbc])
        kb.op("dve", lambda e: e.tensor_scalar(out=cI.t.ap().rearrange("p n h -> p (n h)"), in0=pbc[:, 0:NW], scalar1=bshift[:, 0:1], scalar2=None, op0=ALU.subtract), r=[pbc, bshift], w=[cI])

        kb.barrier()
        if stop == ("A", l):
            kb.finish()
            return nc, dbg
        stackA.close()
        stackB = ExitStack()
        kb.stack = stackB
        obB = [kb.sbuf("obB%d_%d" % (l, i), [128, 512], BF16) for i in range(2)]
        kTc = kb.sbuf("kTc%d" % l, [128, T], BF16)
        qTc = kb.sbuf("qTc%d" % l, [128, T], BF16)
        vh = kb.sbuf("vh%d" % l, [128, NS, 128], BF16)
        pTt = [kb.sbuf("pTt%d_%d" % (l, i), [128, 512], BF16) for i in range(3)]
        biasb = [kb.sbuf("biasb%d_%d" % (l, i), [128, NS], F32) for i in range(2)]
        rden = kb.sbuf("rden%d" % l, [128, 512], F32)
        on = kb.sbuf("on%d" % l, [128, 512], F32)
        rotb = {"s": 0, "p": 0, "b": 0, "o": 0, "ob": 0}

        def nxb(name, n):
            v = rotb[name]
            rotb[name] = (v + 1) % n
            return v

        for c in range(4):
            kb.dma("sp", "ld", kTc[:, :], kT_d[c * 128:(c + 1) * 128, :], r=[kT_d], w=[kTc])
            kb.dma("sp", "ld", qTc[:, :], qT_d[c * 128:(c + 1) * 128, :], r=[qT_d], w=[qTc])
            kb.dma("sp", "ld", vh[:, :, :], v_d[:, c * 128:(c + 1) * 128].rearrange("(n p) f -> p n f", p=128), r=[v_d], w=[vh])
            for I in range(NB):
                oi = nxb("o", 2)
                pO, pD = ps[3 + oi], ps[5 + oi]
                nJ = 4 * (I + 1)
                for hh in range(2):
                    pb = hh * 64
                    h = 2 * c + hh
                    bb = biasb[nxb("b", 2)]
                    kb.op("dve", lambda e: e.tensor_scalar(out=bb[:, 0:nJ], in0=negd[:, 0:nJ, h], scalar1=cI[:, 4 * I + 1, h:h + 1], scalar2=None, op0=ALU.add), r=[negd, cI], w=[bb])
                    def emit_s(J):
                        pS = ps[nxb("s", 3)]
                        kb.op("pe", lambda e: e.matmul(pS[:, :], lhsT=kTc[pb:pb + 64, J * 128:(J + 1) * 128], rhs=qTc[pb:pb + 64, I * 512:(I + 1) * 512], start=True, stop=True, tile_position=(pb, 0)),
                              r=[kTc, qTc], w=[pS])
                        return pS

                    def consume(J, pS):
                        pt = pTt[nxb("p", 3)]
                        kb.op("act", lambda e: e.activation(out=pt[:, :], in_=pS[:, :], func=AF.Exp, bias=bb[:, J:J + 1], scale=0.125), r=[pS, bb], w=[pt])
                        if J >= 4 * I:
                            kb.op("dve", lambda e: e.tensor_tensor(out=pt[:, :], in0=pt[:, :], in1=dmaskb[:, J - 4 * I, :], op=ALU.mult), r=[pt, dmaskb], w=[pt])
                        kb.op("pe", lambda e: e.matmul(pO[pb:pb + 64, :], lhsT=vh[:, J, pb:pb + 64], rhs=pt[:, :], start=(J == 0), stop=(J == nJ - 1), tile_position=(0, pb)), r=[vh, pt], w=[pO], inc=False)
                        kb.op("pe", lambda e: e.matmul(pD[pb:pb + 64, :], lhsT=onesb[:, 0:64], rhs=pt[:, :], start=(J == 0), stop=(J == nJ - 1), tile_position=(0, pb)), r=[onesb, pt], w=[pD])

                    pendq = []
                    for J in range(nJ):
                        pendq.append((J, emit_s(J)))
                        if len(pendq) > 2:
                            consume(*pendq.pop(0))
                    while pendq:
                        consume(*pendq.pop(0))
                kb.op("dve", lambda e: e.reciprocal(out=rden[:, :], in_=pD[:, :]), r=[pD], w=[rden])
                kb.op("dve", lambda e: e.tensor_tensor(out=on[:, :], in0=pO[:, :], in1=rden[:, :], op=ALU.mult), r=[pO, rden], w=[on])
                sq = pTt[nxb("p", 3)]
                kb.op("act", lambda e: e.activation(out=sq[:, :], in_=on[:, :], func=AF.Square), r=[on], w=[sq])
                pm = ps[7]
                kb.op("pe", lambda e: e.matmul(pm[:, :], lhsT=blkb[:, :], rhs=sq[:, :], start=True, stop=True), r=[blkb, sq], w=[pm])
                rstd_from_ms(pm[:, :], [pm], rden, rden[:, :], rden, rden[:, :])
                o = obB[nxb("ob", 2)]
                kb.op("dve", lambda e: e.scalar_tensor_tensor(out=o[:, :], in0=on[:, :], scalar=pl(P_MG + c), in1=rden[:, :], op0=ALU.mult, op1=ALU.mult), r=[on, rden, par], w=[o])
                kb.dma("sp", "st", yT_d[c * 128:(c + 1) * 128, I * 512:(I + 1) * 512], o[:, :], r=[o], w=[yT_d])

        kb.barrier()
        if stop == ("B", l):
            kb.finish()
            return nc, dbg
        stackB.close()
        stackC = ExitStack()
        kb.stack = stackC
        moe = (l % 2 == 1)
        junk = kb.sbuf("junkC%d" % l, [128, D], F32)
        h2b = [kb.sbuf("h2b%d_%d" % (l, i), [128, D], BF16) for i in range(2)]
        stc = kb.sbuf("stc%d" % l, [128, 2 * NSC], F32)
        wgb = [kb.sbuf("wgb%d_%d" % (l, i), [128, 8, 512], BF16) for i in range(2)]
        wub = [kb.sbuf("wub%d_%d" % (l, i), [128, 8, 512], BF16) for i in range(2)]
        wdb = [kb.sbuf("wdb%d_%d" % (l, i), [128, 4, D], BF16) for i in range(2)]
        sgt = [kb.sbuf("sgt%d_%d" % (l, i), [128, 512], BF16) for i in range(2)]
        if moe:
            wrb = kb.sbuf("wrb%d" % l, [128, 8, NE], BF16)
            kb.dma("pool", "w", wrb[:, :, :], wr_d[0].rearrange("(c p) e -> p c e", p=128), w=[wrb])
            rt = kb.sbuf("rt%d" % l, [128, 64], F32)
            trib = kb.sbuf("trib%d" % l, [128, 128], BF16)
            kb.op("dve", lambda e: e.tensor_copy(out=trib[:, :], in_=cst[:, K_TRI:K_TRI + 128]), r=[cst], w=[trib])
        stackC1 = ExitStack()
        kb.stack = stackC1
        wout = kb.sbuf("wout%d" % l, [128, 8, D], BF16)
        for c in range(8):
            kb.dma("pool", "w", wout[:, c, :], w_out_d[l, c * 128:(c + 1) * 128, :], w=[wout])
        x1 = kb.sbuf("x1_%d" % l, [128, NSC, D], F32)
        yTb = kb.sbuf("yTb%d" % l, [128, 8, TBC], BF16)
        h2T = yTb
        hact = kb.sbuf("hact%d" % l, [128, 4, TBC], BF16)
        rc = {"a": 0, "g": 0, "u": 0, "w": 0, "h": 0, "sg": 0}

        def nxc(name, n):
            v = rc[name]
            rc[name] = (v + 1) % n
            return v

        passes = [("full", T // TBC)] if not moe else [("c1", T // TBC)]
        for mode, tbc in [(m_, t_) for m_, n_ in passes for t_ in range(n_)]:
            tok0 = tbc * TBC
            if mode != "c2":
                kb.dma("sp", "ld", x1[:, :, :], x_src[tok0:tok0 + TBC, :].rearrange("(s p) d -> p s d", p=128), r=[x_src], w=[x1])
                kb.dma("sp", "ld", yTb[:, :, :], yT_d[:, tok0:tok0 + TBC].rearrange("(c p) t -> p c t", p=128), r=[yT_d], w=[yTb])
            for s in (range(NSC) if mode != "c2" else []):
                for half in range(2):
                    p = ps[1 + nxc("a", 2)]
                    for c in range(8):
                        kb.op("pe", lambda e: e.matmul(p[:, :], lhsT=yTb[:, c, s * 128:(s + 1) * 128], rhs=wout[:, c, half * 512:(half + 1) * 512], start=(c == 0), stop=(c == 7)),
                              r=[yTb, wout], w=[p], inc=(c == 7))
                    kb.op("dve", lambda e: e.tensor_tensor(out=x1[:, s, half * 512:(half + 1) * 512], in0=x1[:, s, half * 512:(half + 1) * 512], in1=p[:, :], op=ALU.add), r=[x1, p], w=[x1])
            if mode != "c2":
                for s in range(NSC):
                    kb.op("act", lambda e: e.activation(out=junk[:, :], in_=x1[:, s, :], func=AF.Square, accum_out=stc[:, s:s + 1]), r=[x1], w=[junk, stc])
                rstd_tm(kb, stc, epsc, NSC)
            for s in range(NSC):
                hbb = h2b[nxc("h", 2)]
                if mode != "c2":
                    kb.op("act", lambda e: e.activation(out=hbb[:, :], in_=x1[:, s, :], func=AF.Identity, scale=stc[:, NSC + s:NSC + s + 1]), r=[x1, stc], w=[hbb])
                if mode == "c1":
                    kb.dma("sp", "st", h2_d[tok0 + s * 128:tok0 + (s + 1) * 128, :], hbb[:, :], r=[hbb], w=[h2_d])
                    continue
                if mode == "c2":
                    ic = tbc * NSC + s
                    kb.gather("g", x1[:, s, :], x1_d.t.ap(), idxt[:, ic:ic + 1], r=[x1_d, idxt], w=[x1])
                    kb.gather("g", hbb[:, :], h2_d.t.ap(), idxt[:, ic:ic + 1], r=[h2_d, idxt], w=[hbb])
                pT = ps[0]
                pTb = pT.t.ap().bitcast(BF16)
                for c in range(8):
                    kb.op("pe", lambda e: e.transpose(pTb[:, c * 128:(c + 1) * 128], hbb[:, c * 128:(c + 1) * 128], identb[:, :]), r=[hbb, identb], w=[pT], inc=(c == 7))
                kb.op("dve", lambda e: e.tensor_tensor(out=h2T[:, :, s * 128:(s + 1) * 128], in0=pTb[:, 0:1024].rearrange("p (c t) -> p c t", t=128),
                                                       in1=pl(P_G2, 8).unsqueeze(2).to_broadcast([128, 8, 128]), op=ALU.mult), r=[pT, par], w=[h2T])
            if mode == "c1":
                kb.dma("sp", "st", x1_d[tok0:tok0 + TBC, :].rearrange("(s p) d -> p s d", p=128), x1[:, :, :], r=[x1], w=[x1_d])
                continue
            if moe:
                for s in range(NSC):
                    p = ps[1 + nxc("a", 2)]
                    for c in range(8):
                        kb.op("pe", lambda e: e.matmul(p[:, 0:NE], lhsT=h2T[:, c, s * 128:(s + 1) * 128], rhs=wrb[:, c, :], start=(c == 0), stop=(c == 7)), r=[h2T, wrb], w=[p], inc=(c == 7))
                    kb.op("dve", lambda e: e.tensor_tensor(out=rt[:, 0:8], in0=p[:, 0:NE], in1=pl(P_RB, 8), op=ALU.add), r=[p, par], w=[rt])
                    kb.op("dve", lambda e: e.max(out=rt[:, 8:16], in_=rt[:, 0:8]), r=[rt], w=[rt])
                    kb.op("dve", lambda e: e.tensor_tensor(out=rt[:, 16:17], in0=rt[:, 8:9], in1=rt[:, 9:10], op=ALU.subtract), r=[rt], w=[rt])
                    kb.op("act", lambda e: e.activation(out=rt[:, 17:18], in_=rt[:, 16:17], func=AF.Sigmoid), r=[rt], w=[rt])
                    kb.op("act", lambda e: e.activation(out=rt[:, 18:19], in_=rt[:, 16:17], func=AF.Sigmoid, scale=-1.0), r=[rt], w=[rt])
                    kb.op("dve", lambda e: e.tensor_scalar(out=rt[:, 24:32], in0=rt[:, 0:8], scalar1=rt[:, 8:9], scalar2=rt[:, 17:18], op0=ALU.is_equal, op1=ALU.mult), r=[rt], w=[rt])
                    kb.op("dve", lambda e: e.tensor_scalar(out=rt[:, 32:40], in0=rt[:, 0:8], scalar1=rt[:, 9:10], scalar2=rt[:, 18:19], op0=ALU.is_equal, op1=ALU.mult), r=[rt], w=[rt])
                    kb.op("dve", lambda e: e.tensor_tensor(out=gates[:, s, :], in0=rt[:, 24:32], in1=rt[:, 32:40], op=ALU.add), r=[rt], w=[gates])
            experts = range(NE) if moe else [None]
            for ex in experts:
                if ex is None:
                    Wg, Wu, Wd = ffn_g_d.t.ap()[0], ffn_u_d.t.ap()[0], ffn_d_d.t.ap()[0]
                    wbufs = [ffn_g_d, ffn_u_d, ffn_d_d]
                else:
                    Wg, Wu, Wd = moe_g_d.t.ap()[0, ex], moe_u_d.t.ap()[0, ex], moe_d_d.t.ap()[0, ex]
                    wbufs = [moe_g_d, moe_u_d, moe_d_d]
                for fg in range(DFF // 512):
                    wi = nxc("w", 2)
                    kb.dma("pool", "w", wgb[wi][:, :, :], Wg[:, fg * 512:(fg + 1) * 512].rearrange("(c p) f -> p c f", p=128), w=[wgb[wi]])
                    kb.dma("pool", "w", wub[wi][:, :, :], Wu[:, fg * 512:(fg + 1) * 512].rearrange("(c p) f -> p c f", p=128), w=[wub[wi]])
                    kb.dma("pool", "w", wdb[wi][:, :, :], Wd[fg * 512:(fg + 1) * 512, :].rearrange("(k p) d -> p k d", p=128), w=[wdb[wi]])
                    for t4 in range(TBC // 512):
                        tsl = slice(t4 * 512, (t4 + 1) * 512)
                        for k in range(4):
                            pg = ps[3 + nxc("g", 2)]
                            pu = ps[5 + nxc("u", 2)]
                            for c in range(8):
                                kb.op("pe", lambda e: e.matmul(pg[:, :], lhsT=wgb[wi][:, c, k * 128:(k + 1) * 128], rhs=h2T[:, c, tsl], start=(c == 0), stop=(c == 7)), r=[wgb[wi], h2T], w=[pg], inc=(c == 7))
                            for c in range(8):
                                kb.op("pe", lambda e: e.matmul(pu[:, :], lhsT=wub[wi][:, c, k * 128:(k + 1) * 128], rhs=h2T[:, c, tsl], start=(c == 0), stop=(c == 7)), r=[wub[wi], h2T], w=[pu], inc=(c == 7))
                            sg = sgt[nxc("sg", 2)]
                            kb.op("act", lambda e: e.activation(out=sg[:, :], in_=pg[:, :], func=AF.Silu), r=[pg], w=[sg])
                            kb.op("dve", lambda e: e.tensor_tensor(out=hact[:, k, tsl], in0=sg[:, :], in1=pu[:, :], op=ALU.mult), r=[sg, pu], w=[hact])
                    for s in range(NSC):
                        for half in range(2):
                            p = ps[1 + nxc("a", 2)]
                            for k in range(4):
                                kb.op("pe", lambda e: e.matmul(p[:, :], lhsT=hact[:, k, s * 128:(s + 1) * 128], rhs=wdb[wi][:, k, half * 512:(half + 1) * 512], start=(k == 0), stop=(k == 3)),
                                      r=[hact, wdb[wi]], w=[p], inc=(k == 3))
                            xs = x1[:, s, half * 512:(half + 1) * 512]
                            if ex is None:
                                kb.op("dve", lambda e: e.tensor_tensor(out=xs, in0=xs, in1=p[:, :], op=ALU.add), r=[x1, p], w=[x1])
                            else:
                                kb.op("dve", lambda e: e.scalar_tensor_tensor(out=xs, in0=p[:, :], scalar=gates[:, s, ex:ex + 1], in1=xs, op0=ALU.mult, op1=ALU.add), r=[x1, p, gates], w=[x1])
            kb.dma("sp", "st", x_dst[tok0:tok0 + TBC, :].rearrange("(s p) d -> p s d", p=128), x1[:, :, :], r=[x1], w=[x_dst])
        if moe:
            kb.barrier()
            stackC1.close()
            stackR = ExitStack()
            kb.stack = stackR
            NJ = TH // 128
            CAP = ((TH * 2 // NE) * 3 // 2 + 511) // 512 * 512
            NSR = CAP // 128
            xdisp_d = kb.dram("xdisp_s", [NE * CAP, D], BF16)
            ydisp_d = kb.dram("ydisp_s", [NE * CAP, D], F32)
            h2R = kb.sbuf("h2R", [128, 8, CAP], BF16)
            hactR = kb.sbuf("hactR", [128, 4, CAP], BF16)
            yacc = kb.sbuf("yacc", [128, NSR, D], F32)
            slots = kb.sbuf("slots", [128, NJ, 2], I32)
            wts = kb.sbuf("wts", [128, NJ, 2], F32)
            basec = kb.sbuf("basec", [128, NE], F32)
            eoff = kb.sbuf("eoff", [128, NE], F32)
            selb = kb.sbuf("selb", [128, NE], BF16)
            xa = kb.sbuf("xa", [128, D], F32)
            y1 = kb.sbuf("y1", [128, D], F32)
            y2 = kb.sbuf("y2", [128, D], F32)
            kb.op("dve", lambda e: e.memset(basec[:, :], 0.0), w=[basec])
            for ex in range(NE):
                kb.op("dve", lambda e: e.memset(eoff[:, ex:ex + 1], float(ex * CAP)), w=[eoff])

            def transposed(hbb, dst_ap):
                pT = ps[0]
                pTb = pT.t.ap().bitcast(BF16)
                for c in range(8):
                    kb.op("pe", lambda e: e.transpose(pTb[:, c * 128:(c + 1) * 128], hbb[:, c * 128:(c + 1) * 128], identb[:, :]), r=[hbb, identb], w=[pT], inc=(c == 7))
                kb.op("dve", lambda e: e.tensor_tensor(out=dst_ap, in0=pTb[:, 0:1024].rearrange("p (c t) -> p c t", t=128),
                                                       in1=pl(P_G2, 8).unsqueeze(2).to_broadcast([128, 8, 128]), op=ALU.mult), r=[pT, par], w=[h2R])

            for j in range(NJ):
                hi = nxc("h", 2)
                hbb = h2b[hi]
                kb.gather("gh%d" % hi, hbb[:, :], h2_d.t.ap(), idxt[:, j:j + 1], r=[h2_d, idxt], w=[hbb])
                transposed(hbb, h2R[:, :, 0:128])
                p = ps[1 + nxc("a", 2)]
                for c in range(8):
                    kb.op("pe", lambda e: e.matmul(p[:, 0:NE], lhsT=h2R[:, c, 0:128], rhs=wrb[:, c, :], start=(c == 0), stop=(c == 7)), r=[h2R, wrb], w=[p], inc=(c == 7))
                kb.op("dve", lambda e: e.tensor_tensor(out=rt[:, 0:8], in0=p[:, 0:NE], in1=pl(P_RB, 8), op=ALU.add), r=[p, par], w=[rt])
                kb.op("dve", lambda e: e.max(out=rt[:, 8:16], in_=rt[:, 0:8]), r=[rt], w=[rt])
                kb.op("dve", lambda e: e.tensor_tensor(out=rt[:, 16:17], in0=rt[:, 8:9], in1=rt[:, 9:10], op=ALU.subtract), r=[rt], w=[rt])
                kb.op("act", lambda e: e.activation(out=wts[:, j, 0:1], in_=rt[:, 16:17], func=AF.Sigmoid), r=[rt], w=[wts])
                kb.op("act", lambda e: e.activation(out=wts[:, j, 1:2], in_=rt[:, 16:17], func=AF.Sigmoid, scale=-1.0), r=[rt], w=[wts])
                kb.op("dve", lambda e: e.tensor_scalar(out=rt[:, 24:32], in0=rt[:, 0:8], scalar1=rt[:, 8:9], scalar2=None, op0=ALU.is_equal), r=[rt], w=[rt])
                kb.op("dve", lambda e: e.tensor_scalar(out=rt[:, 32:40], in0=rt[:, 0:8], scalar1=rt[:, 9:10], scalar2=None, op0=ALU.is_equal), r=[rt], w=[rt])
                kb.op("dve", lambda e: e.tensor_tensor(out=rt[:, 40:48], in0=rt[:, 24:32], in1=rt[:, 32:40], op=ALU.add), r=[rt], w=[rt])
                kb.op("dve", lambda e: e.tensor_copy(out=selb[:, :], in_=rt[:, 40:48]), r=[rt], w=[selb])
                pp, ptot = ps[3], ps[5]
                kb.op("pe", lambda e: e.matmul(pp[:, 0:NE], lhsT=trib[:, :], rhs=selb[:, :], start=True, stop=True), r=[trib, selb], w=[pp])
                kb.op("pe", lambda e: e.matmul(ptot[:, 0:NE], lhsT=onesb[:, :], rhs=selb[:, :], start=True, stop=True), r=[onesb, selb], w=[ptot])
                kb.op("dve", lambda e: e.tensor_tensor(out=rt[:, 48:56], in0=pp[:, 0:NE], in1=rt[:, 40:48], op=ALU.subtract), r=[pp, rt], w=[rt])
                kb.op("dve", lambda e: e.tensor_tensor(out=rt[:, 48:56], in0=rt[:, 48:56], in1=basec[:, :], op=ALU.add), r=[rt, basec], w=[rt])
                kb.op("dve", lambda e: e.tensor_tensor(out=rt[:, 48:56], in0=rt[:, 48:56], in1=eoff[:, :], op=ALU.add), r=[rt, eoff], w=[rt])
                kb.op("dve", lambda e: e.tensor_tensor(out=basec[:, :], in0=basec[:, :], in1=ptot[:, 0:NE], op=ALU.add), r=[basec, ptot], w=[basec])
                for k in range(2):
                    kb.op("dve", lambda e: e.tensor_tensor(out=rt[:, 56:64], in0=rt[:, 24 + 8 * k:32 + 8 * k], in1=rt[:, 48:56], op=ALU.mult), r=[rt], w=[rt])
                    kb.op("dve", lambda e: e.tensor_reduce(out=rt[:, 20 + k:21 + k], in_=rt[:, 56:64], axis=mybir.AxisListType.X, op=ALU.add), r=[rt], w=[rt])
                kb.op("dve", lambda e: e.tensor_copy(out=slots[:, j, :], in_=rt[:, 20:22]), r=[rt], w=[slots])
                for k in range(2):
                    kb.ncall += 1
                    kb._wait("pool", kb._deps([hbb, slots], [xdisp_d]))
                    inst = nc.gpsimd.indirect_dma_start(out=xdisp_d.t.ap(), out_offset=bass.IndirectOffsetOnAxis(slots[:, j, k:k + 1], 0), in_=hbb[:, :], in_offset=None)
                    sk = "d:sc%d" % hi
                    if sk not in kb.sems:
                        kb._newsem(sk)
                    kb.cnt[sk] += 16
                    inst.then_inc(kb.sems[sk], 16)
                    kb._mark(sk, kb.cnt[sk], [hbb, slots], [xdisp_d])

            for ex in range(NE):
                for s in range(NSR):
                    hi = nxc("h", 2)
                    hbb = h2b[hi]
                    kb.dma("sp", "lh%d" % hi, hbb[:, :], xdisp_d[ex * CAP + s * 128:ex * CAP + (s + 1) * 128, :], r=[xdisp_d], w=[hbb])
                    transposed(hbb, h2R[:, :, s * 128:(s + 1) * 128])
                Wg, Wu, Wd = moe_g_d.t.ap()[0, ex], moe_u_d.t.ap()[0, ex], moe_d_d.t.ap()[0, ex]
                for fg in range(DFF // 512):
                    wi = nxc("w", 2)
                    kb.dma("pool", "w", wgb[wi][:, :, :], Wg[:, fg * 512:(fg + 1) * 512].rearrange("(c p) f -> p c f", p=128), w=[wgb[wi]])
                    kb.dma("pool", "w", wub[wi][:, :, :], Wu[:, fg * 512:(fg + 1) * 512].rearrange("(c p) f -> p c f", p=128), w=[wub[wi]])
                    kb.dma("pool", "w", wdb[wi][:, :, :], Wd[fg * 512:(fg + 1) * 512, :].rearrange("(k p) d -> p k d", p=128), w=[wdb[wi]])
                    for t4 in range(CAP // 512):
                        tsl = slice(t4 * 512, (t4 + 1) * 512)
                        for k in range(4):
                            pg = ps[3 + nxc("g", 2)]
                            pu = ps[5 + nxc("u", 2)]
                            for c in range(8):
                                kb.op("pe", lambda e: e.matmul(pg[:, :], lhsT=wgb[wi][:, c, k * 128:(k + 1) * 128], rhs=h2R[:, c, tsl], start=(c == 0), stop=(c == 7)), r=[wgb[wi], h2R], w=[pg], inc=(c == 7))
                            for c in range(8):
                                kb.op("pe", lambda e: e.matmul(pu[:, :], lhsT=wub[wi][:, c, k * 128:(k + 1) * 128], rhs=h2R[:, c, tsl], start=(c == 0), stop=(c == 7)), r=[wub[wi], h2R], w=[pu], inc=(c == 7))
                            sg = sgt[nxc("sg", 2)]
                            kb.op("act", lambda e: e.activation(out=sg[:, :], in_=pg[:, :], func=AF.Silu), r=[pg], w=[sg])
                            kb.op("dve", lambda e: e.tensor_tensor(out=hactR[:, k, tsl], in0=sg[:, :], in1=pu[:, :], op=ALU.mult), r=[sg, pu], w=[hactR])
                    for s in range(NSR):
                        for half in range(2):
                            p = ps[1 + nxc("a", 2)]
                            for k in range(4):
                                kb.op("pe", lambda e: e.matmul(p[:, :], lhsT=hactR[:, k, s * 128:(s + 1) * 128], rhs=wdb[wi][:, k, half * 512:(half + 1) * 512], start=(k == 0), stop=(k == 3)),
                                      r=[hactR, wdb[wi]], w=[p], inc=(k == 3))
                            ys = yacc[:, s, half * 512:(half + 1) * 512]
                            if fg == 0:
                                kb.op("dve", lambda e: e.tensor_copy(out=ys, in_=p[:, :]), r=[p], w=[yacc])
                            else:
                                kb.op("dve", lambda e: e.tensor_tensor(out=ys, in0=ys, in1=p[:, :], op=ALU.add), r=[yacc, p], w=[yacc])
                kb.dma("sp", "st", ydisp_d[ex * CAP:(ex + 1) * CAP, :].rearrange("(s p) d -> p s d", p=128), yacc[:, :, :], r=[yacc], w=[ydisp_d])

            for j in range(NJ):
                kb.gather("gx", xa[:, :], x1_d.t.ap(), idxt[:, j:j + 1], r=[x1_d, idxt], w=[xa])
                kb.gather("gy1", y1[:, :], ydisp_d.t.ap(), slots[:, j, 0:1], r=[ydisp_d, slots], w=[y1])
                kb.gather("gy2", y2[:, :], ydisp_d.t.ap(), slots[:, j, 1:2], r=[ydisp_d, slots], w=[y2])
                kb.op("dve", lambda e: e.scalar_tensor_tensor(out=xa[:, :], in0=y1[:, :], scalar=wts[:, j, 0:1], in1=xa[:, :], op0=ALU.mult, op1=ALU.add), r=[xa, y1, wts], w=[xa])
                kb.op("dve", lambda e: e.scalar_tensor_tensor(out=xa[:, :], in0=y2[:, :], scalar=wts[:, j, 1:2], in1=xa[:, :], op0=ALU.mult, op1=ALU.add), r=[xa, y2, wts], w=[xa])
                kb.dma("sp", "st", yout[j * 128:(j + 1) * 128, :], xa[:, :], r=[xa], w=[yout])
            kb.barrier()
            stackR.close()
        else:
            kb.barrier()
            stackC1.close()
        kb.stack = stackC
        kb.barrier()
        stackC.close()
        kb.stack = None

    kb.finish()
    return nc, dbg


def rstd_tm(kb, st, epsc, n):
    kb.op("act", lambda e: e.activation(out=st[:, n:2 * n], in_=st[:, 0:n], func=AF.Ln, bias=epsc[:, 0:1], scale=1.0 / D), r=[st, epsc], w=[st])
    kb.op("act", lambda e: e.activation(out=st[:, n:2 * n], in_=st[:, n:2 * n], func=AF.Exp, scale=-0.5), r=[st], w=[st])


_CACHE = {}


def kernel(**inputs):
    x = np.ascontiguousarray(inputs["x"], dtype=np.float32)
    B, S, _ = x.shape
    key = S
    if key not in _CACHE:
        _CACHE[key] = build(S)[0]
    nc = _CACHE[key]
    consts = make_consts()
    params = make_params(inputs)
    shared = {k: np.ascontiguousarray(inputs[k], dtype=np.float32) for k in
              ("w_in", "w_out", "ffn_w_gate", "ffn_w_up", "ffn_w_down", "moe_router_w", "moe_w_gate", "moe_w_up", "moe_w_down")}
    n = 8
    TH = S // 2
    in_maps = []
    for cid in range(n):
        m = dict(shared)
        m["x"] = x[cid % B]
        m["consts"] = consts
        m["params"] = params
        rank = cid // B
        m["tokidx"] = (rank * TH + np.arange(TH, dtype=np.int32)).reshape(TH // 128, 128).T.copy()
        in_maps.append(m)
    res = run_bass_kernel_spmd(nc, in_maps, core_ids=list(range(n)))
    out = np.stack([np.concatenate([res.results[b]["y"], res.results[b + B]["y"]], axis=0) for b in range(B)], axis=0)
    return out.astype(np.float32)
```

```python
import numpy as np
from contextlib import ExitStack
import concourse.bass as bass
import concourse.mybir as mybir
from concourse.bass_utils import run_bass_kernel_spmd

F32 = mybir.dt.float32
BF16 = mybir.dt.bfloat16
I32 = mybir.dt.int32
AF = mybir.ActivationFunctionType
ALU = mybir.AluOpType

D = 1024
DIN = 3336
DFF = 3584
NE = 8
EPS = 1e-6
TINY = 1e-30
C_Q, C_K, C_V, C_F = 0, 512, 1024, 1536
C_CX, C_CB, C_CC = 1544, 1800, 2056
C_RQ, C_RF, C_RI, C_RG = 2312, 2568, 2824, 3080

K_ID, K_BLK, K_TRI, K_ONES, K_SEL = 0, 128, 256, 384, 512
K_DM = 640
K_CM = K_DM + 2048
K_SEG = K_CM + 64
K_SWAP = K_SEG + 512
NCONST = K_SWAP + 128

P_G1, P_G2, P_GQ, P_GK, P_CW, P_LB, P_MG, P_FB, P_RB, P_GQR, P_GKR = 0, 8, 16, 17, 18, 24, 28, 36, 44, 52, 116
NPAR = 180


def make_consts():
    c = np.zeros((128, NCONST), np.float32)
    p = np.arange(128)
    c[:, K_ID:K_ID + 128] = np.eye(128)
    c[:, K_BLK:K_BLK + 128] = ((p[:, None] // 64) == (p[None, :] // 64)) / 64.0
    c[:, K_TRI:K_TRI + 128] = (p[:, None] <= p[None, :])
    c[:, K_ONES:K_ONES + 128] = 1.0
    c[127, K_SEL:K_SEL + 128] = 1.0
    q = np.arange(512)
    for j in range(4):
        c[:, K_DM + j * 512:K_DM + (j + 1) * 512] = ((j * 128 + p[:, None]) <= q[None, :])
    t = np.arange(64)
    c[:, K_CM:K_CM + 64] = ((p[:, None] % 64) <= t[None, :])
    c[:, K_SEG:K_SEG + 512] = ((q % 64) != 0)[None, :]
    c[:, K_SWAP:K_SWAP + 128] = (p[:, None] == ((p[None, :] + 64) % 128))
    return c


def make_params(inp):
    L = 2
    P = np.zeros((L, 128, NPAR), np.float32)
    for l in range(L):
        P[l, :, P_G1:P_G1 + 8] = inp["norm_mix"][l].reshape(8, 128).T
        P[l, :, P_G2:P_G2 + 8] = inp["norm_ffn"][l].reshape(8, 128).T
        P[l, :, P_GQ] = np.tile(inp["q_norm_gain"][l], 2)
        P[l, :, P_GK] = np.tile(inp["k_norm_gain"][l], 2)
        P[l, :, P_CW:P_CW + 6] = inp["conv_w"][l].reshape(3, 2, 128).transpose(2, 0, 1).reshape(128, 6)
        P[l, :, P_LB:P_LB + 4] = inp["hgrn_lb_logits"].reshape(2, 2, 128).transpose(2, 0, 1).reshape(128, 4)
        P[l, :, P_MG:P_MG + 8] = inp["mix_out_gain"][l].reshape(8, 128).T
        P[l, :, P_FB:P_FB + 8] = inp["attn_f_bias"][l][None, :]
        P[l, :, P_RB:P_RB + 8] = inp["moe_router_b"][0][None, :]
        P[l, :, P_GQR:P_GQR + 64] = inp["q_norm_gain"][l][None, :]
        P[l, :, P_GKR:P_GKR + 64] = inp["k_norm_gain"][l][None, :]
    return P


class Buf:
    __slots__ = ("t", "w", "r", "nowaw")

    def __init__(self, t, nowaw=False):
        self.t = t
        self.w = {}
        self.r = {}
        self.nowaw = nowaw

    def __getitem__(self, idx):
        return self.t.ap()[idx]


class KB:
    def __init__(self, nc):
        self.nc = nc
        self.eng = {"pe": nc.tensor, "act": nc.scalar, "dve": nc.vector, "pool": nc.gpsimd, "sp": nc.sync}
        self.sems = {}
        self.cnt = {}
        self.waited = {e: {} for e in self.eng}
        self.nsem = 0
        self.stack = None
        self.ncall = 0
        import os as _os
        self.noself = _os.environ.get('NOSELF', '0') == '1'
        import os
        self.cut = int(os.environ['KCUT']) if 'KCUT' in os.environ else None
        for e in self.eng:
            self._newsem(e)

    def _newsem(self, key):
        self.nsem += 1
        self.sems[key] = self.nc.alloc_semaphore(name="s%d_%s" % (self.nsem, key.replace(":", "_")))
        self.cnt[key] = 0

    def sbuf(self, name, shape, dt):
        if self.stack is not None:
            return Buf(self.stack.enter_context(self.nc.sbuf_tensor(name, list(shape), dt)))
        return Buf(self.nc.alloc_sbuf_tensor(name, list(shape), dt))

    def psum(self, name, shape, dt):
        return Buf(self.nc.alloc_psum_tensor(name, list(shape), dt))

    def dram(self, name, shape, dt, kind=None):
        if kind is None:
            t = self.nc.dram_tensor(name, list(shape), dt)
        else:
            t = self.nc.dram_tensor(name, list(shape), dt, kind=kind)
        return Buf(t, nowaw=True)

    def _wait(self, en, toks):
        e = self.eng[en]
        wd = self.waited[en]
        for k, v in toks.items():
            if en == "pe" and k == "pe":
                continue
            if self.noself and k == en:
                continue
            if wd.get(k, 0) < v:
                e.wait_ge(self.sems[k], v)
                wd[k] = v

    def _deps(self, r, w):
        toks = {}
        for b in r:
            for k, v in b.w.items():
                if toks.get(k, 0) < v:
                    toks[k] = v
        for b in w:
            for k, v in b.r.items():
                if toks.get(k, 0) < v:
                    toks[k] = v
            if not b.nowaw:
                for k, v in b.w.items():
                    if toks.get(k, 0) < v:
                        toks[k] = v
        return toks

    def _mark(self, key, val, r, w):
        for b in r:
            if b.r.get(key, 0) < val:
                b.r[key] = val
        for b in w:
            if b.nowaw:
                if b.w.get(key, 0) < val:
                    b.w[key] = val
            else:
                b.w = {key: val}
            b.r = {}

    def op(self, en, fn, r=(), w=(), inc=True):
        self.ncall += 1
        if self.cut is not None and self.ncall > self.cut:
            return None
        self._wait(en, self._deps(r, w))
        inst = fn(self.eng[en])
        if inc:
            self.cnt[en] += 1
            inst.then_inc(self.sems[en], 1)
            val = self.cnt[en]
        else:
            val = self.cnt[en] + 1
        self._mark(en, val, r, w)
        return inst

    def dma(self, q, stream, out, in_, r=(), w=(), **kw):
        key = "d:" + stream
        self.ncall += 1
        if self.cut is not None and self.ncall > self.cut:
            return None
        if key not in self.sems:
            self._newsem(key)
        self._wait(q, self._deps(r, w))
        inst = self.eng[q].dma_start(out=out, in_=in_, **kw)
        self.cnt[key] += 16
        inst.then_inc(self.sems[key], 16)
        self._mark(key, self.cnt[key], r, w)
        return inst

    def gather(self, stream, out, in_full, idx_ap, r=(), w=()):
        key = "d:" + stream
        if key not in self.sems:
            self._newsem(key)
        self._wait("pool", self._deps(r, w))
        inst = self.nc.gpsimd.indirect_dma_start(out=out, out_offset=None, in_=in_full, in_offset=bass.IndirectOffsetOnAxis(idx_ap, 0))
        self.cnt[key] += 16
        inst.then_inc(self.sems[key], 16)
        self._mark(key, self.cnt[key], r, w)
        return inst

    def barrier(self):
        for en in self.eng:
            self._wait(en, dict(self.cnt))

    def finish(self):
        self._wait("sp", dict(self.cnt))


def build(T, L=2, debug=False, stop=None):
    assert T % 512 == 0
    NB = T // 512
    NS = T // 128
    TBC = min(1024, T)
    NSC = TBC // 128
    nc = bass.Bass("TRN2", target_bir_lowering=False)
    kb = KB(nc)

    def ext_in(name, shape):
        return Buf(nc.dram_tensor(name, list(shape), F32, kind="ExternalInput"), nowaw=True)

    xin = ext_in("x", [T, D])
    consts_d = ext_in("consts", [128, NCONST])
    params_d = ext_in("params", [L, 128, NPAR])
    w_in_d = ext_in("w_in", [L, D, DIN])
    w_out_d = ext_in("w_out", [L, D, D])
    ffn_g_d = ext_in("ffn_w_gate", [1, D, DFF])
    ffn_u_d = ext_in("ffn_w_up", [1, D, DFF])
    ffn_d_d = ext_in("ffn_w_down", [1, DFF, D])
    wr_d = ext_in("moe_router_w", [1, D, NE])
    moe_g_d = ext_in("moe_w_gate", [1, NE, D, DFF])
    moe_u_d = ext_in("moe_w_up", [1, NE, D, DFF])
    moe_d_d = ext_in("moe_w_down", [1, NE, DFF, D])
    TH = T // 2
    yout = Buf(nc.dram_tensor("y", [TH, D], F32, kind="ExternalOutput"), nowaw=True)
    tokidx_d = Buf(nc.dram_tensor("tokidx", [128, TH // 128], I32, kind="ExternalInput"), nowaw=True)

    dbg = {}

    def scratch(name, shape, dt):
        if debug:
            b = Buf(nc.dram_tensor(name, list(shape), dt, kind="ExternalOutput"), nowaw=True)
            dbg[name] = b
            return b
        return kb.dram(name, shape, dt)

    qT_d = scratch("qT_s", [512, T], BF16)
    kT_d = scratch("kT_s", [512, T], BF16)
    v_d = scratch("v_s", [T, 512], BF16)
    yT_d = scratch("yT_s", [1024, T], BF16)
    xmid_d = scratch("xmid_s", [T, D], F32)
    x1_d = kb.dram("x1_s", [T, D], F32)
    h2_d = kb.dram("h2_s", [T, D], BF16)

    cst = kb.sbuf("cst", [128, NCONST], F32)
    par = kb.sbuf("par", [128, L, NPAR], F32)
    identb = kb.sbuf("identb", [128, 128], BF16)
    blkb = kb.sbuf("blkb", [128, 128], BF16)
    onesb = kb.sbuf("onesb", [128, 128], BF16)
    dmaskb = kb.sbuf("dmaskb", [128, 4, 512], BF16)
    cmaskb = kb.sbuf("cmaskb", [128, 64], F32)
    epsc = kb.sbuf("epsc", [128, 1], F32)
    onec = kb.sbuf("onec", [128, 1], F32)
    lsig = kb.sbuf("lsig", [128, NS, 8], F32)
    negd = kb.sbuf("negd", [128, NS, 8], F32)
    cI = kb.sbuf("cI", [128, NS, 8], F32)
    lbt = kb.sbuf("lbt", [128, 2], F32)
    omlt = kb.sbuf("omlt", [128, 2], F32)
    nomlt = kb.sbuf("nomlt", [128, 2], F32)
    bshift = kb.sbuf("bshift", [128, 1], F32)
    small = kb.sbuf("small", [128, 64], F32)
    idxt = kb.sbuf("idxt", [128, TH // 128], I32)

    ps = [kb.psum("ps%d" % i, [128, 512], F32) for i in range(8)]

    kb.dma("sp", "ld", cst[:, :], consts_d[:, :], w=[cst])
    kb.dma("sp", "ld", idxt[:, :], tokidx_d[:, :], w=[idxt])
    kb.dma("sp", "ld", par[:, :, :], params_d.t.ap().rearrange("l p n -> p l n"), w=[par])
    kb.op("dve", lambda e: e.tensor_copy(out=identb[:, :], in_=cst[:, K_ID:K_ID + 128]), r=[cst], w=[identb])
    kb.op("dve", lambda e: e.tensor_copy(out=blkb[:, :], in_=cst[:, K_BLK:K_BLK + 128]), r=[cst], w=[blkb])
    kb.op("dve", lambda e: e.tensor_copy(out=onesb[:, :], in_=cst[:, K_ONES:K_ONES + 128]), r=[cst], w=[onesb])
    kb.op("dve", lambda e: e.tensor_copy(out=dmaskb[:, :, :], in_=cst[:, K_DM:K_DM + 2048].rearrange("p (j q) -> p j q", q=512)), r=[cst], w=[dmaskb])
    kb.op("dve", lambda e: e.tensor_copy(out=cmaskb[:, :], in_=cst[:, K_CM:K_CM + 64]), r=[cst], w=[cmaskb])
    kb.op("dve", lambda e: e.memset(epsc[:, :], EPS), w=[epsc])
    kb.op("dve", lambda e: e.memset(onec[:, :], 1.0), w=[onec])

    def rstd_from_ms(ms_ap, ms_bufs, out_b, out_ap, tmp_b, tmp_ap):
        kb.op("act", lambda e: e.activation(out=tmp_ap, in_=ms_ap, func=AF.Ln, bias=epsc[:, 0:1], scale=1.0), r=ms_bufs + [epsc], w=[tmp_b])
        kb.op("act", lambda e: e.activation(out=out_ap, in_=tmp_ap, func=AF.Exp, scale=-0.5), r=[tmp_b], w=[out_b])

    for l in range(L):
        x_src = xin if l == 0 else xmid_d
        x_dst = xmid_d if l == 0 else yout
        pl = lambda c0, n=1: par[:, l, c0:c0 + n]

        stackA = ExitStack()
        kb.stack = stackA
        win = kb.sbuf("win%d" % l, [128, 8, DIN], BF16)
        for c in range(8):
            kb.dma("pool", "w", win[:, c, :], w_in_d[l, c * 128:(c + 1) * 128, :], w=[win])
        xt = kb.sbuf("xt%d" % l, [128, 4, D], F32)
        hb = kb.sbuf("hb%d" % l, [128, 4, D], BF16)
        hT = kb.sbuf("hT%d" % l, [128, 8, 512], BF16)
        junk = kb.sbuf("junk%d" % l, [128, D], F32)
        st4 = kb.sbuf("st4%d" % l, [128, 8], F32)
        fa = [kb.sbuf("fa%d_%d" % (l, i), [128, 512], F32) for i in range(6)]
        fb = [kb.sbuf("fb%d_%d" % (l, i), [128, 512], BF16) for i in range(3)]
        ob = [kb.sbuf("ob%d_%d" % (l, i), [128, 512], BF16) for i in range(2)]
        vb = kb.sbuf("vb%d" % l, [128, 4, 512], BF16)
        vi = kb.sbuf("vi%d" % l, [128, 4, 256], BF16)
        ucv = [kb.sbuf("ucv%d_%d" % (l, j), [128, 514], F32) for j in range(2)]
        sig = kb.sbuf("sig%d" % l, [128, 512], F32)
        kk = kb.sbuf("kk%d" % l, [128, 512], F32)
        cc = kb.sbuf("cc%d" % l, [128, 512], F32)
        qs = kb.sbuf("qs%d" % l, [128, 512], F32)
        gs = [kb.sbuf("gs%d_%d" % (l, j), [128, 512], F32) for j in range(2)]
        qe = [kb.sbuf("qe%d_%d" % (l, j), [128, 512], BF16) for j in range(2)]
        ke = [kb.sbuf("ke%d_%d" % (l, j), [128, 512], BF16) for j in range(2)]
        qec = [kb.sbuf("qec%d_%d" % (l, j), [128, 512], BF16) for j in range(2)]
        kdT = kb.sbuf("kdT%d" % l, [128, 512], BF16)
        kd = kb.sbuf("kd%d" % l, [128, 4, 256], BF16)
        ecl = kb.sbuf("ecl%d" % l, [128, 2, 8], F32)
        state = kb.sbuf("state%d" % l, [128, 2, 64], F32)
        stateb = kb.sbuf("stateb%d" % l, [128, 2, 64], BF16)
        atsb = kb.sbuf("atsb%d" % l, [128, 128], BF16)
        osb = kb.sbuf("osb%d" % l, [128, 2, 512], F32)
        vi2 = kb.sbuf("vi2_%d" % l, [128, 8, 256], BF16)
        kd2 = kb.sbuf("kd2_%d" % l, [128, 8, 256], BF16)
        segm = kb.sbuf("segm%d" % l, [128, 512], F32)

        kb.op("dve", lambda e: e.tensor_copy(out=segm[:, :], in_=cst[:, K_SEG:K_SEG + 512]), r=[cst], w=[segm])
        kb.op("dve", lambda e: e.memset(state[:, :, :], 0.0), w=[state])
        kb.op("dve", lambda e: e.memset(stateb[:, :, :], 0.0), w=[stateb])
        for j in range(2):
            kb.op("dve", lambda e: e.memset(ucv[j][:, :], 0.0), w=[ucv[j]])
        if l == 0:
            kb.op("dve", lambda e: e.memset(lbt[:, :], 0.0), w=[lbt])
        else:
            kb.op("dve", lambda e: e.tensor_tensor(out=small[:, 0:2], in0=pl(P_LB + 2, 2), in1=pl(P_LB, 2), op=ALU.subtract), r=[par], w=[small])
            kb.op("act", lambda e: e.activation(out=lbt[:, :], in_=small[:, 0:2], func=AF.Sigmoid), r=[small], w=[lbt])
        kb.op("dve", lambda e: e.tensor_scalar(out=omlt[:, :], in0=lbt[:, :], scalar1=-1.0, scalar2=1.0, op0=ALU.mult, op1=ALU.add), r=[lbt], w=[omlt])
        kb.op("dve", lambda e: e.tensor_scalar(out=nomlt[:, :], in0=omlt[:, :], scalar1=-1.0, scalar2=None, op0=ALU.mult), r=[omlt], w=[nomlt])
        kb.op("dve", lambda e: e.tensor_reduce(out=small[:, 8:9], in_=pl(P_GQR, 64), axis=mybir.AxisListType.X, op=ALU.max, apply_absolute_value=True), r=[par], w=[small])
        kb.op("dve", lambda e: e.tensor_reduce(out=small[:, 9:10], in_=pl(P_GKR, 64), axis=mybir.AxisListType.X, op=ALU.max, apply_absolute_value=True), r=[par, small], w=[small])
        kb.op("dve", lambda e: e.scalar_tensor_tensor(out=bshift[:, :], in0=small[:, 8:9], scalar=8.0, in1=small[:, 9:10], op0=ALU.mult, op1=ALU.mult), r=[small], w=[bshift])

        rot = {"fm": 0, "tm": 0, "fa": 0, "fb": 0, "ob": 0}

        def nxt(name, n):
            v = rot[name]
            rot[name] = (v + 1) % n
            return v

        def fm_chunk(col0):
            p = ps[1 + nxt("fm", 2)]
            for c in range(8):
                kb.op("pe", lambda e: e.matmul(p[:, :], lhsT=win[:, c, col0:col0 + 128], rhs=hT[:, c, :], start=(c == 0), stop=(c == 7)),
                      r=[win, hT], w=[p], inc=(c == 7))
            return p

        def headnorm_store(src_ap, src_bufs, gain_ap, dst_d, row0, tok0, mul_b=None):
            sq = fb[nxt("fb", 3)]
            kb.op("act", lambda e: e.activation(out=sq[:, :], in_=src_ap, func=AF.Square), r=src_bufs, w=[sq])
            pm = ps[5]
            kb.op("pe", lambda e: e.matmul(pm[:, :], lhsT=blkb[:, :], rhs=sq[:, :], start=True, stop=True), r=[blkb, sq], w=[pm])
            t1 = fa[nxt("fa", 6)]
            rs = fa[nxt("fa", 6)]
            rstd_from_ms(pm[:, :], [pm], rs, rs[:, :], t1, t1[:, :])
            o = ob[nxt("ob", 2)]
            if mul_b is None:
                kb.op("dve", lambda e: e.scalar_tensor_tensor(out=o[:, :], in0=src_ap, scalar=gain_ap, in1=rs[:, :], op0=ALU.mult, op1=ALU.mult),
                      r=src_bufs + [rs, par], w=[o])
            else:
                kb.op("dve", lambda e: e.scalar_tensor_tensor(out=t1[:, :], in0=src_ap, scalar=gain_ap, in1=rs[:, :], op0=ALU.mult, op1=ALU.mult),
                      r=src_bufs + [rs, par], w=[t1])
                kb.op("dve", lambda e: e.tensor_tensor(out=o[:, :], in0=t1[:, :], in1=mul_b[:, :], op=ALU.mult), r=[t1, mul_b], w=[o])
            kb.dma("sp", "st", dst_d[row0:row0 + 128, tok0:tok0 + 512], o[:, :], r=[o], w=[dst_d])

        for tb in range(NB):
            tok0 = tb * 512
            kb.dma("sp", "ld", xt[:, :, :], x_src[tok0:tok0 + 512, :].rearrange("(s p) d -> p s d", p=128), r=[x_src], w=[xt])
            for s in range(4):
                kb.op("act", lambda e: e.activation(out=junk[:, :], in_=xt[:, s, :], func=AF.Square, accum_out=st4[:, s:s + 1]), r=[xt], w=[junk, st4])
            rstd_tm(kb, st4, epsc, 4)
            for s in range(4):
                kb.op("act", lambda e: e.activation(out=hb[:, s, :], in_=xt[:, s, :], func=AF.Identity, scale=st4[:, 4 + s:5 + s]), r=[xt, st4], w=[hb])
            pT = ps[0]
            pTb = pT.t.ap().bitcast(BF16)
            for c in range(8):
                half = (c % 2) * 512
                for s in range(4):
                    kb.op("pe", lambda e: e.transpose(pTb[:, half + s * 128:half + (s + 1) * 128], hb[:, s, c * 128:(c + 1) * 128], identb[:, :]),
                          r=[hb, identb], w=[pT], inc=(s == 3))
                en = "dve" if c % 2 == 0 else "act"
                if en == "dve":
                    kb.op("dve", lambda e: e.tensor_scalar(out=hT[:, c, :], in0=pTb[:, half:half + 512], scalar1=pl(P_G1 + c), scalar2=None, op0=ALU.mult), r=[pT, par], w=[hT])
                else:
                    kb.op("act", lambda e: e.activation(out=hT[:, c, :], in_=pTb[:, half:half + 512], func=AF.Identity, scale=pl(P_G1 + c)), r=[pT, par], w=[hT])

            for which, col0, dst, gcol in (("q", C_Q, qT_d, P_GQ), ("k", C_K, kT_d, P_GK)):
                for c in range(4):
                    p = fm_chunk(col0 + c * 128)
                    headnorm_store(p[:, :], [p], pl(gcol), dst, c * 128, tok0)

            for s in range(4):
                p = ps[3 + nxt("tm", 2)]
                for c in range(8):
                    kb.op("pe", lambda e: e.matmul(p[:, :], lhsT=hT[:, c, s * 128:(s + 1) * 128], rhs=win[:, c, C_V:C_V + 512], start=(c == 0), stop=(c == 7)),
                          r=[win, hT], w=[p], inc=(c == 7))
                kb.op("act", lambda e: e.activation(out=vb[:, s, :], in_=p[:, :], func=AF.Copy), r=[p], w=[vb])
                p2 = ps[3 + nxt("tm", 2)]
                for c in range(8):
                    kb.op("pe", lambda e: e.matmul(p2[:, 0:256], lhsT=hT[:, c, s * 128:(s + 1) * 128], rhs=win[:, c, C_RI:C_RI + 256], start=(c == 0), stop=(c == 7)),
                          r=[win, hT], w=[p2], inc=False)
                for c in range(8):
                    kb.op("pe", lambda e: e.matmul(p2[:, 256:264], lhsT=hT[:, c, s * 128:(s + 1) * 128], rhs=win[:, c, C_F:C_F + 8], start=(c == 0), stop=(c == 7)),
                          r=[win, hT], w=[p2], inc=(c == 7))
                kb.op("act", lambda e: e.activation(out=vi[:, s, :], in_=p2[:, 0:256], func=AF.Copy), r=[p2], w=[vi])
                blk = tb * 4 + s
                kb.op("dve", lambda e: e.tensor_tensor(out=small[:, 16:24], in0=p2[:, 256:264], in1=pl(P_FB, 8), op=ALU.add), r=[p2, par, vi], w=[small])
                kb.op("act", lambda e: e.activation(out=small[:, 24:32], in_=small[:, 16:24], func=AF.Abs), r=[small], w=[small])
                kb.op("act", lambda e: e.activation(out=small[:, 32:40], in_=small[:, 24:32], func=AF.Exp, scale=-1.0), r=[small], w=[small])
                kb.op("act", lambda e: e.activation(out=small[:, 40:48], in_=small[:, 32:40], func=AF.Ln, bias=onec[:, 0:1], scale=1.0), r=[small, onec], w=[small])
                kb.op("dve", lambda e: e.tensor_single_scalar(out=small[:, 48:56], in_=small[:, 16:24], scalar=0.0, op=ALU.min), r=[small], w=[small])
                kb.op("dve", lambda e: e.tensor_tensor(out=lsig[:, blk, :], in0=small[:, 48:56], in1=small[:, 40:48], op=ALU.subtract), r=[small], w=[lsig])
            kb.dma("sp", "st", v_d[tok0:tok0 + 512, :].rearrange("(s p) f -> p s f", p=128), vb[:, :, :], r=[vb], w=[v_d])

            for j in range(2):
                px = fm_chunk(C_CX + j * 128)
                t0 = fa[nxt("fa", 6)]
                kb.op("act", lambda e: e.activation(out=t0[:, :], in_=px[:, :], func=AF.Copy), r=[px], w=[t0])
                pc = fm_chunk(C_CC + j * 128)
                u = ucv[j]
                kb.op("dve", lambda e: e.tensor_tensor(out=u[:, 2:514], in0=t0[:, :], in1=pc[:, :], op=ALU.mult), r=[t0, pc], w=[u])
                t1 = fa[nxt("fa", 6)]
                kb.op("dve", lambda e: e.tensor_scalar(out=t1[:, :], in0=u[:, 0:512], scalar1=pl(P_CW + 0 * 2 + j), scalar2=None, op0=ALU.mult), r=[u, par], w=[t1])
                kb.op("dve", lambda e: e.scalar_tensor_tensor(out=t1[:, :], in0=u[:, 1:513], scalar=pl(P_CW + 1 * 2 + j), in1=t1[:, :], op0=ALU.mult, op1=ALU.add), r=[u, par, t1], w=[t1])
                kb.op("dve", lambda e: e.scalar_tensor_tensor(out=t1[:, :], in0=u[:, 2:514], scalar=pl(P_CW + 2 * 2 + j), in1=t1[:, :], op0=ALU.mult, op1=ALU.add), r=[u, par, t1], w=[t1])
                pbg = fm_chunk(C_CB + j * 128)
                kb.op("dve", lambda e: e.tensor_tensor(out=t0[:, :], in0=t1[:, :], in1=pbg[:, :], op=ALU.mult), r=[t1, pbg], w=[t0])
                kb.op("dve", lambda e: e.tensor_copy(out=small[:, 56:58], in_=u[:, 512:514]), r=[u], w=[small])
                kb.op("dve", lambda e: e.tensor_copy(out=u[:, 0:2], in_=small[:, 56:58]), r=[small], w=[u])
                headnorm_store(t0[:, :], [t0], pl(P_MG + 4 + j), yT_d, 512 + j * 128, tok0)

            for j in range(2):
                pf = fm_chunk(C_RF + j * 128)
                kb.op("act", lambda e: e.activation(out=sig[:, :], in_=pf[:, :], func=AF.Sigmoid), r=[pf], w=[sig])
                tf = fa[nxt("fa", 6)]
                kb.op("dve", lambda e: e.tensor_scalar(out=tf[:, :], in0=sig[:, :], scalar1=omlt[:, j:j + 1], scalar2=lbt[:, j:j + 1], op0=ALU.mult, op1=ALU.add), r=[sig, omlt, lbt], w=[tf])
                kb.op("dve", lambda e: e.tensor_single_scalar(out=tf[:, :], in_=tf[:, :], scalar=TINY, op=ALU.max), r=[tf], w=[tf])
                kb.op("act", lambda e: e.activation(out=tf[:, :], in_=tf[:, :], func=AF.Ln), r=[tf], w=[tf])
                kb.op("dve", lambda e: e.tensor_scalar(out=kk[:, :], in0=sig[:, :], scalar1=nomlt[:, j:j + 1], scalar2=omlt[:, j:j + 1], op0=ALU.mult, op1=ALU.add), r=[sig, omlt, nomlt], w=[kk])
                kb.op("dve", lambda e: e.tensor_tensor_scan(out=cc[:, :], data0=segm[:, :], data1=tf[:, :], initial=0.0, op0=ALU.mult, op1=ALU.add), r=[segm, tf], w=[cc])
                c3 = cc.t.ap().rearrange("p (n t) -> p n t", t=64)
                pq = fm_chunk(C_RQ + j * 128)
                kb.op("act", lambda e: e.activation(out=qs[:, :], in_=pq[:, :], func=AF.Silu), r=[pq], w=[qs])
                pg = fm_chunk(C_RG + j * 128)
                kb.op("act", lambda e: e.activation(out=gs[j][:, :], in_=pg[:, :], func=AF.Silu), r=[pg], w=[gs[j]])
                d1 = fa[nxt("fa", 6)]
                kb.op("dve", lambda e: e.tensor_tensor(out=d1.t.ap().rearrange("p (n t) -> p n t", t=64), in0=c3, in1=c3[:, :, 31:32].to_broadcast([128, 8, 64]), op=ALU.subtract), r=[cc], w=[d1])
                ex = fa[nxt("fa", 6)]
                kb.op("act", lambda e: e.activation(out=ex[:, :], in_=d1[:, :], func=AF.Exp), r=[d1], w=[ex])
                kb.op("dve", lambda e: e.tensor_tensor(out=qe[j][:, :], in0=qs[:, :], in1=ex[:, :], op=ALU.mult), r=[qs, ex], w=[qe[j]])
                kb.op("act", lambda e: e.activation(out=ex[:, :], in_=d1[:, :], func=AF.Exp, scale=-1.0), r=[d1], w=[ex])
                kb.op("dve", lambda e: e.tensor_tensor(out=ke[j][:, :], in0=kk[:, :], in1=ex[:, :], op=ALU.mult), r=[kk, ex], w=[ke[j]])
                kb.op("act", lambda e: e.activation(out=ex[:, :], in_=cc[:, :], func=AF.Exp), r=[cc], w=[ex])
                kb.op("dve", lambda e: e.tensor_tensor(out=qec[j][:, :], in0=qs[:, :], in1=ex[:, :], op=ALU.mult), r=[qs, ex], w=[qec[j]])
                kb.op("dve", lambda e: e.tensor_tensor(out=d1.t.ap().rearrange("p (n t) -> p n t", t=64), in0=c3[:, :, 63:64].to_broadcast([128, 8, 64]), in1=c3, op=ALU.subtract), r=[cc], w=[d1])
                kb.op("act", lambda e: e.activation(out=ex[:, :], in_=d1[:, :], func=AF.Exp), r=[d1], w=[ex])
                kb.op("dve", lambda e: e.tensor_tensor(out=kdT[:, :], in0=kk[:, :], in1=ex[:, :], op=ALU.mult), r=[kk, ex], w=[kdT])
                kb.op("act", lambda e: e.activation(out=ecl[:, j, :], in_=c3[:, :, 63], func=AF.Exp), r=[cc], w=[ecl])
                pT = ps[0]
                pTb = pT.t.ap().bitcast(BF16)
                for s in range(4):
                    kb.op("pe", lambda e: e.transpose(pTb[:, s * 128:(s + 1) * 128], kdT[:, s * 128:(s + 1) * 128], identb[:, :]), r=[kdT, identb], w=[pT], inc=(s == 3))
                kb.op("dve", lambda e: e.tensor_copy(out=kd[:, :, j * 128:(j + 1) * 128], in_=pTb[:, 0:512].rearrange("p (s f) -> p s f", f=128)), r=[pT], w=[kd])

            for pr in range(2):
                for dst in range(2):
                    kb.dma("sp", "cp", vi2.t.ap().rearrange("p (s two) f -> p s two f", two=2)[dst * 64:(dst + 1) * 64, :, pr, :], vi[pr * 64:(pr + 1) * 64, :, :], r=[vi], w=[vi2])
                    kb.dma("sp", "cp", kd2.t.ap().rearrange("p (s two) f -> p s two f", two=2)[dst * 64:(dst + 1) * 64, :, pr, :], kd[pr * 64:(pr + 1) * 64, :, :], r=[kd], w=[kd2])
            for ch in range(8):
                cols = slice(ch * 64, (ch + 1) * 64)
                for hh in range(2):
                    pb = hh * 64
                    ph = ps[6 + hh]
                    for j in range(2):
                        kb.op("pe", lambda e: e.matmul(ph[pb:pb + 64, j * 64:(j + 1) * 64], lhsT=ke[j][pb:pb + 64, cols], rhs=qe[j][pb:pb + 64, cols], start=True, stop=True, tile_position=(pb, pb)),
                              r=[ke[j], qe[j]], w=[ph], inc=(j == 1))
                for hh in range(2):
                    pb = hh * 64
                    ph = ps[6 + hh]
                    kb.op("dve", lambda e: e.tensor_tensor(out=atsb[pb:pb + 64, 0:128].rearrange("p (h t) -> p h t", t=64), in0=ph[pb:pb + 64, 0:128].rearrange("p (h t) -> p h t", t=64),
                                                           in1=cmaskb[pb:pb + 64, :].unsqueeze(1).to_broadcast([64, 2, 64]), op=ALU.mult), r=[ph, cmaskb], w=[atsb])
                for hh in range(2):
                    pb = hh * 64
                    ph = ps[6 + hh]
                    for j in range(2):
                        hd = j * 2 + hh
                        oslc = ph[pb:pb + 64, 128 + j * 64:128 + (j + 1) * 64]
                        kb.op("pe", lambda e: e.matmul(oslc, lhsT=vi2[pb:pb + 64, ch, hd * 64:(hd + 1) * 64], rhs=atsb[pb:pb + 64, j * 64:(j + 1) * 64], start=True, stop=False, tile_position=(pb, pb)),
                              r=[vi2, atsb], w=[ph], inc=False)
                        kb.op("pe", lambda e: e.matmul(oslc, lhsT=stateb[pb:pb + 64, j, :], rhs=qec[j][pb:pb + 64, cols], start=False, stop=True, tile_position=(pb, pb)),
                              r=[stateb, qec[j]], w=[ph], inc=False)
                        kb.op("pe", lambda e: e.matmul(ph[pb:pb + 64, 256 + j * 64:256 + (j + 1) * 64], lhsT=kd2[pb:pb + 64, ch, hd * 64:(hd + 1) * 64], rhs=vi2[pb:pb + 64, ch, hd * 64:(hd + 1) * 64], start=True, stop=True, tile_position=(pb, pb)),
                              r=[kd2, vi2], w=[ph], inc=(j == 1))
                kb.op("dve", lambda e: e.tensor_tensor(out=state[:, :, :], in0=state[:, :, :], in1=ecl[:, :, ch:ch + 1].to_broadcast([128, 2, 64]), op=ALU.mult), r=[state, ecl], w=[state])
                for hh in range(2):
                    pb = hh * 64
                    ph = ps[6 + hh]
                    kb.op("act", lambda e: e.activation(out=osb[pb:pb + 64, :, cols], in_=ph[pb:pb + 64, 128:256].rearrange("p (j t) -> p j t", t=64), func=AF.Copy), r=[ph], w=[osb])
                    kb.op("dve", lambda e: e.tensor_tensor(out=state[pb:pb + 64, :, :], in0=state[pb:pb + 64, :, :], in1=ph[pb:pb + 64, 256:384].rearrange("p (j v) -> p j v", v=64), op=ALU.add), r=[state, ph, osb], w=[state])
                kb.op("dve", lambda e: e.tensor_copy(out=stateb[:, :, :], in_=state[:, :, :]), r=[state], w=[stateb])
            for j in range(2):
                headnorm_store(osb[:, j, :], [osb], pl(P_MG + 6 + j), yT_d, 768 + j * 128, tok0, mul_b=gs[j])

        lflat = lsig.t.ap().rearrange("p n h -> p (n h)")
        NW = NS * 8
        tri = cst[:, K_TRI:K_TRI + 128]
        onesf = cst[:, K_ONES:K_ONES + 128]
        sel = cst[:, K_SEL:K_SEL + 128]
        pcs, ptot = ps[1], ps[2]
        kb.op("pe", lambda e: e.matmul(pcs[:, 0:NW], lhsT=tri, rhs=lflat, start=True, stop=True), r=[cst, lsig], w=[pcs])
        kb.op("pe", lambda e: e.matmul(ptot[:, 0:NW], lhsT=onesf, rhs=lflat, start=True, stop=True), r=[cst, lsig], w=[ptot])
        tot = fa[0]
        kb.op("act", lambda e: e.activation(out=tot[:, 0:NW], in_=ptot[:, 0:NW], func=AF.Copy), r=[ptot], w=[tot])
        kb.op("dve", lambda e: e.memset(cI[:, 0, :], 0.0), w=[cI])
        for b in range(1, NS):
            kb.op("dve", lambda e: e.tensor_tensor(out=cI[:, b, :], in0=cI[:, b - 1, :], in1=tot[:, (b - 1) * 8:b * 8], op=ALU.add), r=[cI, tot], w=[cI])
        dcum = fa[1]
        kb.op("dve", lambda e: e.tensor_tensor(out=dcum[:, 0:NW], in0=pcs[:, 0:NW], in1=cI.t.ap().rearrange("p n h -> p (n h)"), op=ALU.add), r=[pcs, cI], w=[dcum])
        kb.op("dve", lambda e: e.tensor_scalar(out=negd.t.ap().rearrange("p n h -> p (n h)"), in0=dcum[:, 0:NW], scalar1=-1.0, scalar2=None, op0=ALU.mult), r=[dcum], w=[negd])
        pbc = ps[3]
        kb.op("pe", lambda e: e.matmul(pbc[:, 0:NW], lhsT=sel, rhs=dcum[:, 0:NW], start=True, stop=True), r=[cst, dcum], w=[pbc])
        kb.op("dve", lambda e: e.tensor_scalar(out=cI.t.ap().rearrange("p n h -> p (n h)"), in0=pbc[:, 0:NW], scalar1=bshift[:, 0:1], scalar2=None, op0=ALU.subtract), r=[pbc, bshift], w=[cI])

        kb.barrier()
        if stop == ("A", l):
            kb.finish()
            return nc, dbg
        stackA.close()
        stackB = ExitStack()
        kb.stack = stackB
        obB = [kb.sbuf("obB%d_%d" % (l, i), [128, 512], BF16) for i in range(2)]
        kTc = kb.sbuf("kTc%d" % l, [128, T], BF16)
        qTc = kb.sbuf("qTc%d" % l, [128, T], BF16)
        vA = kb.sbuf("vA%d" % l, [128, NS, 128], BF16)
        vB = kb.sbuf("vB%d" % l, [128, NS, 128], BF16)
        dcat = kb.sbuf("dcat%d" % l, [128, 512], F32)
        kb.op("dve", lambda e: e.memset(vA[:, :, 64:128], 1.0), w=[vA])
        kb.op("dve", lambda e: e.memset(vB[:, :, 0:64], 1.0), w=[vB])
        pTt = [kb.sbuf("pTt%d_%d" % (l, i), [128, 512], BF16) for i in range(3)]
        biasb = [kb.sbuf("biasb%d_%d" % (l, i), [128, NS], F32) for i in range(2)]
        rden = kb.sbuf("rden%d" % l, [128, 512], F32)
        on = kb.sbuf("on%d" % l, [128, 512], F32)
        rotb = {"s": 0, "p": 0, "b": 0, "o": 0, "ob": 0}

        def nxb(name, n):
            v = rotb[name]
            rotb[name] = (v + 1) % n
            return v

        for c in range(4):
            kb.dma("sp", "ld", kTc[:, :], kT_d[c * 128:(c + 1) * 128, :], r=[kT_d], w=[kTc])
            kb.dma("sp", "ld", qTc[:, :], qT_d[c * 128:(c + 1) * 128, :], r=[qT_d], w=[qTc])
            kb.dma("sp", "ld", vA[:, :, 0:64], v_d[:, c * 128:c * 128 + 64].rearrange("(n p) f -> p n f", p=128), r=[v_d], w=[vA])
            kb.dma("sp", "ld", vB[:, :, 64:128], v_d[:, c * 128 + 64:(c + 1) * 128].rearrange("(n p) f -> p n f", p=128), r=[v_d], w=[vB])
            for I in range(NB):
                oi = nxb("o", 2)
                pO, pD = ps[3 + oi], ps[5 + oi]
                nJ = 4 * (I + 1)
                for hh in range(2):
                    pb = hh * 64
                    h = 2 * c + hh
                    bb = biasb[nxb("b", 2)]
                    kb.op("dve", lambda e: e.tensor_scalar(out=bb[:, 0:nJ], in0=negd[:, 0:nJ, h], scalar1=cI[:, 4 * I + 1, h:h + 1], scalar2=None, op0=ALU.add), r=[negd, cI], w=[bb])
                    def emit_s(J):
                        pS = ps[nxb("s", 3)]
                        kb.op("pe", lambda e: e.matmul(pS[:, :], lhsT=kTc[pb:pb + 64, J * 128:(J + 1) * 128], rhs=qTc[pb:pb + 64, I * 512:(I + 1) * 512], start=True, stop=True, tile_position=(pb, 0)),
                              r=[kTc, qTc], w=[pS])
                        return pS

                    def consume(J, pS):
                        pt = pTt[nxb("p", 3)]
                        kb.op("act", lambda e: e.activation(out=pt[:, :], in_=pS[:, :], func=AF.Exp, bias=bb[:, J:J + 1], scale=0.125), r=[pS, bb], w=[pt])
                        if J >= 4 * I:
                            kb.op("dve", lambda e: e.tensor_tensor(out=pt[:, :], in0=pt[:, :], in1=dmaskb[:, J - 4 * I, :], op=ALU.mult), r=[pt, dmaskb], w=[pt])
                        pX, vX = (pO, vA) if hh == 0 else (pD, vB)
                        kb.op("pe", lambda e: e.matmul(pX[:, :], lhsT=vX[:, J, :], rhs=pt[:, :], start=(J == 0), stop=(J == nJ - 1)), r=[vX, pt], w=[pX])

                    pendq = []
                    for J in range(nJ):
                        pendq.append((J, emit_s(J)))
                        if len(pendq) > 2:
                            consume(*pendq.pop(0))
                    while pendq:
                        consume(*pendq.pop(0))
                kb.op("act", lambda e: e.activation(out=dcat[0:64, :], in_=pD[0:64, :], func=AF.Copy), r=[pD], w=[dcat])
                kb.op("act", lambda e: e.activation(out=dcat[64:128, :], in_=pO[64:128, :], func=AF.Copy), r=[pO], w=[dcat])
                pm = ps[7]
                kb.op("pe", lambda e: e.matmul(pm[:, :], lhsT=cst[:, K_SWAP:K_SWAP + 128], rhs=dcat[:, :], start=True, stop=True), r=[cst, dcat], w=[pm])
                kb.op("dve", lambda e: e.reciprocal(out=rden[:, :], in_=pm[:, :]), r=[pm], w=[rden])
                kb.op("dve", lambda e: e.tensor_tensor(out=on[0:64, :], in0=pO[0:64, :], in1=rden[0:64, :], op=ALU.mult), r=[pO, rden], w=[on])
                kb.op("dve", lambda e: e.tensor_tensor(out=on[64:128, :], in0=pD[64:128, :], in1=rden[64:128, :], op=ALU.mult), r=[pD, rden], w=[on])
                sq = pTt[nxb("p", 3)]
                kb.op("act", lambda e: e.activation(out=sq[:, :], in_=on[:, :], func=AF.Square), r=[on], w=[sq])
                pm = ps[7]
                kb.op("pe", lambda e: e.matmul(pm[:, :], lhsT=blkb[:, :], rhs=sq[:, :], start=True, stop=True), r=[blkb, sq], w=[pm])
                rstd_from_ms(pm[:, :], [pm], rden, rden[:, :], rden, rden[:, :])
                o = obB[nxb("ob", 2)]
                kb.op("dve", lambda e: e.scalar_tensor_tensor(out=o[:, :], in0=on[:, :], scalar=pl(P_MG + c), in1=rden[:, :], op0=ALU.mult, op1=ALU.mult), r=[on, rden, par], w=[o])
                kb.dma("sp", "st", yT_d[c * 128:(c + 1) * 128, I * 512:(I + 1) * 512], o[:, :], r=[o], w=[yT_d])

        kb.barrier()
        if stop == ("B", l):
            kb.finish()
            return nc, dbg
        stackB.close()
        stackC = ExitStack()
        kb.stack = stackC
        moe = (l % 2 == 1)
        junk = kb.sbuf("junkC%d" % l, [128, D], F32)
        h2b = [kb.sbuf("h2b%d_%d" % (l, i), [128, D], BF16) for i in range(2)]
        stc = kb.sbuf("stc%d" % l, [128, 2 * NSC], F32)
        wgb = [kb.sbuf("wgb%d_%d" % (l, i), [128, 8, 512], BF16) for i in range(2)]
        wub = [kb.sbuf("wub%d_%d" % (l, i), [128, 8, 512], BF16) for i in range(2)]
        wdb = [kb.sbuf("wdb%d_%d" % (l, i), [128, 4, D], BF16) for i in range(2)]
        sgt = [kb.sbuf("sgt%d_%d" % (l, i), [128, 512], BF16) for i in range(2)]
        if moe:
            wrb = kb.sbuf("wrb%d" % l, [128, 8, NE], BF16)
            kb.dma("pool", "w", wrb[:, :, :], wr_d[0].rearrange("(c p) e -> p c e", p=128), w=[wrb])
            rt = kb.sbuf("rt%d" % l, [128, 64], F32)
            trib = kb.sbuf("trib%d" % l, [128, 128], BF16)
            kb.op("dve", lambda e: e.tensor_copy(out=trib[:, :], in_=cst[:, K_TRI:K_TRI + 128]), r=[cst], w=[trib])
        stackC1 = ExitStack()
        kb.stack = stackC1
        wout = kb.sbuf("wout%d" % l, [128, 8, D], BF16)
        for c in range(8):
            kb.dma("pool", "w", wout[:, c, :], w_out_d[l, c * 128:(c + 1) * 128, :], w=[wout])
        x1 = kb.sbuf("x1_%d" % l, [128, NSC, D], F32)
        yTb = kb.sbuf("yTb%d" % l, [128, 8, TBC], BF16)
        h2T = yTb
        hact = kb.sbuf("hact%d" % l, [128, 4, TBC], BF16)
        rc = {"a": 0, "g": 0, "u": 0, "w": 0, "h": 0, "sg": 0}

        def nxc(name, n):
            v = rc[name]
            rc[name] = (v + 1) % n
            return v

        passes = [("full", T // TBC)] if not moe else [("c1", T // TBC)]
        for mode, tbc in [(m_, t_) for m_, n_ in passes for t_ in range(n_)]:
            tok0 = tbc * TBC
            if mode != "c2":
                kb.dma("sp", "ld", x1[:, :, :], x_src[tok0:tok0 + TBC, :].rearrange("(s p) d -> p s d", p=128), r=[x_src], w=[x1])
                kb.dma("sp", "ld", yTb[:, :, :], yT_d[:, tok0:tok0 + TBC].rearrange("(c p) t -> p c t", p=128), r=[yT_d], w=[yTb])
            for s in (range(NSC) if mode != "c2" else []):
                for half in range(2):
                    p = ps[1 + nxc("a", 2)]
                    for c in range(8):
                        kb.op("pe", lambda e: e.matmul(p[:, :], lhsT=yTb[:, c, s * 128:(s + 1) * 128], rhs=wout[:, c, half * 512:(half + 1) * 512], start=(c == 0), stop=(c == 7)),
                              r=[yTb, wout], w=[p], inc=(c == 7))
                    kb.op("dve", lambda e: e.tensor_tensor(out=x1[:, s, half * 512:(half + 1) * 512], in0=x1[:, s, half * 512:(half + 1) * 512], in1=p[:, :], op=ALU.add), r=[x1, p], w=[x1])
            if mode != "c2":
                for s in range(NSC):
                    kb.op("act", lambda e: e.activation(out=junk[:, :], in_=x1[:, s, :], func=AF.Square, accum_out=stc[:, s:s + 1]), r=[x1], w=[junk, stc])
                rstd_tm(kb, stc, epsc, NSC)
            for s in range(NSC):
                hbb = h2b[nxc("h", 2)]
                if mode != "c2":
                    kb.op("act", lambda e: e.activation(out=hbb[:, :], in_=x1[:, s, :], func=AF.Identity, scale=stc[:, NSC + s:NSC + s + 1]), r=[x1, stc], w=[hbb])
                if mode == "c1":
                    kb.dma("sp", "st", h2_d[tok0 + s * 128:tok0 + (s + 1) * 128, :], hbb[:, :], r=[hbb], w=[h2_d])
                    continue
                if mode == "c2":
                    ic = tbc * NSC + s
                    kb.gather("g", x1[:, s, :], x1_d.t.ap(), idxt[:, ic:ic + 1], r=[x1_d, idxt], w=[x1])
                    kb.gather("g", hbb[:, :], h2_d.t.ap(), idxt[:, ic:ic + 1], r=[h2_d, idxt], w=[hbb])
                pT = ps[0]
                pTb = pT.t.ap().bitcast(BF16)
                for c in range(8):
                    kb.op("pe", lambda e: e.transpose(pTb[:, c * 128:(c + 1) * 128], hbb[:, c * 128:(c + 1) * 128], identb[:, :]), r=[hbb, identb], w=[pT], inc=(c == 7))
                kb.op("dve", lambda e: e.tensor_tensor(out=h2T[:, :, s * 128:(s + 1) * 128], in0=pTb[:, 0:1024].rearrange("p (c t) -> p c t", t=128),
                                                       in1=pl(P_G2, 8).unsqueeze(2).to_broadcast([128, 8, 128]), op=ALU.mult), r=[pT, par], w=[h2T])
            if mode == "c1":
                kb.dma("sp", "st", x1_d[tok0:tok0 + TBC, :].rearrange("(s p) d -> p s d", p=128), x1[:, :, :], r=[x1], w=[x1_d])
                continue
            if moe:
                for s in range(NSC):
                    p = ps[1 + nxc("a", 2)]
                    for c in range(8):
                        kb.op("pe", lambda e: e.matmul(p[:, 0:NE], lhsT=h2T[:, c, s * 128:(s + 1) * 128], rhs=wrb[:, c, :], start=(c == 0), stop=(c == 7)), r=[h2T, wrb], w=[p], inc=(c == 7))
                    kb.op("dve", lambda e: e.tensor_tensor(out=rt[:, 0:8], in0=p[:, 0:NE], in1=pl(P_RB, 8), op=ALU.add), r=[p, par], w=[rt])
                    kb.op("dve", lambda e: e.max(out=rt[:, 8:16], in_=rt[:, 0:8]), r=[rt], w=[rt])
                    kb.op("dve", lambda e: e.tensor_tensor(out=rt[:, 16:17], in0=rt[:, 8:9], in1=rt[:, 9:10], op=ALU.subtract), r=[rt], w=[rt])
                    kb.op("act", lambda e: e.activation(out=rt[:, 17:18], in_=rt[:, 16:17], func=AF.Sigmoid), r=[rt], w=[rt])
                    kb.op("act", lambda e: e.activation(out=rt[:, 18:19], in_=rt[:, 16:17], func=AF.Sigmoid, scale=-1.0), r=[rt], w=[rt])
                    kb.op("dve", lambda e: e.tensor_scalar(out=rt[:, 24:32], in0=rt[:, 0:8], scalar1=rt[:, 8:9], scalar2=rt[:, 17:18], op0=ALU.is_equal, op1=ALU.mult), r=[rt], w=[rt])
                    kb.op("dve", lambda e: e.tensor_scalar(out=rt[:, 32:40], in0=rt[:, 0:8], scalar1=rt[:, 9:10], scalar2=rt[:, 18:19], op0=ALU.is_equal, op1=ALU.mult), r=[rt], w=[rt])
                    kb.op("dve", lambda e: e.tensor_tensor(out=gates[:, s, :], in0=rt[:, 24:32], in1=rt[:, 32:40], op=ALU.add), r=[rt], w=[gates])
            experts = range(NE) if moe else [None]
            for ex in experts:
                if ex is None:
                    Wg, Wu, Wd = ffn_g_d.t.ap()[0], ffn_u_d.t.ap()[0], ffn_d_d.t.ap()[0]
                    wbufs = [ffn_g_d, ffn_u_d, ffn_d_d]
                else:
                    Wg, Wu, Wd = moe_g_d.t.ap()[0, ex], moe_u_d.t.ap()[0, ex], moe_d_d.t.ap()[0, ex]
                    wbufs = [moe_g_d, moe_u_d, moe_d_d]
                for fg in range(DFF // 512):
                    wi = nxc("w", 2)
                    kb.dma("pool", "w", wgb[wi][:, :, :], Wg[:, fg * 512:(fg + 1) * 512].rearrange("(c p) f -> p c f", p=128), w=[wgb[wi]])
                    kb.dma("pool", "w", wub[wi][:, :, :], Wu[:, fg * 512:(fg + 1) * 512].rearrange("(c p) f -> p c f", p=128), w=[wub[wi]])
                    kb.dma("pool", "w", wdb[wi][:, :, :], Wd[fg * 512:(fg + 1) * 512, :].rearrange("(k p) d -> p k d", p=128), w=[wdb[wi]])
                    for t4 in range(TBC // 512):
                        tsl = slice(t4 * 512, (t4 + 1) * 512)
                        for k in range(4):
                            pg = ps[3 + nxc("g", 2)]
                            pu = ps[5 + nxc("u", 2)]
                            for c in range(8):
                                kb.op("pe", lambda e: e.matmul(pg[:, :], lhsT=wgb[wi][:, c, k * 128:(k + 1) * 128], rhs=h2T[:, c, tsl], start=(c == 0), stop=(c == 7)), r=[wgb[wi], h2T], w=[pg], inc=(c == 7))
                            for c in range(8):
                                kb.op("pe", lambda e: e.matmul(pu[:, :], lhsT=wub[wi][:, c, k * 128:(k + 1) * 128], rhs=h2T[:, c, tsl], start=(c == 0), stop=(c == 7)), r=[wub[wi], h2T], w=[pu], inc=(c == 7))
                            sg = sgt[nxc("sg", 2)]
                            kb.op("act", lambda e: e.activation(out=sg[:, :], in_=pg[:, :], func=AF.Silu), r=[pg], w=[sg])
                            kb.op("dve", lambda e: e.tensor_tensor(out=hact[:, k, tsl], in0=sg[:, :], in1=pu[:, :], op=ALU.mult), r=[sg, pu], w=[hact])
                    for s in range(NSC):
                        for half in range(2):
                            p = ps[1 + nxc("a", 2)]
                            for k in range(4):
                                kb.op("pe", lambda e: e.matmul(p[:, :], lhsT=hact[:, k, s * 128:(s + 1) * 128], rhs=wdb[wi][:, k, half * 512:(half + 1) * 512], start=(k == 0), stop=(k == 3)),
                                      r=[hact, wdb[wi]], w=[p], inc=(k == 3))
                            xs = x1[:, s, half * 512:(half + 1) * 512]
                            if ex is None:
                                kb.op("dve", lambda e: e.tensor_tensor(out=xs, in0=xs, in1=p[:, :], op=ALU.add), r=[x1, p], w=[x1])
                            else:
                                kb.op("dve", lambda e: e.scalar_tensor_tensor(out=xs, in0=p[:, :], scalar=gates[:, s, ex:ex + 1], in1=xs, op0=ALU.mult, op1=ALU.add), r=[x1, p, gates], w=[x1])
            kb.dma("sp", "st", x_dst[tok0:tok0 + TBC, :].rearrange("(s p) d -> p s d", p=128), x1[:, :, :], r=[x1], w=[x_dst])
        if moe:
            kb.barrier()
            stackC1.close()
            stackR = ExitStack()
            kb.stack = stackR
            NJ = TH // 128
            CAP = ((TH * 2 // NE) * 3 // 2 + 511) // 512 * 512
            NSR = CAP // 128
            xdisp_d = kb.dram("xdisp_s", [NE * CAP, D], BF16)
            ydisp_d = kb.dram("ydisp_s", [NE * CAP, D], F32)
            h2R = kb.sbuf("h2R", [128, 8, CAP], BF16)
            hactR = kb.sbuf("hactR", [128, 4, CAP], BF16)
            yacc = kb.sbuf("yacc", [128, NSR, D], F32)
            slots = kb.sbuf("slots", [128, NJ, 2], I32)
            wts = kb.sbuf("wts", [128, NJ, 2], F32)
            basec = kb.sbuf("basec", [128, NE], F32)
            eoff = kb.sbuf("eoff", [128, NE], F32)
            selb = kb.sbuf("selb", [128, NE], BF16)
            xa = kb.sbuf("xa", [128, D], F32)
            y1 = kb.sbuf("y1", [128, D], F32)
            y2 = kb.sbuf("y2", [128, D], F32)
            kb.op("dve", lambda e: e.memset(basec[:, :], 0.0), w=[basec])
            for ex in range(NE):
                kb.op("dve", lambda e: e.memset(eoff[:, ex:ex + 1], float(ex * CAP)), w=[eoff])

            def transposed(hbb, dst_ap):
                pT = ps[0]
                pTb = pT.t.ap().bitcast(BF16)
                for c in range(8):
                    kb.op("pe", lambda e: e.transpose(pTb[:, c * 128:(c + 1) * 128], hbb[:, c * 128:(c + 1) * 128], identb[:, :]), r=[hbb, identb], w=[pT], inc=(c == 7))
                kb.op("dve", lambda e: e.tensor_tensor(out=dst_ap, in0=pTb[:, 0:1024].rearrange("p (c t) -> p c t", t=128),
                                                       in1=pl(P_G2, 8).unsqueeze(2).to_broadcast([128, 8, 128]), op=ALU.mult), r=[pT, par], w=[h2R])

            for j in range(NJ):
                hi = nxc("h", 2)
                hbb = h2b[hi]
                kb.gather("gh%d" % hi, hbb[:, :], h2_d.t.ap(), idxt[:, j:j + 1], r=[h2_d, idxt], w=[hbb])
                transposed(hbb, h2R[:, :, 0:128])
                p = ps[1 + nxc("a", 2)]
                for c in range(8):
                    kb.op("pe", lambda e: e.matmul(p[:, 0:NE], lhsT=h2R[:, c, 0:128], rhs=wrb[:, c, :], start=(c == 0), stop=(c == 7)), r=[h2R, wrb], w=[p], inc=(c == 7))
                kb.op("dve", lambda e: e.tensor_tensor(out=rt[:, 0:8], in0=p[:, 0:NE], in1=pl(P_RB, 8), op=ALU.add), r=[p, par], w=[rt])
                kb.op("dve", lambda e: e.max(out=rt[:, 8:16], in_=rt[:, 0:8]), r=[rt], w=[rt])
                kb.op("dve", lambda e: e.tensor_tensor(out=rt[:, 16:17], in0=rt[:, 8:9], in1=rt[:, 9:10], op=ALU.subtract), r=[rt], w=[rt])
                kb.op("act", lambda e: e.activation(out=wts[:, j, 0:1], in_=rt[:, 16:17], func=AF.Sigmoid), r=[rt], w=[wts])
                kb.op("act", lambda e: e.activation(out=wts[:, j, 1:2], in_=rt[:, 16:17], func=AF.Sigmoid, scale=-1.0), r=[rt], w=[wts])
                kb.op("dve", lambda e: e.tensor_scalar(out=rt[:, 24:32], in0=rt[:, 0:8], scalar1=rt[:, 8:9], scalar2=None, op0=ALU.is_equal), r=[rt], w=[rt])
                kb.op("dve", lambda e: e.tensor_scalar(out=rt[:, 32:40], in0=rt[:, 0:8], scalar1=rt[:, 9:10], scalar2=None, op0=ALU.is_equal), r=[rt], w=[rt])
                kb.op("dve", lambda e: e.tensor_tensor(out=rt[:, 40:48], in0=rt[:, 24:32], in1=rt[:, 32:40], op=ALU.add), r=[rt], w=[rt])
                kb.op("dve", lambda e: e.tensor_copy(out=selb[:, :], in_=rt[:, 40:48]), r=[rt], w=[selb])
                pp, ptot = ps[3], ps[5]
                kb.op("pe", lambda e: e.matmul(pp[:, 0:NE], lhsT=trib[:, :], rhs=selb[:, :], start=True, stop=True), r=[trib, selb], w=[pp])
                kb.op("pe", lambda e: e.matmul(ptot[:, 0:NE], lhsT=onesb[:, :], rhs=selb[:, :], start=True, stop=True), r=[onesb, selb], w=[ptot])
                kb.op("dve", lambda e: e.tensor_tensor(out=rt[:, 48:56], in0=pp[:, 0:NE], in1=rt[:, 40:48], op=ALU.subtract), r=[pp, rt], w=[rt])
                kb.op("dve", lambda e: e.tensor_tensor(out=rt[:, 48:56], in0=rt[:, 48:56], in1=basec[:, :], op=ALU.add), r=[rt, basec], w=[rt])
                kb.op("dve", lambda e: e.tensor_tensor(out=rt[:, 48:56], in0=rt[:, 48:56], in1=eoff[:, :], op=ALU.add), r=[rt, eoff], w=[rt])
                kb.op("dve", lambda e: e.tensor_tensor(out=basec[:, :], in0=basec[:, :], in1=ptot[:, 0:NE], op=ALU.add), r=[basec, ptot], w=[basec])
                for k in range(2):
                    kb.op("dve", lambda e: e.tensor_tensor(out=rt[:, 56:64], in0=rt[:, 24 + 8 * k:32 + 8 * k], in1=rt[:, 48:56], op=ALU.mult), r=[rt], w=[rt])
                    kb.op("dve", lambda e: e.tensor_reduce(out=rt[:, 20 + k:21 + k], in_=rt[:, 56:64], axis=mybir.AxisListType.X, op=ALU.add), r=[rt], w=[rt])
                kb.op("dve", lambda e: e.tensor_copy(out=slots[:, j, :], in_=rt[:, 20:22]), r=[rt], w=[slots])
                for k in range(2):
                    kb.ncall += 1
                    kb._wait("pool", kb._deps([hbb, slots], [xdisp_d]))
                    inst = nc.gpsimd.indirect_dma_start(out=xdisp_d.t.ap(), out_offset=bass.IndirectOffsetOnAxis(slots[:, j, k:k + 1], 0), in_=hbb[:, :], in_offset=None)
                    sk = "d:sc%d" % hi
                    if sk not in kb.sems:
                        kb._newsem(sk)
                    kb.cnt[sk] += 16
                    inst.then_inc(kb.sems[sk], 16)
                    kb._mark(sk, kb.cnt[sk], [hbb, slots], [xdisp_d])

            for ex in range(NE):
                for s in range(NSR):
                    hi = nxc("h", 2)
                    hbb = h2b[hi]
                    kb.dma("sp", "lh%d" % hi, hbb[:, :], xdisp_d[ex * CAP + s * 128:ex * CAP + (s + 1) * 128, :], r=[xdisp_d], w=[hbb])
                    transposed(hbb, h2R[:, :, s * 128:(s + 1) * 128])
                Wg, Wu, Wd = moe_g_d.t.ap()[0, ex], moe_u_d.t.ap()[0, ex], moe_d_d.t.ap()[0, ex]
                for fg in range(DFF // 512):
                    wi = nxc("w", 2)
                    kb.dma("pool", "w", wgb[wi][:, :, :], Wg[:, fg * 512:(fg + 1) * 512].rearrange("(c p) f -> p c f", p=128), w=[wgb[wi]])
                    kb.dma("pool", "w", wub[wi][:, :, :], Wu[:, fg * 512:(fg + 1) * 512].rearrange("(c p) f -> p c f", p=128), w=[wub[wi]])
                    kb.dma("pool", "w", wdb[wi][:, :, :], Wd[fg * 512:(fg + 1) * 512, :].rearrange("(k p) d -> p k d", p=128), w=[wdb[wi]])
                    for t4 in range(CAP // 512):
                        tsl = slice(t4 * 512, (t4 + 1) * 512)
                        for k in range(4):
                            pg = ps[3 + nxc("g", 2)]
                            pu = ps[5 + nxc("u", 2)]
                            for c in range(8):
                                kb.op("pe", lambda e: e.matmul(pg[:, :], lhsT=wgb[wi][:, c, k * 128:(k + 1) * 128], rhs=h2R[:, c, tsl], start=(c == 0), stop=(c == 7)), r=[wgb[wi], h2R], w=[pg], inc=(c == 7))
                            for c in range(8):
                                kb.op("pe", lambda e: e.matmul(pu[:, :], lhsT=wub[wi][:, c, k * 128:(k + 1) * 128], rhs=h2R[:, c, tsl], start=(c == 0), stop=(c == 7)), r=[wub[wi], h2R], w=[pu], inc=(c == 7))
                            sg = sgt[nxc("sg", 2)]
                            kb.op("act", lambda e: e.activation(out=sg[:, :], in_=pg[:, :], func=AF.Silu), r=[pg], w=[sg])
                            kb.op("dve", lambda e: e.tensor_tensor(out=hactR[:, k, tsl], in0=sg[:, :], in1=pu[:, :], op=ALU.mult), r=[sg, pu], w=[hactR])
                    for s in range(NSR):
                        for half in range(2):
                            p = ps[1 + nxc("a", 2)]
                            for k in range(4):
                                kb.op("pe", lambda e: e.matmul(p[:, :], lhsT=hactR[:, k, s * 128:(s + 1) * 128], rhs=wdb[wi][:, k, half * 512:(half + 1) * 512], start=(k == 0), stop=(k == 3)),
                                      r=[hactR, wdb[wi]], w=[p], inc=(k == 3))
                            ys = yacc[:, s, half * 512:(half + 1) * 512]
                            if fg == 0:
                                kb.op("dve", lambda e: e.tensor_copy(out=ys, in_=p[:, :]), r=[p], w=[yacc])
                            else:
                                kb.op("dve", lambda e: e.tensor_tensor(out=ys, in0=ys, in1=p[:, :], op=ALU.add), r=[yacc, p], w=[yacc])
                kb.dma("sp", "st", ydisp_d[ex * CAP:(ex + 1) * CAP, :].rearrange("(s p) d -> p s d", p=128), yacc[:, :, :], r=[yacc], w=[ydisp_d])

            for j in range(NJ):
                kb.gather("gx", xa[:, :], x1_d.t.ap(), idxt[:, j:j + 1], r=[x1_d, idxt], w=[xa])
                kb.gather("gy1", y1[:, :], ydisp_d.t.ap(), slots[:, j, 0:1], r=[ydisp_d, slots], w=[y1])
                kb.gather("gy2", y2[:, :], ydisp_d.t.ap(), slots[:, j, 1:2], r=[ydisp_d, slots], w=[y2])
                kb.op("dve", lambda e: e.scalar_tensor_tensor(out=xa[:, :], in0=y1[:, :], scalar=wts[:, j, 0:1], in1=xa[:, :], op0=ALU.mult, op1=ALU.add), r=[xa, y1, wts], w=[xa])
                kb.op("dve", lambda e: e.scalar_tensor_tensor(out=xa[:, :], in0=y2[:, :], scalar=wts[:, j, 1:2], in1=xa[:, :], op0=ALU.mult, op1=ALU.add), r=[xa, y2, wts], w=[xa])
                kb.dma("sp", "st", yout[j * 128:(j + 1) * 128, :], xa[:, :], r=[xa], w=[yout])
            kb.barrier()
            stackR.close()
        else:
            kb.barrier()
            stackC1.close()
        kb.stack = stackC
        kb.barrier()
        stackC.close()
        kb.stack = None

    kb.finish()
    return nc, dbg


def rstd_tm(kb, st, epsc, n):
    kb.op("act", lambda e: e.activation(out=st[:, n:2 * n], in_=st[:, 0:n], func=AF.Ln, bias=epsc[:, 0:1], scale=1.0 / D), r=[st, epsc], w=[st])
    kb.op("act", lambda e: e.activation(out=st[:, n:2 * n], in_=st[:, n:2 * n], func=AF.Exp, scale=-0.5), r=[st], w=[st])


_CACHE = {}


def kernel(**inputs):
    x = np.ascontiguousarray(inputs["x"], dtype=np.float32)
    B, S, _ = x.shape
    key = S
    if key not in _CACHE:
        _CACHE[key] = build(S)[0]
    nc = _CACHE[key]
    consts = make_consts()
    params = make_params(inputs)
    shared = {k: np.ascontiguousarray(inputs[k], dtype=np.float32) for k in
              ("w_in", "w_out", "ffn_w_gate", "ffn_w_up", "ffn_w_down", "moe_router_w", "moe_w_gate", "moe_w_up", "moe_w_down")}
    n = 8
    TH = S // 2
    in_maps = []
    for cid in range(n):
        m = dict(shared)
        m["x"] = x[cid % B]
        m["consts"] = consts
        m["params"] = params
        rank = cid // B
        m["tokidx"] = (rank * TH + np.arange(TH, dtype=np.int32)).reshape(TH // 128, 128).T.copy()
        in_maps.append(m)
    res = run_bass_kernel_spmd(nc, in_maps, core_ids=list(range(n)))
    out = np.stack([np.concatenate([res.results[b]["y"], res.results[b + B]["y"]], axis=0) for b in range(B)], axis=0)
    return out.astype(np.float32)
```
